# Optimizing a Trainium2 kernel written in Bass

```python
import jax, jax.numpy as jnp
from jax import lax
import numpy as np

D_MODEL = 1024
BATCH = 16
SEQ = 4096
DEPTH = 2

GRID_W = 64
HEAD_DIM = 64
ROPE_THETA = 10000.0
EPS = 1e-6
Q_BLOCK = 128
NEG = -1e30

MLA_HEADS = 8
MLA_Q_RANK = 256
MLA_KV_RANK = 128
MLA_NOPE = 64
MLA_ROPE = 32
MLA_V = 64
GQA_HEADS = 8
GQA_KV_HEADS = 2
EVEN_SPLITS = tuple(int(v) for v in np.cumsum([MLA_Q_RANK, MLA_KV_RANK, MLA_ROPE,
                                                GQA_HEADS * HEAD_DIM, GQA_KV_HEADS * HEAD_DIM]))
EVEN_IN = EVEN_SPLITS[-1] + GQA_KV_HEADS * HEAD_DIM
EVEN_MIX = MLA_HEADS * MLA_V + GQA_HEADS * HEAD_DIM
DIL_HEADS = D_MODEL // HEAD_DIM
DIL_PATTERNS = ((128, 1), (512, 4), (2048, 16))
DIL_BLOCK = 64
D_FF = 2816
N_EXPERTS = 8
TOP_K = 2
D_FF_EXPERT = 3584
MOE_BLOCK = 256
N_EVEN = (DEPTH + 1) // 2
N_ODD = DEPTH // 2

kernel_name = "hybrid_mla_axialgqa_dilated_moe_encoder"


def rmsnorm(x, g):
    x32 = x.astype(jnp.float32)
    y = x32 * lax.rsqrt(jnp.mean(x32 * x32, axis=-1, keepdims=True) + EPS)
    return (y * g.astype(jnp.float32)).astype(x.dtype)


def modulate(x, g, shift, scale):
    return rmsnorm(x, g) * (1.0 + scale[:, None, :]) + shift[:, None, :]


def rope(x, pos):
    dh = x.shape[-1]
    inv = ROPE_THETA ** (-jnp.arange(0, dh, 2, dtype=jnp.float32) / dh)
    ang = pos.astype(jnp.float32)[:, None] * inv[None, :]
    cos = jnp.cos(ang)[None, :, None, :]
    sin = jnp.sin(ang)[None, :, None, :]
    x1, x2 = jnp.split(x.astype(jnp.float32), 2, axis=-1)
    return jnp.concatenate([x1 * cos - x2 * sin, x1 * sin + x2 * cos], axis=-1).astype(x.dtype)


def axial_rope(x, row_idx, col_idx):
    half = x.shape[-1] // 2
    return jnp.concatenate([rope(x[..., :half], row_idx), rope(x[..., half:], col_idx)], axis=-1)


def blocked_attention(q, k, v):
    B, S, H, dq = q.shape
    Hk = k.shape[2]
    R = H // Hk
    dv = v.shape[-1]
    nblk = S // Q_BLOCK
    qb = q.reshape(B, nblk, Q_BLOCK, Hk, R, dq).transpose(1, 0, 2, 3, 4, 5)
    scale = dq ** -0.5

    def one_block(qblk):
        s = jnp.einsum('bqgrd,bkgd->bgrqk', qblk, k, preferred_element_type=jnp.float32) * scale
        p = jax.nn.softmax(s, axis=-1).astype(v.dtype)
        return jnp.einsum('bgrqk,bkgd->bqgrd', p, v)

    out = lax.map(one_block, qb)
    return out.transpose(1, 0, 2, 3, 4, 5).reshape(B, S, H, dv)


def mixer_even(h, w_in, q_norm, w_uq, kv_norm, w_ukv, mla_q_gain, mla_k_gain,
               gqa_q_gain, gqa_k_gain, w_out):
    B, S, _ = h.shape
    pos = jnp.arange(S, dtype=jnp.int32)
    rows = S // GRID_W
    row_idx = jnp.repeat(jnp.arange(rows, dtype=jnp.int32), GRID_W)
    col_idx = jnp.tile(jnp.arange(GRID_W, dtype=jnp.int32), rows)
    c_q, c_kv, k_pe, q_b, k_b, v_b = jnp.split(h @ w_in, EVEN_SPLITS, axis=-1)

    q_a = (rmsnorm(c_q, q_norm) @ w_uq).reshape(B, S, MLA_HEADS, MLA_NOPE + MLA_ROPE)
    kv = (rmsnorm(c_kv, kv_norm) @ w_ukv).reshape(B, S, MLA_HEADS, MLA_NOPE + MLA_V)
    k_nope, v_a = jnp.split(kv, [MLA_NOPE], axis=-1)
    k_a = jnp.concatenate(
        [k_nope, jnp.broadcast_to(k_pe[:, :, None, :], (B, S, MLA_HEADS, MLA_ROPE))], axis=-1)
    q_a = rmsnorm(q_a, mla_q_gain)
    k_a = rmsnorm(k_a, mla_k_gain)
    q_a = jnp.concatenate([q_a[..., :MLA_NOPE], rope(q_a[..., MLA_NOPE:], pos)], axis=-1)
    k_a = jnp.concatenate([k_a[..., :MLA_NOPE], rope(k_a[..., MLA_NOPE:], pos)], axis=-1)
    o_a = blocked_attention(q_a, k_a, v_a).reshape(B, S, MLA_HEADS * MLA_V)

    q_b = rmsnorm(q_b.reshape(B, S, GQA_HEADS, HEAD_DIM), gqa_q_gain)
    k_b = rmsnorm(k_b.reshape(B, S, GQA_KV_HEADS, HEAD_DIM), gqa_k_gain)
    v_b = v_b.reshape(B, S, GQA_KV_HEADS, HEAD_DIM)
    q_b = axial_rope(q_b, row_idx, col_idx)
    k_b = axial_rope(k_b, row_idx, col_idx)
    o_b = blocked_attention(q_b, k_b, v_b).reshape(B, S, GQA_HEADS * HEAD_DIM)

    return jnp.concatenate([o_a, o_b], axis=-1) @ w_out


def dilated_branch(q, k, v, dilation, half):
    B, S, H, dh = q.shape
    L = S // dilation
    nb = -(-L // DIL_BLOCK)
    Lp = nb * DIL_BLOCK
    qs = jnp.pad(q.reshape(B, L, dilation, H, dh), ((0, 0), (0, Lp - L), (0, 0), (0, 0), (0, 0)))
    pad_kv = ((0, 0), (DIL_BLOCK, Lp - L + DIL_BLOCK), (0, 0), (0, 0), (0, 0))
    ks = jnp.pad(k.reshape(B, L, dilation, H, dh), pad_kv)
    vs = jnp.pad(v.reshape(B, L, dilation, H, dh), pad_kv)
    a = jnp.arange(DIL_BLOCK)[:, None]
    b = jnp.arange(3 * DIL_BLOCK)[None, :]
    in_band = jnp.abs(b - DIL_BLOCK - a) <= half
    scale = dh ** -0.5

    def one_block(i):
        qb = lax.dynamic_slice_in_dim(qs, i * DIL_BLOCK, DIL_BLOCK, axis=1)
        kb = lax.dynamic_slice_in_dim(ks, i * DIL_BLOCK, 3 * DIL_BLOCK, axis=1)
        vb = lax.dynamic_slice_in_dim(vs, i * DIL_BLOCK, 3 * DIL_BLOCK, axis=1)
        kj = (i - 1) * DIL_BLOCK + b
        valid = in_band & (kj >= 0) & (kj < L)
        s = jnp.einsum('bqrhd,bkrhd->brhqk', qb, kb, preferred_element_type=jnp.float32) * scale
        s = jnp.where(valid, s, NEG)
        m = jnp.max(s, axis=-1, keepdims=True)
        p = jnp.exp(s - m)
        den = jnp.sum(p, axis=-1, keepdims=True)
        o = jnp.einsum('brhqk,bkrhd->bqrhd', (p / den).astype(v.dtype), vb)
        lse = (m + jnp.log(den))[..., 0]
        return o, lse.transpose(0, 3, 1, 2)

    o, lse = lax.map(one_block, jnp.arange(nb))
    o = o.transpose(1, 0, 2, 3, 4, 5).reshape(B, Lp, dilation, H, dh)[:, :L].reshape(B, S, H, dh)
    lse = lse.transpose(1, 0, 2, 3, 4).reshape(B, Lp, dilation, H)[:, :L].reshape(B, S, H)
    return o, lse


def mixer_odd(h, w_qkv, q_gain, k_gain, w_out):
    B, S, D = h.shape
    pos = jnp.arange(S, dtype=jnp.int32)
    q, k, v = jnp.split(h @ w_qkv, 3, axis=-1)
    q = rope(rmsnorm(q.reshape(B, S, DIL_HEADS, HEAD_DIM), q_gain), pos)
    k = rope(rmsnorm(k.reshape(B, S, DIL_HEADS, HEAD_DIM), k_gain), pos)
    v = v.reshape(B, S, DIL_HEADS, HEAD_DIM)
    outs, lses = [], []
    for window, dilation in DIL_PATTERNS:
        o, l = dilated_branch(q, k, v, dilation, window // (2 * dilation))
        outs.append(o)
        lses.append(l)
    w = jax.nn.softmax(jnp.stack(lses, axis=0), axis=0)
    o = sum(w[g][..., None] * outs[g].astype(jnp.float32) for g in range(len(DIL_PATTERNS)))
    return o.astype(h.dtype).reshape(B, S, D) @ w_out


def swiglu(h, wg, wu, wd):
    return (jax.nn.silu(h @ wg) * (h @ wu)) @ wd


def moe_swiglu(h, router, wg, wu, wd):
    B, S, D = h.shape
    T = B * S
    xt = h.reshape(T, D)
    logits = jnp.einsum('td,de->te', xt, router, preferred_element_type=jnp.float32)
    top_val, top_idx = lax.top_k(logits, TOP_K)
    gates = jax.nn.softmax(top_val, axis=-1)
    A = T * TOP_K
    e_flat = top_idx.reshape(A).astype(jnp.int32)
    g_flat = gates.reshape(A)
    tok_flat = jnp.repeat(jnp.arange(T, dtype=jnp.int32), TOP_K)
    order = jnp.argsort(e_flat)
    e_s, tok_s, g_s = e_flat[order], tok_flat[order], g_flat[order]
    counts = jnp.bincount(e_flat, length=N_EXPERTS)
    starts = jnp.cumsum(counts) - counts
    pcounts = (counts + MOE_BLOCK - 1) // MOE_BLOCK * MOE_BLOCK
    pends = jnp.cumsum(pcounts)
    pstarts = pends - pcounts
    dest = pstarts[e_s] + jnp.arange(A, dtype=jnp.int32) - starts[e_s]
    n_blk = -(-A // MOE_BLOCK) + N_EXPERTS
    P = n_blk * MOE_BLOCK
    tok_pad = jnp.zeros((P,), jnp.int32).at[dest].set(tok_s)
    g_pad = jnp.zeros((P,), jnp.float32).at[dest].set(g_s)
    blk_e = jnp.minimum(jnp.searchsorted(pends, jnp.arange(n_blk, dtype=jnp.int32) * MOE_BLOCK,
                                         side='right'), N_EXPERTS - 1)

    def one_block(args):
        tok, g, e = args
        xb = xt[tok]
        yb = (jax.nn.silu(xb @ wg[e]) * (xb @ wu[e])) @ wd[e]
        return (yb.astype(jnp.float32) * g[:, None]).astype(h.dtype)

    yb = lax.map(one_block, (tok_pad.reshape(n_blk, MOE_BLOCK), g_pad.reshape(n_blk, MOE_BLOCK), blk_e))
    y = jnp.zeros_like(xt).at[tok_pad].add(yb.reshape(P, D))
    return y.reshape(B, S, D)


def setup_inputs(seed: int = 0) -> dict:
    key = jax.random.key(seed)
    ks = iter(jax.random.split(key, 64))
    D = D_MODEL

    def w(shape, fan_in, s=1.0):
        return s * fan_in ** -0.5 * jax.random.normal(next(ks), shape, jnp.float32)

    def gain(shape):
        return 1.0 + 0.1 * jax.random.normal(next(ks), shape, jnp.float32)

    def bias(shape):
        return 0.02 * jax.random.normal(next(ks), shape, jnp.float32)

    return {
        "x": jax.random.normal(next(ks), (BATCH, SEQ, D), jnp.float32),
        "c": jax.random.normal(next(ks), (BATCH, D), jnp.float32),
        "ada_even_w": w((N_EVEN, D, 6 * D), D, 0.5),
        "ada_even_b": bias((N_EVEN, 6 * D)),
        "norm_even_mix": gain((N_EVEN, D)),
        "norm_even_ffn": gain((N_EVEN, D)),
        "even_w_in": w((N_EVEN, D, EVEN_IN), D),
        "mla_q_norm": gain((N_EVEN, MLA_Q_RANK)),
        "mla_w_uq": w((N_EVEN, MLA_Q_RANK, MLA_HEADS * (MLA_NOPE + MLA_ROPE)), MLA_Q_RANK),
        "mla_kv_norm": gain((N_EVEN, MLA_KV_RANK)),
        "mla_w_ukv": w((N_EVEN, MLA_KV_RANK, MLA_HEADS * (MLA_NOPE + MLA_V)), MLA_KV_RANK),
        "mla_q_gain": gain((N_EVEN, MLA_NOPE + MLA_ROPE)),
        "mla_k_gain": gain((N_EVEN, MLA_NOPE + MLA_ROPE)),
        "gqa_q_gain": gain((N_EVEN, HEAD_DIM)),
        "gqa_k_gain": gain((N_EVEN, HEAD_DIM)),
        "even_w_out": w((N_EVEN, EVEN_MIX, D), EVEN_MIX),
        "ffn_w_gate": w((N_EVEN, D, D_FF), D),
        "ffn_w_up": w((N_EVEN, D, D_FF), D),
        "ffn_w_down": w((N_EVEN, D_FF, D), D_FF),
        "ada_odd_w": w((N_ODD, D, 6 * D), D, 0.5),
        "ada_odd_b": bias((N_ODD, 6 * D)),
        "norm_odd_mix": gain((N_ODD, D)),
        "norm_odd_ffn": gain((N_ODD, D)),
        "dil_w_qkv": w((N_ODD, D, 3 * DIL_HEADS * HEAD_DIM), D),
        "dil_q_gain": gain((N_ODD, HEAD_DIM)),
        "dil_k_gain": gain((N_ODD, HEAD_DIM)),
        "dil_w_out": w((N_ODD, DIL_HEADS * HEAD_DIM, D), DIL_HEADS * HEAD_DIM),
        "moe_router": w((N_ODD, D, N_EXPERTS), D),
        "moe_w_gate": w((N_ODD, N_EXPERTS, D, D_FF_EXPERT), D),
        "moe_w_up": w((N_ODD, N_EXPERTS, D, D_FF_EXPERT), D),
        "moe_w_down": w((N_ODD, N_EXPERTS, D_FF_EXPERT, D), D_FF_EXPERT),
    }


def reference(x, c, ada_even_w, ada_even_b, norm_even_mix, norm_even_ffn, even_w_in,
              mla_q_norm, mla_w_uq, mla_kv_norm, mla_w_ukv, mla_q_gain, mla_k_gain,
              gqa_q_gain, gqa_k_gain, even_w_out, ffn_w_gate, ffn_w_up, ffn_w_down,
              ada_odd_w, ada_odd_b, norm_odd_mix, norm_odd_ffn, dil_w_qkv, dil_q_gain,
              dil_k_gain, dil_w_out, moe_router, moe_w_gate, moe_w_up, moe_w_down):
    B, S, D = x.shape
    sc = jax.nn.silu(c)
    for layer in range(DEPTH):
        i = layer // 2
        if layer % 2 == 0:
            mod = (sc @ ada_even_w[i] + ada_even_b[i]).reshape(B, 6, D)
            h = modulate(x, norm_even_mix[i], mod[:, 0], mod[:, 1])
            x = x + mod[:, 2][:, None, :] * mixer_even(
                h, even_w_in[i], mla_q_norm[i], mla_w_uq[i], mla_kv_norm[i], mla_w_ukv[i],
                mla_q_gain[i], mla_k_gain[i], gqa_q_gain[i], gqa_k_gain[i], even_w_out[i])
            h = modulate(x, norm_even_ffn[i], mod[:, 3], mod[:, 4])
            x = x + mod[:, 5][:, None, :] * swiglu(h, ffn_w_gate[i], ffn_w_up[i], ffn_w_down[i])
        else:
            mod = (sc @ ada_odd_w[i] + ada_odd_b[i]).reshape(B, 6, D)
            h = modulate(x, norm_odd_mix[i], mod[:, 0], mod[:, 1])
            x = x + mod[:, 2][:, None, :] * mixer_odd(
                h, dil_w_qkv[i], dil_q_gain[i], dil_k_gain[i], dil_w_out[i])
            h = modulate(x, norm_odd_ffn[i], mod[:, 3], mod[:, 4])
            x = x + mod[:, 5][:, None, :] * moe_swiglu(
                h, moe_router[i], moe_w_gate[i], moe_w_up[i], moe_w_down[i])
    return x
```

```python
import os
import numpy as np
from contextlib import ExitStack
import concourse.bass as bass
import concourse.mybir as mybir
from concourse.bass_utils import run_bass_kernel_spmd

F32 = mybir.dt.float32
BF16 = mybir.dt.bfloat16
AF = mybir.ActivationFunctionType
ALU = mybir.AluOpType
AX = mybir.AxisListType

NCORES = 8
D = 1024
S = 4096
NB = 2
T = NB * S
TT = 512
NT = T // TT
EPS = 1e-6
DFF = 2816
NE = 8
NE_IN = 1 if os.environ.get('K_SMALLMOE') else 8
DFE = 3584
THETA = 10000.0


_UN = [0]


def _un(n):
    _UN[0] += 1
    return f"t{_UN[0]}_{n}"


class Tok:
    __slots__ = ("name", "w", "r", "x")

    def __init__(self, name="", x=False):
        self.name = name
        self.w = []
        self.r = []
        self.x = x


def toks(n, name=""):
    return [Tok(f"{name}{i}") for i in range(n)]


class Ring:
    def __init__(self, items):
        self.items = list(items)
        self.i = 0

    def next(self):
        it = self.items[self.i % len(self.items)]
        self.i += 1
        return it


class Sched:
    ROT = 60000

    def __init__(self, nc, es):
        self.nc = nc
        self.es = es
        self.eng = {"pe": nc.tensor, "act": nc.scalar, "dve": nc.vector, "pool": nc.gpsimd, "sp": nc.sync}
        self.esem = {}
        self.known = {e: {} for e in self.eng}
        self.nsem = 0
        for e in self.eng:
            self.esem[e] = [self._newsem(e), 0]
        self.dq = {}
        for q, n in (("sp", 8), ("pool", 6), ("act", 2)):
            self.dq[q] = [[self._newsem("d" + q), 0] for _ in range(n)]
        self.dqi = {q: 0 for q in self.dq}
        self.all_dsems = [s for q in self.dq for s in self.dq[q]]
        self.old_esems = []

    def _newsem(self, name):
        self.nsem += 1
        return self.es.enter_context(self.nc.semaphore(f"s_{name}_{self.nsem}"))

    def _wait(self, e, evs):
        k = self.known[e]
        h = self.eng[e]
        own = self.esem[e][0] if e == "pe" else None
        for (sem, val) in evs:
            if val <= 0 or sem is own:
                continue
            key = id(sem)
            if k.get(key, 0) >= val:
                continue
            h.wait_ge(sem, val)
            k[key] = val

    def _deps(self, reads, writes, e=None):
        evs = []
        own = self.esem[e][0] if e in self.esem else None
        for t in reads:
            evs += t.w
            if t.x:
                evs += [ev for ev in t.r if ev[0] is not own]
        for t in writes:
            evs += t.w
            evs += t.r
        return evs

    @staticmethod
    def _record(ev, reads, writes):
        for t in reads:
            t.r = [x for x in t.r if x[0] is not ev[0]] + [ev]
        for t in writes:
            t.w = [ev]
            t.r = []

    def op(self, e, fn, reads=(), writes=()):
        self._wait(e, self._deps(reads, writes, e))
        inst = fn(self.eng[e])
        st = self.esem[e]
        if st[1] >= self.ROT:
            self.old_esems.append((st[0], st[1]))
            st[0] = self._newsem(e)
            st[1] = 0
        st[1] += 1
        inst.then_inc(st[0], 1)
        self._record((st[0], st[1]), reads, writes)

    def dma(self, q, pairs, reads=(), writes=()):
        if os.environ.get("K_NOSTORE") and len(reads) > 0:
            return
        evs = self._deps(reads, writes)
        slot = self.dq[q][self.dqi[q] % len(self.dq[q])]
        self.dqi[q] += 1
        evs.append((slot[0], slot[1]))
        self._wait(q, evs)
        h = self.eng[q]
        for (o, i) in pairs:
            h.dma_start(out=o, in_=i).then_inc(slot[0], 16)
        slot[1] += 16 * len(pairs)
        self._record((slot[0], slot[1]), reads, writes)

    def dma_fn(self, q, fns, reads=(), writes=()):
        evs = self._deps(reads, writes)
        slot = self.dq[q][self.dqi[q] % len(self.dq[q])]
        self.dqi[q] += 1
        evs.append((slot[0], slot[1]))
        self._wait(q, evs)
        for fn in fns:
            fn(self.eng[q]).then_inc(slot[0], 16)
        slot[1] += 16 * len(fns)
        self._record((slot[0], slot[1]), reads, writes)

    def barrier(self, engines=None):
        evs = [(st[0], st[1]) for st in self.esem.values()]
        evs += list(self.old_esems)
        evs += [(s[0], s[1]) for s in self.all_dsems]
        for e in (engines or self.eng):
            self._wait(e, evs)


def _perm_rot(n, half):
    i = np.arange(n)
    return np.where(i % (2 * half) < half, i + half, i - half)


PERM_MLA32 = _perm_rot(32, 16)
PERM_AX64 = _perm_rot(64, 16)
PERM_L164 = _perm_rot(64, 32)


def _rope_tables():
    pos = np.arange(S, dtype=np.float32)
    row = (np.arange(S) // 64).astype(np.float32)
    col = (np.arange(S) % 64).astype(np.float32)

    def inv(dh):
        return (THETA ** (-np.arange(0, dh, 2, dtype=np.float32) / dh)).astype(np.float32)

    c_mla = np.ones((96, S), np.float32)
    s_mla = np.zeros((96, S), np.float32)
    iv = inv(32)
    for i in range(32):
        ang = pos * iv[i % 16]
        c_mla[64 + i] = np.cos(ang)
        s_mla[64 + i] = (-1.0 if i < 16 else 1.0) * np.sin(ang)
    c_ax = np.zeros((128, S), np.float32)
    s_ax = np.zeros((128, S), np.float32)
    for p in range(128):
        i = p % 64
        blk, ii = i // 32, i % 32
        ang = (row if blk == 0 else col) * iv[ii % 16]
        c_ax[p] = np.cos(ang)
        s_ax[p] = (-1.0 if ii < 16 else 1.0) * np.sin(ang)
    c_l1 = np.zeros((128, S), np.float32)
    s_l1 = np.zeros((128, S), np.float32)
    iv64 = inv(64)
    for p in range(128):
        i = p % 64
        ang = pos * iv64[i % 32]
        c_l1[p] = np.cos(ang)
        s_l1[p] = (-1.0 if i < 32 else 1.0) * np.sin(ang)
    return c_mla, s_mla, c_ax, s_ax, c_l1, s_l1


def _masks():
    m = np.zeros((128, 8, 512), np.float32)
    i = np.arange(128)[:, None]
    v = np.arange(256)[None, :]
    mg = ((v >= i) & (v <= i + 128)).astype(np.float32)
    m[:, 0, 0:256] = mg
    m[:, 1, 0:256] = mg * (i >= 64)
    m[:, 2, 0:256] = mg * (i < 64)
    return m


VCOLS = {}


def _pack_vecs(inp):
    cols = []

    def add(name, arr):
        VCOLS[name] = (sum(c.shape[1] for c in cols), arr.shape[1])
        cols.append(np.ascontiguousarray(arr, dtype=np.float32))

    def chunked(v):
        return v.reshape(-1, 128).T

    def pad128(v):
        o = np.zeros((128,), np.float32)
        o[: v.shape[0]] = v
        return o[:, None]

    add("g_mix0", chunked(inp["norm_even_mix"][0]))
    add("g_ffn0", chunked(inp["norm_even_ffn"][0]))
    add("g_mix1", chunked(inp["norm_odd_mix"][0]))
    add("g_ffn1", chunked(inp["norm_odd_ffn"][0]))
    add("qn", chunked(inp["mla_q_norm"][0]))
    add("kvn", chunked(inp["mla_kv_norm"][0]))
    gq = inp["mla_q_gain"][0]
    gk = inp["mla_k_gain"][0]
    p96 = np.concatenate([np.arange(64), 64 + PERM_MLA32])
    add("gqa96", pad128(gq))
    add("gqa96p", pad128(gq[p96]))
    add("gka96", pad128(gk))
    add("gka96p", pad128(gk[p96]))
    for nm, key, perm in (("gqb", "gqa_q_gain", PERM_AX64), ("gkb", "gqa_k_gain", PERM_AX64),
                          ("gq1", "dil_q_gain", PERM_L164), ("gk1", "dil_k_gain", PERM_L164)):
        g = inp[key][0]
        add(nm, np.tile(g, 2)[:, None])
        add(nm + "p", np.tile(g[perm], 2)[:, None])
    add("b0", chunked(inp["ada_even_b"][0]))
    add("b1", chunked(inp["ada_odd_b"][0]))
    return np.concatenate(cols, axis=1)


_CONST_CACHE = {}


def _consts():
    if not _CONST_CACHE:
        c_mla, s_mla, c_ax, s_ax, c_l1, s_l1 = _rope_tables()
        sel = np.zeros((8, 1024), np.float32)
        for e in range(8):
            sel[e, e * 128:(e + 1) * 128] = 1.0
        tri = np.triu(np.ones((128, 128), np.float32), k=1)
        cst = np.zeros((128, 64), np.float32)
        cst[:, 0] = np.arange(128)
        cst[:, 1:41] = 512.0 * np.arange(40)[None, :]
        _CONST_CACHE.update(dict(tri=tri, cst=cst))
        _CONST_CACHE.update(dict(c_mla=c_mla, s_mla=s_mla, c_ax=c_ax, s_ax=s_ax, c_l1=c_l1, s_l1=s_l1,
                                 masks=_masks(), ident=np.eye(128, dtype=np.float32), sel=sel))
    return _CONST_CACHE


def _shared_inputs(inp):
    f = lambda a: np.ascontiguousarray(a, dtype=np.float32)
    w_in = inp["even_w_in"][0]
    qb0, kb0 = 416, 928
    pq = np.concatenate([qb0 + 64 * h + PERM_AX64 for h in range(8)])
    pk = np.concatenate([kb0 + 64 * h + PERM_AX64 for h in range(2)])
    w_in_perm = w_in[:, np.concatenate([pq, pk])]
    w_kpe2 = np.concatenate([w_in[:, 320:416], w_in[:, 320:384], w_in[:, 384 + PERM_MLA32]], axis=1)
    w_uq = inp["mla_w_uq"][0]
    puq = np.concatenate([np.concatenate([96 * h + np.arange(64), 96 * h + 64 + PERM_MLA32]) for h in range(8)])
    w_qkv = inp["dil_w_qkv"][0]
    pqk = np.concatenate([64 * h + PERM_L164 for h in range(32)])
    sh = dict(
        vecs=_pack_vecs(inp),
        ada_w0=f(inp["ada_even_w"][0]), ada_w1=f(inp["ada_odd_w"][0]),
        w_in=f(w_in), w_in_perm=f(w_in_perm), w_kpe2=f(w_kpe2),
        w_uq=f(w_uq), w_uq_perm=f(w_uq[:, puq]), w_ukv=f(inp["mla_w_ukv"][0]),
        w_out0=f(inp["even_w_out"][0]),
        wg0=f(inp["ffn_w_gate"][0]), wu0=f(inp["ffn_w_up"][0]), wd0=f(inp["ffn_w_down"][0]),
        w_qkv=f(w_qkv), w_qk_perm=f(w_qkv[:, pqk]), w_out1=f(inp["dil_w_out"][0]),
        router=f(inp["moe_router"][0]),
        moe_wg=f(inp["moe_w_gate"][0][:NE_IN]), moe_wu=f(inp["moe_w_up"][0][:NE_IN]), moe_wd=f(inp["moe_w_down"][0][:NE_IN]),
    )
    sh.update(_consts())
    return sh


INPUT_SHAPES = dict(
    xT=[D, T], cT=[128, 8, NB], vecs=None,
    ada_w0=[D, 6 * D], ada_w1=[D, 6 * D],
    w_in=[D, 1184], w_in_perm=[D, 640], w_kpe2=[D, 192],
    w_uq=[256, 768], w_uq_perm=[256, 768], w_ukv=[128, 1024],
    w_out0=[D, D], wg0=[D, DFF], wu0=[D, DFF], wd0=[DFF, D],
    w_qkv=[D, 3 * D], w_qk_perm=[D, 2 * D], w_out1=[D, D],
    router=[D, NE], moe_wg=[NE_IN, D, DFE], moe_wu=[NE_IN, D, DFE], moe_wd=[NE_IN, DFE, D],
    c_mla=[96, S], s_mla=[96, S], c_ax=[128, S], s_ax=[128, S], c_l1=[128, S], s_l1=[128, S],
    masks=[128, 8, 512], ident=[128, 128], sel=[8, 1024], tri=[128, 128], cst=[128, 64],
)


class Prog:
    def __init__(self, dbg=(), nvec=None):
        self.nc = nc = bass.Bass("TRN2", target_bir_lowering=False)
        self.dbg = set(dbg)
        self.I = {}
        for name, shp in INPUT_SHAPES.items():
            if shp is None:
                shp = [128, nvec]
            self.I[name] = nc.dram_tensor(name, list(shp), F32, kind="ExternalInput").ap()
        self.out = nc.dram_tensor("outT", [D, T], F32, kind="ExternalOutput").ap()
        self.scr = {}

    def scratch(self, name, shape, dtype):
        kind = "ExternalOutput" if name in self.dbg else "Internal"
        ap = self.nc.dram_tensor(name, list(shape), dtype, kind=kind).ap()
        self.scr[name] = ap
        return ap

    def vcol(self, name, j=0, n=1):
        o, w = VCOLS[name]
        return self.vecs[:, o + j:o + j + n]

    def build(self, phases):
        nc = self.nc
        with ExitStack() as es:
            self.es = es
            self.S = Sc = Sched(nc, es)
            I_ = self.I
            sb = lambda n, s, d: es.enter_context(nc.sbuf_tensor(_un(n), s, d))
            nvec = self.I["vecs"].shape[1]
            self.vecs = sb("vecs", [128, nvec], F32)
            self.t_vecs = Tok("vecs")
            Sc.dma("sp", [(self.vecs[:], self.I["vecs"][:, :])], writes=[self.t_vecs])
            self.ones_bf = sb("ones_bf", [128, 128], BF16)
            self.bd_bf = sb("bd_bf", [128, 128], BF16)
            self.ones_f = sb("ones_f", [128, 64], F32)
            self.eps_t = sb("eps_t", [128, 1], F32)
            self.t_const = Tok("const")
            Sc.op("dve", lambda e: e.memset(self.ones_bf[:], 1.0), writes=[self.t_const])
            Sc.op("dve", lambda e: e.memset(self.bd_bf[:], 0.0), writes=[self.t_const])
            Sc.op("dve", lambda e: e.memset(self.bd_bf[0:64, 0:64], 1.0), writes=[self.t_const])
            Sc.op("dve", lambda e: e.memset(self.bd_bf[64:128, 64:128], 1.0), writes=[self.t_const])
            Sc.op("dve", lambda e: e.memset(self.ones_f[:], 1.0), writes=[self.t_const])
            Sc.op("dve", lambda e: e.memset(self.eps_t[:], EPS), writes=[self.t_const])
            self.M = [sb(f"M{l}", [128, 48, NB], F32) for l in range(2)]
            self.A = [sb(f"A{l}", [128, 2, 8, NB], F32) for l in range(2)]
            self.t_mod = Tok("mod")
            self.scratch("qaT", [768, T], BF16)
            self.scratch("kaT", [768, T], BF16)
            self.scratch("vA", [T, 512], BF16)
            self.scratch("qbT", [512, T], BF16)
            self.scratch("kbT", [128, T], BF16)
            self.scratch("vB", [T, 128], BF16)
            self.scratch("at0", [D, T], BF16)
            self.scratch("x2T", [D, T], F32)
            self.scratch("q1T", [D, T], BF16)
            self.scratch("k1T", [D, T], BF16)
            self.scratch("v1", [T, D], BF16)
            self.scratch("at1", [D, T], BF16)
            self.scratch("wg0b", [D, DFF], BF16)
            self.scratch("wu0b", [D, DFF], BF16)
            self.scratch("wd0b", [DFF, D], BF16)
            self.scratch("mwg", [NE, D, DFE], BF16)
            self.scratch("mwu", [NE, D, DFE], BF16)
            self.scratch("mwd", [NE, DFE, D], BF16)
            NBLK = 40
            self.scratch("x3T", [D, T], F32)
            self.scratch("h_tok", [T, D], BF16)
            self.scratch("Xs", [NBLK * 512, D], BF16)
            self.scratch("Ys", [NBLK * 512, D], F32)
            self.scratch("mwg2", [NE * 14 * 128, 2048], BF16)
            self.scratch("mwu2", [NE * 14 * 128, 2048], BF16)
            self.scratch("mwd2", [NE * 14 * 128, 2048], BF16)
            self.rt_m1 = sb("rt_m1", [128, 64, NE], F32)
            self.rt_m2 = sb("rt_m2", [128, 64, NE], F32)
            self.rt_g = sb("rt_g", [128, 64, 2], F32)
            self.rt_s = sb("rt_s", [128, 2, 64], mybir.dt.int32)
            self.rt_w = sb("rt_w", [128, NBLK, 14], mybir.dt.int32)
            self.t_rt = Tok("rt")
            self.cast_jobs = []
            for dn, sn, rows in (("wg0b", "wg0", D), ("wu0b", "wu0", D), ("wd0b", "wd0", DFF)):
                for r in range(0, rows, 128):
                    self.cast_jobs.append((self.scr[dn][r:r + 128, :], I_[sn][r:r + 128, :]))
            SORTED = not os.environ.get("K_DENSE")
            for e_ in range(NE_IN):
                if not SORTED:
                    for dn, sn, rows in (("mwg", "moe_wg", D), ("mwu", "moe_wu", D), ("mwd", "moe_wd", DFE)):
                        for r in range(0, rows, 128):
                            self.cast_jobs.append((self.scr[dn][e_, r:r + 128, :], I_[sn][e_, r:r + 128, :]))
                    continue
                for dn, sn in (("mwg2", "moe_wg"), ("mwu2", "moe_wu")):
                    dv = self.scr[dn].rearrange("(e fg p) (kc c) -> e kc p fg c", e=NE, fg=14, p=128, kc=8)
                    for kc in range(8):
                        self.cast_jobs.append((dv[e_, kc], I_[sn][e_, kc * 128:(kc + 1) * 128, :], (14, 256)))
                dv = self.scr["mwd2"].rearrange("(e h g p) (fl c) -> e g fl p h c", e=NE, h=2, g=7, p=128, fl=4)
                for fc in range(28):
                    self.cast_jobs.append((dv[e_, fc // 4, fc % 4], I_["moe_wd"][e_, fc * 128:(fc + 1) * 128, :], (2, 512)))
            for ph in phases:
                getattr(self, "phase_" + ph)()
                Sc.barrier()
            Sc.barrier()

    def rstd_from_ss(self, ss_ps, t_ss, np_, dim, tmp, t_tmp, out, t_out, n=TT):
        Sc = self.S
        Sc.op("act", lambda e: e.activation(out=tmp[0:np_, 0:n], in_=ss_ps[0:np_, 0:n], func=AF.Ln,
                                            bias=self.eps_t[0:np_, :], scale=1.0 / dim),
              reads=[t_ss, self.t_const], writes=[t_tmp])
        Sc.op("act", lambda e: e.activation(out=out[0:np_, 0:n], in_=tmp[0:np_, 0:n], func=AF.Exp, scale=-0.5),
              reads=[t_tmp], writes=[t_out])

    def norm_tile(self, R, xt, t_x, l, n, b, hT, t_h, h32=None, t_h32=None):
        Sc = self.S
        ss, t_ss = R["ss"].next()
        for c in range(8):
            sq, t_sq = R["sq"].next()
            Sc.op("act", lambda e: e.activation(out=sq[:], in_=xt[:, c, :], func=AF.Square), reads=[t_x[c]], writes=[t_sq])
            Sc.op("pe", lambda e: e.matmul(ss[:], lhsT=self.ones_bf[:], rhs=sq[:], start=(c == 0), stop=(c == 7)),
                  reads=[t_sq, self.t_const], writes=[t_ss])
        tmp, t_tmp = R["f32"].next()
        rs, t_rs = R["rstd"].next()
        self.rstd_from_ss(ss, t_ss, 128, float(D), tmp, t_tmp, rs, t_rs)
        for c in range(8):
            tm, t_tm = R["f32"].next()
            Sc.op("dve", lambda e: e.tensor_tensor(out=tm[:], in0=xt[:, c, :], in1=rs[:], op=ALU.mult),
                  reads=[t_x[c], t_rs], writes=[t_tm])
            if h32 is not None:
                Sc.op("act", lambda e: e.activation(out=h32[:, c, :], in_=tm[:], func=AF.Identity,
                                                    bias=self.M[l][:, (24 if n else 0) + c, b:b + 1],
                                                    scale=self.A[l][:, n, c, b:b + 1]),
                      reads=[t_tm, self.t_mod], writes=[t_h32[c]])
                Sc.op("pool", lambda e: e.tensor_copy(out=hT[:, c, :], in_=h32[:, c, :]), reads=[t_h32[c]], writes=[t_h[c]])
            else:
                Sc.op("act", lambda e: e.activation(out=hT[:, c, :], in_=tm[:], func=AF.Identity,
                                                    bias=self.M[l][:, (24 if n else 0) + c, b:b + 1],
                                                    scale=self.A[l][:, n, c, b:b + 1]),
                      reads=[t_tm, self.t_mod], writes=[t_h[c]])

    def headnorm_rope(self, R, ps, t_ps, psp, t_psp, np_, dim, ones_l, gname, ctab, stab, t_tab, out_ap, t_out):
        Sc = self.S
        SUB = int(os.environ.get("K_SUB", 99))
        if SUB <= 1:
            return
        sq, t_sq = R["sq"].next()
        Sc.op("act", lambda e: e.activation(out=sq[0:np_, :], in_=ps[0:np_, :], func=AF.Square), reads=[t_ps], writes=[t_sq])
        ss, t_ss = R["ss"].next()
        Sc.op("pe", lambda e: e.matmul(ss[0:np_, :], lhsT=ones_l, rhs=sq[0:np_, :], start=True, stop=True),
              reads=[t_sq, self.t_const], writes=[t_ss])
        tmp, t_tmp = R["f32"].next()
        rs, t_rs = R["rstd"].next()
        self.rstd_from_ss(ss, t_ss, np_, float(dim), tmp, t_tmp, rs, t_rs)
        if SUB <= 2:
            return
        t1, t_t1 = R["f32"].next()
        t2, t_t2 = R["f32"].next()
        Sc.op("dve", lambda e: e.scalar_tensor_tensor(out=t1[0:np_, :], in0=ps[0:np_, :], scalar=self.vcol(gname)[0:np_, :],
                                                      in1=ctab[0:np_, :], op0=ALU.mult, op1=ALU.mult),
              reads=[t_ps, t_tab, self.t_vecs], writes=[t_t1])
        Sc.op("dve", lambda e: e.scalar_tensor_tensor(out=t2[0:np_, :], in0=psp[0:np_, :], scalar=self.vcol(gname + "p")[0:np_, :],
                                                      in1=stab[0:np_, :], op0=ALU.mult, op1=ALU.mult),
              reads=[t_psp, t_tab, self.t_vecs], writes=[t_t2])
        if SUB <= 3:
            return
        Sc.op("pool", lambda e: e.tensor_tensor(out=t1[0:np_, :], in0=t1[0:np_, :], in1=t2[0:np_, :], op=ALU.add),
              reads=[t_t2], writes=[t_t1])
        Sc.op(os.environ.get("K_FIN", "pool"), lambda e: e.tensor_tensor(out=out_ap, in0=t1[0:np_, :], in1=rs[0:np_, :], op=ALU.mult),
              reads=[t_t1, t_rs], writes=[t_out])

    def mk_rings(self, es, nsq=4, nf32=6, nrstd=3, nss=2, nmm=int(os.environ.get("K_NMM", 6))):
        nc = self.nc
        R = {}
        R["sq"] = Ring([(es.enter_context(nc.sbuf_tensor(_un(f"sq{i}"), [128, TT], BF16)), Tok()) for i in range(nsq)])
        R["f32"] = Ring([(es.enter_context(nc.sbuf_tensor(_un(f"f32_{i}"), [128, TT], F32)), Tok()) for i in range(nf32)])
        R["rstd"] = Ring([(es.enter_context(nc.sbuf_tensor(_un(f"rstd{i}"), [128, TT], F32)), Tok()) for i in range(nrstd)])
        R["ss"] = Ring([(es.enter_context(nc.psum_tensor(_un(f"ssps{i}"), [128, TT], F32)), Tok(x=True)) for i in range(nss)])
        R["mm"] = Ring([(es.enter_context(nc.psum_tensor(_un(f"mmps{i}"), [128, TT], F32)), Tok(x=True)) for i in range(nmm)])
        return R

    def phase_p0(self):
        nc, Sc, I = self.nc, self.S, self.I
        with ExitStack() as es:
            sb = lambda n, s, d: es.enter_context(nc.sbuf_tensor(_un(n), s, d))
            cT_t = sb("cT_t", [128, 8, NB], F32)
            sc_t = sb("sc_t", [128, 8, NB], F32)
            t_c, t_sc = Tok(), Tok()
            Sc.dma("sp", [(cT_t[:], I["cT"][:, :, :])], writes=[t_c])
            Sc.op("act", lambda e: e.activation(out=sc_t[:], in_=cT_t[:], func=AF.Silu), reads=[t_c], writes=[t_sc])
            wr = Ring([(sb(f"adaw{i}", [128, 8, 768], F32), Tok()) for i in range(2)])
            mps = es.enter_context(nc.psum_tensor(_un("mps"), [128, 48, NB], F32))
            t_mps = Tok()
            for l in range(2):
                wsrc = I["ada_w%d" % l].rearrange("(kc p) n -> p kc n", p=128)
                for g in range(8):
                    wt, t_w = wr.next()
                    Sc.dma("sp", [(wt[:], wsrc[:, :, g * 768:(g + 1) * 768])], writes=[t_w])
                    for j in range(6):
                        ch = g * 6 + j

                        def f(e):
                            for kc in range(8):
                                last = e.matmul(mps[:, ch, :], lhsT=wt[:, kc, j * 128:(j + 1) * 128], rhs=sc_t[:, kc, :],
                                                start=(kc == 0), stop=(kc == 7))
                            return last
                        Sc.op("pe", f, reads=[t_w, t_sc], writes=[t_mps])
                bo = VCOLS["b%d" % l][0]
                for b in range(NB):
                    Sc.op("dve", lambda e: e.tensor_tensor(out=self.M[l][:, :, b], in0=mps[:, :, b],
                                                           in1=self.vecs[:, bo:bo + 48], op=ALU.add),
                          reads=[t_mps, self.t_vecs], writes=[self.t_mod])
                for n in range(2):
                    go = VCOLS[("g_mix%d" if n == 0 else "g_ffn%d") % l][0]
                    for b in range(NB):
                        Sc.op("dve", lambda e: e.scalar_tensor_tensor(
                            out=self.A[l][:, n, :, b], in0=self.M[l][:, (8 if n == 0 else 32):(16 if n == 0 else 40), b],
                            scalar=1.0, in1=self.vecs[:, go:go + 8], op0=ALU.add, op1=ALU.mult),
                            reads=[self.t_mod, self.t_vecs], writes=[self.t_mod])

    def phase_p1(self):
        nc, Sc, I = self.nc, self.S, self.I
        with ExitStack() as es:
            sb = lambda n, s, d: es.enter_context(nc.sbuf_tensor(_un(n), s, d))
            R = self.mk_rings(es)
            w_in = sb("w_in", [128, 8, 1184], BF16)
            w_inp = sb("w_inp", [128, 8, 640], BF16)
            w_kpe = sb("w_kpe", [128, 8, 192], BF16)
            w_uq = sb("w_uq", [128, 2, 768], BF16)
            w_uqp = sb("w_uqp", [128, 2, 768], BF16)
            w_ukv = sb("w_ukv", [128, 1024], BF16)
            w_ukvv = sb("w_ukvv", [128, 8, 64], BF16)
            t_w = Tok()
            self.mk_stage(es, n=2, w=1184, nb=0)
            rr = lambda a: a.rearrange("(kc p) n -> p kc n", p=128)
            for kc in range(8):
                self.load_cast(w_in[:, kc, :], rr(I["w_in"])[:, kc, :], t_w)
                self.load_cast(w_inp[:, kc, :], rr(I["w_in_perm"])[:, kc, :], t_w)
                self.load_cast(w_kpe[:, kc, :], rr(I["w_kpe2"])[:, kc, :], t_w)
            for kc in range(2):
                self.load_cast(w_uq[:, kc, :], rr(I["w_uq"])[:, kc, :], t_w)
                self.load_cast(w_uqp[:, kc, :], rr(I["w_uq_perm"])[:, kc, :], t_w)
            self.load_cast(w_ukv[:], I["w_ukv"][:, :], t_w)
            for h in range(8):
                self.load_cast(w_ukvv[:, h, :], I["w_ukv"][:, 128 * h + 64:128 * h + 128], t_w)
            xr = Ring([(sb(f"xt{i}", [128, 8, TT], F32), toks(8)) for i in range(2)])
            hr = Ring([(sb(f"hT{i}", [128, 8, TT], BF16), toks(8)) for i in range(2)])
            tabr = Ring([(sb(f"tab{i}", [128, 4, TT], F32), Tok()) for i in range(2)])
            cqn = sb("cqn", [128, 2, TT], BF16)
            t_cqn = toks(2)
            ckvn = sb("ckvn", [128, TT], BF16)
            t_ckvn = Tok()
            sqpe = sb("sqpe", [128, TT], BF16)
            t_sqpe = Tok()
            Rpe = sb("Rpe", [128, TT], F32)
            t_Rpe = Tok()
            qa_st = Ring([(sb(f"qa_st{i}", [96, 8, TT], BF16), Tok()) for i in range(2)])
            ka_st = Ring([(sb(f"ka_st{i}", [96, 8, TT], BF16), Tok()) for i in range(2)])
            qb_st = Ring([(sb(f"qb_st{i}", [128, 4, TT], BF16), Tok()) for i in range(2)])
            kb_st = Ring([(sb(f"kb_st{i}", [128, TT], BF16), Tok()) for i in range(2)])
            va_st = Ring([(sb(f"va_st{i}", [128, 4, 512], BF16), Tok()) for i in range(2)])
            vb_st = Ring([(sb(f"vb_st{i}", [128, 4, 128], BF16), Tok()) for i in range(2)])
            xsrc = I["xT"].rearrange("(c p) t -> p c t", p=128)
            qa_dst = self.scr["qaT"].rearrange("(h p) t -> p h t", p=96)
            ka_dst = self.scr["kaT"].rearrange("(h p) t -> p h t", p=96)
            qb_dst = self.scr["qbT"].rearrange("(c p) t -> p c t", p=128)
            va_dst = self.scr["vA"].rearrange("(u p) f -> p u f", p=128)
            vb_dst = self.scr["vB"].rearrange("(u p) f -> p u f", p=128)
            t_scr = Tok()

            LIM = int(os.environ.get('K_LIM', 99))
            NT_RUN = int(os.environ.get('K_NT', NT))
            loaded = []

            def p1_loads(j):
                s0_, t0_ = (j % (S // TT)) * TT, j * TT
                xt_, t_x_ = xr.next()
                for c in range(8):
                    Sc.dma("sp", [(xt_[:, c, :], xsrc[:, c, t0_:t0_ + TT])], writes=[t_x_[c]])
                tab_, t_tab_ = tabr.next()
                Sc.dma("sp", [(tab_[0:96, 0, :], I["c_mla"][:, s0_:s0_ + TT]), (tab_[0:96, 1, :], I["s_mla"][:, s0_:s0_ + TT]),
                              (tab_[:, 2, :], I["c_ax"][:, s0_:s0_ + TT]), (tab_[:, 3, :], I["s_ax"][:, s0_:s0_ + TT])],
                       writes=[t_tab_])
                loaded.append((xt_, t_x_, tab_, t_tab_))

            for it in range(NT_RUN):
                b, s0, t0 = it // (S // TT), (it % (S // TT)) * TT, it * TT
                if it == 0:
                    p1_loads(0)
                if it + 1 < NT_RUN:
                    p1_loads(it + 1)
                xt, t_x, tab, t_tab = loaded.pop(0)
                hT, t_h = hr.next()
                self.norm_tile(R, xt, t_x, 0, 0, b, hT, t_h)

                def proj(w, c0, m, po=0):
                    ps, t_ps = R["mm"].next()

                    def f(e):
                        for kc in range(8):
                            last = e.matmul(ps[po:po + m, :], lhsT=w[:, kc, c0:c0 + m], rhs=hT[:, kc, :],
                                            start=(kc == 0), stop=(kc == 7))
                        return last
                    Sc.op("pe", f, reads=t_h + [t_w], writes=[t_ps])
                    return ps, t_ps

                if LIM <= 1:
                    continue
                cq = [proj(w_in, 128 * c, 128) for c in range(2)]
                ss, t_ss = R["ss"].next()
                for c in range(2):
                    sq, t_sq = R["sq"].next()
                    Sc.op("act", lambda e: e.activation(out=sq[:], in_=cq[c][0][:], func=AF.Square), reads=[cq[c][1]], writes=[t_sq])
                    Sc.op("pe", lambda e: e.matmul(ss[:], lhsT=self.ones_bf[:], rhs=sq[:], start=(c == 0), stop=(c == 1)),
                          reads=[t_sq, self.t_const], writes=[t_ss])
                tmp, t_tmp = R["f32"].next()
                rs, t_rs = R["rstd"].next()
                self.rstd_from_ss(ss, t_ss, 128, 256.0, tmp, t_tmp, rs, t_rs)
                for c in range(2):
                    Sc.op("dve", lambda e: e.scalar_tensor_tensor(out=cqn[:, c, :], in0=cq[c][0][:], scalar=self.vcol("qn", c),
                                                                  in1=rs[:], op0=ALU.mult, op1=ALU.mult),
                          reads=[cq[c][1], t_rs, self.t_vecs], writes=[t_cqn[c]])
                ckv, t_ckv = proj(w_in, 256, 128)
                sq, t_sq = R["sq"].next()
                Sc.op("act", lambda e: e.activation(out=sq[:], in_=ckv[:], func=AF.Square), reads=[t_ckv], writes=[t_sq])
                ss, t_ss = R["ss"].next()
                Sc.op("pe", lambda e: e.matmul(ss[:], lhsT=self.ones_bf[:], rhs=sq[:], start=True, stop=True),
                      reads=[t_sq, self.t_const], writes=[t_ss])
                tmp, t_tmp = R["f32"].next()
                rs, t_rs = R["rstd"].next()
                self.rstd_from_ss(ss, t_ss, 128, 128.0, tmp, t_tmp, rs, t_rs)
                Sc.op("dve", lambda e: e.scalar_tensor_tensor(out=ckvn[:], in0=ckv[:], scalar=self.vcol("kvn"),
                                                              in1=rs[:], op0=ALU.mult, op1=ALU.mult),
                      reads=[t_ckv, t_rs, self.t_vecs], writes=[t_ckvn])
                if LIM <= 2:
                    continue
                kpe, t_kpe = proj(w_kpe, 0, 96)
                kpp, t_kpp = proj(w_kpe, 96, 96)
                Sc.op("act", lambda e: e.activation(out=sqpe[64:96, :], in_=kpe[64:96, :], func=AF.Square), reads=[t_kpe], writes=[t_sqpe])
                t1, t_t1 = R["f32"].next()
                Sc.op("dve", lambda e: e.scalar_tensor_tensor(out=Rpe[64:96, :], in0=kpe[64:96, :], scalar=self.vcol("gka96")[64:96, :],
                                                              in1=tab[64:96, 0, :], op0=ALU.mult, op1=ALU.mult),
                      reads=[t_kpe, t_tab, self.t_vecs], writes=[t_Rpe])
                Sc.op("dve", lambda e: e.scalar_tensor_tensor(out=t1[64:96, :], in0=kpp[64:96, :], scalar=self.vcol("gka96p")[64:96, :],
                                                              in1=tab[64:96, 1, :], op0=ALU.mult, op1=ALU.mult),
                      reads=[t_kpp, t_tab, self.t_vecs], writes=[t_t1])
                Sc.op("pool", lambda e: e.tensor_tensor(out=Rpe[64:96, :], in0=Rpe[64:96, :], in1=t1[64:96, :], op=ALU.add),
                      reads=[t_t1], writes=[t_Rpe])
                if LIM <= 3:
                    continue
                qbs, t_qbs = qb_st.next()
                for c in range(4):
                    ps, t_ps = proj(w_in, 416 + 128 * c, 128)
                    pp, t_pp = proj(w_inp, 128 * c, 128)
                    self.headnorm_rope(R, ps, t_ps, pp, t_pp, 128, 64, self.bd_bf[:], "gqb", tab[:, 2, :], tab[:, 3, :], t_tab,
                                       qbs[:, c, :], t_qbs)
                Sc.dma("sp", [(qb_dst[:, :, t0:t0 + TT], qbs[:])], reads=[t_qbs], writes=[Tok()])
                kbs, t_kbs = kb_st.next()
                ps, t_ps = proj(w_in, 928, 128)
                pp, t_pp = proj(w_inp, 512, 128)
                self.headnorm_rope(R, ps, t_ps, pp, t_pp, 128, 64, self.bd_bf[:], "gkb", tab[:, 2, :], tab[:, 3, :], t_tab,
                                   kbs[:], t_kbs)
                Sc.dma("sp", [(self.scr["kbT"][:, t0:t0 + TT], kbs[:])], reads=[t_kbs], writes=[Tok()])
                if LIM <= 4:
                    continue
                vbs, t_vbs = vb_st.next()
                ps, t_ps = R["mm"].next()
                for u in range(4):
                    def f(e):
                        for kc in range(8):
                            last = e.matmul(ps[:, u * 128:(u + 1) * 128], lhsT=hT[:, kc, u * 128:(u + 1) * 128],
                                            rhs=w_in[:, kc, 1056:1184], start=(kc == 0), stop=(kc == 7))
                        return last
                    Sc.op("pe", f, reads=t_h + [t_w], writes=[t_ps])
                Sc.op("dve", lambda e: e.tensor_copy(out=vbs[:].rearrange("p u f -> p (u f)"), in_=ps[:]), reads=[t_ps], writes=[t_vbs])
                Sc.dma("sp", [(vb_dst[:, 4 * it:4 * it + 4, :], vbs[:])], reads=[t_vbs], writes=[Tok()])
                if LIM <= 5:
                    continue
                qas, t_qas = qa_st.next()
                for h in range(8):
                    ps, t_ps = R["mm"].next()
                    pp, t_pp = R["mm"].next()
                    for (pt, tp, w) in ((ps, t_ps, w_uq), (pp, t_pp, w_uqp)):
                        def f(e):
                            for kc in range(2):
                                last = e.matmul(pt[0:96, :], lhsT=w[:, kc, 96 * h:96 * h + 96], rhs=cqn[:, kc, :],
                                                start=(kc == 0), stop=(kc == 1))
                            return last
                        Sc.op("pe", f, reads=t_cqn + [t_w], writes=[tp])
                    self.headnorm_rope(R, ps, t_ps, pp, t_pp, 96, 96, self.ones_bf[0:96, 0:96], "gqa96",
                                       tab[:, 0, :], tab[:, 1, :], t_tab, qas[0:96, h, :], t_qas)
                Sc.dma("sp", [(qa_dst[:, :, t0:t0 + TT], qas[:])], reads=[t_qas], writes=[Tok()])
                if LIM <= 6:
                    continue
                kas, t_kas = ka_st.next()
                for h in range(8):
                    ps, t_ps = R["mm"].next()
                    Sc.op("pe", lambda e: e.matmul(ps[0:64, :], lhsT=w_ukv[:, 128 * h:128 * h + 64], rhs=ckvn[:], start=True, stop=True),
                          reads=[t_ckvn, t_w], writes=[t_ps])
                    sq, t_sq = R["sq"].next()
                    Sc.op("act", lambda e: e.activation(out=sq[0:64, :], in_=ps[0:64, :], func=AF.Square), reads=[t_ps], writes=[t_sq])
                    ss, t_ss = R["ss"].next()

                    def f(e):
                        e.matmul(ss[0:96, :], lhsT=self.ones_bf[0:64, 0:96], rhs=sq[0:64, :], start=True, stop=False)
                        return e.matmul(ss[0:96, :], lhsT=self.ones_bf[64:96, 0:96], rhs=sqpe[64:96, :], start=False, stop=True)
                    Sc.op("pe", f, reads=[t_sq, t_sqpe, self.t_const], writes=[t_ss])
                    tmp, t_tmp = R["f32"].next()
                    rs, t_rs = R["rstd"].next()
                    self.rstd_from_ss(ss, t_ss, 96, 96.0, tmp, t_tmp, rs, t_rs)
                    Sc.op("dve", lambda e: e.scalar_tensor_tensor(out=kas[0:64, h, :], in0=ps[0:64, :], scalar=self.vcol("gka96")[0:64, :],
                                                                  in1=rs[0:64, :], op0=ALU.mult, op1=ALU.mult),
                          reads=[t_ps, t_rs, self.t_vecs], writes=[t_kas])
                    Sc.op("pool", lambda e: e.tensor_tensor(out=kas[64:96, h, :], in0=Rpe[64:96, :], in1=rs[64:96, :], op=ALU.mult),
                          reads=[t_Rpe, t_rs], writes=[t_kas])
                Sc.dma("sp", [(ka_dst[:, :, t0:t0 + TT], kas[:])], reads=[t_kas], writes=[Tok()])
                if LIM <= 7:
                    continue
                vas, t_vas = va_st.next()
                for u in range(4):
                    ps, t_ps = R["mm"].next()
                    Sc.op("pe", lambda e: e.matmul(ps[:], lhsT=ckvn[:, u * 128:(u + 1) * 128], rhs=w_ukvv[:].rearrange("k h x -> k (h x)"),
                                                   start=True, stop=True), reads=[t_ckvn, t_w], writes=[t_ps])
                    Sc.op("dve", lambda e: e.tensor_copy(out=vas[:, u, :], in_=ps[:]), reads=[t_ps], writes=[t_vas])
                Sc.dma("sp", [(va_dst[:, 4 * it:4 * it + 4, :], vas[:])], reads=[t_vas], writes=[Tok()])


    def attn_epilogue(self, o, t_o, RR, dst_ap):
        Sc = self.S
        rc, t_rc = RR["rec"].next()
        Sc.op("dve", lambda e: e.reciprocal(out=rc[64:65, :], in_=o[64:65, :]), reads=[t_o], writes=[t_rc])
        bc, t_bc = RR["bc"].next()
        Sc.op("pe", lambda e: e.matmul(bc[0:64, :], lhsT=self.ones_f[64:65, 0:64], rhs=rc[64:65, :], start=True, stop=True),
              reads=[t_rc, self.t_const], writes=[t_bc])
        ob, t_ob = RR["osb"].next()
        Sc.op("dve", lambda e: e.tensor_copy(out=ob[0:64, :], in_=o[0:64, :]), reads=[t_o], writes=[t_ob])
        ot, t_ot = RR["outb"].next()
        Sc.op("dve", lambda e: e.tensor_tensor(out=ot[0:64, :], in0=ob[0:64, :], in1=bc[0:64, :], op=ALU.mult),
              reads=[t_ob, t_bc], writes=[t_ot])
        Sc.dma("sp", [(dst_ap, ot[0:64, :])], reads=[t_ot], writes=[Tok()])

    def mk_attn_rings(self, es, nS=4, nO=2):
        nc = self.nc
        sb = lambda n, s, d: es.enter_context(nc.sbuf_tensor(_un(n), s, d))
        ps = lambda n: es.enter_context(nc.psum_tensor(_un(n), [128, TT], F32))
        RR = {}
        RR["s"] = Ring([(ps(f"sps{i}"), Tok(x=True)) for i in range(nS)])
        RR["o"] = Ring([(ps(f"ops{i}"), Tok(x=True)) for i in range(nO)])
        RR["bc"] = Ring([(ps("bcps"), Tok(x=True))])
        RR["pT"] = Ring([(sb(f"pT{i}", [128, TT], BF16), Tok()) for i in range(nS + 1)])
        RR["rec"] = Ring([(sb(f"rec{i}", [128, TT], F32), Tok()) for i in range(2)])
        RR["osb"] = Ring([(sb(f"osb{i}", [128, TT], F32), Tok()) for i in range(2)])
        RR["outb"] = Ring([(sb(f"outb{i}", [128, TT], BF16), Tok()) for i in range(2)])
        return RR

    def phase_p2(self):
        nc, Sc, I = self.nc, self.S, self.I
        with ExitStack() as es:
            sb = lambda n, s, d: es.enter_context(nc.sbuf_tensor(_un(n), s, d))
            self.mk_stage(es, n=2, w=3584)
            RR = self.mk_attn_rings(es, nS=5, nO=2)
            qr = Ring([(sb(f"qT{i}", [96, S], BF16), Tok()) for i in range(2)])
            kr = Ring([(sb(f"kT{i}", [96, S], BF16), Tok()) for i in range(2)])
            qrB = Ring([(sb(f"qTb{i}", [128, S], BF16), Tok()) for i in range(2)])
            krB = Ring([(sb(f"kTb{i}", [128, S], BF16), Tok()) for i in range(2)])
            for (bf_, t_bf) in qrB.items + krB.items:
                Sc.op("pool", lambda e: e.memset(bf_[:], 0.0), writes=[t_bf])
            vr = Ring([(sb(f"vt{i}", [128, 32, 65], BF16), Tok()) for i in range(2)])
            for vt, t_v in vr.items:
                Sc.op("dve", lambda e: e.memset(vt[:, :, 64:65], 1.0), writes=[t_v])
            vA = self.scr["vA"].rearrange("(n p) f -> p n f", p=128)
            vB = self.scr["vB"].rearrange("(n p) f -> p n f", p=128)
            items = []
            for b in range(NB):
                tb = b * S
                for h in range(8):
                    items.append(dict(b=b, kv=("a", b, h), dq=96, dk=96, sc=96.0 ** -0.5, orow=64 * h,
                                      k=self.scr["kaT"][96 * h:96 * h + 96, tb:tb + S],
                                      q=self.scr["qaT"][96 * h:96 * h + 96, tb:tb + S],
                                      v=vA[:, 32 * b:32 * b + 32, 64 * h:64 * h + 64]))
                for h in range(8):
                    g = h // 4
                    items.append(dict(b=b, kv=("b", b, g), dq=64, dk=128, sc=64.0 ** -0.5, orow=512 + 64 * h,
                                      k=self.scr["kbT"][64 * g:64 * g + 64, tb:tb + S],
                                      q=self.scr["qbT"][64 * h:64 * h + 64, tb:tb + S],
                                      v=vB[:, 32 * b:32 * b + 32, 64 * g:64 * g + 64]))
            NIT = int(os.environ.get("K_NI", len(items)))
            items = items[:NIT]
            cur = {"kv": None}

            def loads(i):
                it = items[i]
                dq = it["dq"]
                if it["kv"] != cur["kv"]:
                    cur["kv"] = it["kv"]
                    kT, t_k = (krB if it["dk"] == 128 else kr).next()
                    vt, t_v = vr.next()
                    Sc.dma("sp", [(kT[0:dq, :], it["k"])], writes=[t_k])
                    Sc.dma("sp", [(vt[:, :, 0:64], it["v"])], writes=[t_v])
                    cur["k"] = (kT, t_k, vt, t_v)
                qT, t_q = (qrB if it["dk"] == 128 else qr).next()
                Sc.dma("sp", [(qT[0:dq, :], it["q"])], writes=[t_q])
                it["bufs"] = cur["k"] + (qT, t_q)

            loads(0)
            deferred = []
            for i, it in enumerate(items):
                if i + 1 < len(items):
                    loads(i + 1)
                kT, t_k, vt, t_v, qT, t_q = it["bufs"]
                dq, scl, tb = it["dk"], it["sc"], it["b"] * S
                for qt in range(S // TT):
                    self.pump_casts(2)
                    o, t_o = RR["o"].next()
                    pend = []

                    def pv(kt, p, t_p):
                        Sc.op("pe", lambda e: e.matmul(o[0:65, :], lhsT=vt[:, kt, :], rhs=p[:], start=(kt == 0), stop=(kt == 31)),
                              reads=[t_p, t_v], writes=[t_o])
                    for kt in range(32):
                        if kt == 14 and deferred:
                            deferred.pop(0)()
                        s_, t_s = RR["s"].next()
                        Sc.op("pe", lambda e: e.matmul(s_[:], lhsT=kT[0:dq, kt * 128:(kt + 1) * 128], rhs=qT[0:dq, qt * TT:(qt + 1) * TT],
                                                       start=True, stop=True), reads=[t_k, t_q], writes=[t_s])
                        p, t_p = RR["pT"].next()
                        Sc.op("act", lambda e: e.activation(out=p[:], in_=s_[:], func=AF.Exp, scale=scl), reads=[t_s], writes=[t_p])
                        pend.append((kt, p, t_p))
                        if len(pend) > 3:
                            pv(*pend.pop(0))
                    while pend:
                        pv(*pend.pop(0))
                    deferred.append(lambda o=o, t_o=t_o, dst=self.scr["at0"][it["orow"]:it["orow"] + 64, tb + qt * TT:tb + (qt + 1) * TT]:
                                    self.attn_epilogue(o, t_o, RR, dst))
            while deferred:
                deferred.pop(0)()
            while self.cast_jobs or getattr(self, "pump_inflight", None):
                self.pump_casts(1)

    def outproj_residual(self, R, w_out, t_w, at, t_at, xt, t_x, l, b):
        Sc = self.S
        for dc in range(8):
            ps, t_ps = R["mm"].next()

            def f(e):
                for kc in range(8):
                    last = e.matmul(ps[:], lhsT=w_out[:, kc, dc * 128:(dc + 1) * 128], rhs=at[:, kc, :], start=(kc == 0), stop=(kc == 7))
                return last
            Sc.op("pe", f, reads=t_at + [t_w], writes=[t_ps])
            Sc.op("dve", lambda e: e.scalar_tensor_tensor(out=xt[:, dc, :], in0=ps[:], scalar=self.M[l][:, 16 + dc, b:b + 1],
                                                          in1=xt[:, dc, :], op0=ALU.mult, op1=ALU.add),
                  reads=[t_ps, self.t_mod], writes=[t_x[dc]])

    def phase_p3(self):
        nc, Sc, I = self.nc, self.S, self.I
        NFC = DFF // 128
        with ExitStack() as es:
            sb = lambda n, s, d: es.enter_context(nc.sbuf_tensor(_un(n), s, d))
            R = self.mk_rings(es, nss=1, nmm=3)
            R["y"] = Ring([(es.enter_context(nc.psum_tensor(_un(f"yps{i}"), [128, TT], F32)), Tok(x=True)) for i in range(4)])
            self.mk_stage(es, n=2, w=1024, nb=0)
            w_out = sb("w_out", [128, 8, D], BF16)
            t_w = Tok()
            rr = lambda a: a.rearrange("(kc p) n -> p kc n", p=128)
            for kc in range(8):
                self.load_cast(w_out[:, kc, :], rr(I["w_out0"])[:, kc, :], t_w)
            xr = Ring([(sb(f"xt{i}", [128, 8, TT], F32), toks(8)) for i in range(2)])
            ar = Ring([(sb(f"at{i}", [128, 8, TT], BF16), toks(8)) for i in range(2)])
            hT = sb("hT", [128, 8, TT], BF16)
            t_h = toks(8)
            H = sb("H", [128, NFC, TT], BF16)
            t_H = toks(NFC)
            wgr = Ring([(sb(f"wg{i}", [128, 8, 256], BF16), Tok()) for i in range(3)])
            wur = Ring([(sb(f"wu{i}", [128, 8, 256], BF16), Tok()) for i in range(3)])
            wdr = Ring([(sb(f"wd{i}", [128, 2, 512], BF16), Tok()) for i in range(3)])
            xsrc = I["xT"].rearrange("(c p) t -> p c t", p=128)
            asrc = self.scr["at0"].rearrange("(c p) t -> p c t", p=128)
            xdst = self.scr["x2T"].rearrange("(c p) t -> p c t", p=128)
            wg_src = rr(self.scr["wg0b"])
            wu_src = rr(self.scr["wu0b"])
            wd_src = self.scr["wd0b"].rearrange("(fc p) d -> p fc d", p=128)
            NT_RUN = int(os.environ.get('K_NT', NT))
            loaded = []

            def loads(j):
                t0_ = j * TT
                xt_, t_x_ = xr.next()
                at_, t_at_ = ar.next()
                for c in range(8):
                    Sc.dma("sp", [(xt_[:, c, :], xsrc[:, c, t0_:t0_ + TT])], writes=[t_x_[c]])
                    Sc.dma("sp", [(at_[:, c, :], asrc[:, c, t0_:t0_ + TT])], writes=[t_at_[c]])
                loaded.append((xt_, t_x_, at_, t_at_))

            loads(0)
            for it in range(NT_RUN):
                b, t0 = it // (S // TT), it * TT
                if it + 1 < NT_RUN:
                    loads(it + 1)
                xt, t_x, at, t_at = loaded.pop(0)
                self.outproj_residual(R, w_out, t_w, at, t_at, xt, t_x, 0, b)
                self.norm_tile(R, xt, t_x, 0, 1, b, hT, t_h)
                for fg in range(NFC // 2):
                    wg, t_wg = wgr.next()
                    wu, t_wu = wur.next()
                    Sc.dma("sp", [(wg[:], wg_src[:, :, fg * 256:(fg + 1) * 256])], writes=[t_wg])
                    Sc.dma("sp", [(wu[:], wu_src[:, :, fg * 256:(fg + 1) * 256])], writes=[t_wu])
                    for fl in range(2):
                        f_ = fg * 2 + fl
                        gps, t_g = R["mm"].next()
                        ups, t_u = R["mm"].next()
                        for (pt, tp, w, tw) in ((gps, t_g, wg, t_wg), (ups, t_u, wu, t_wu)):
                            def f(e):
                                for kc in range(8):
                                    last = e.matmul(pt[:], lhsT=w[:, kc, fl * 128:(fl + 1) * 128], rhs=hT[:, kc, :],
                                                    start=(kc == 0), stop=(kc == 7))
                                return last
                            Sc.op("pe", f, reads=t_h + [tw], writes=[tp])
                        sg, t_sg = R["f32"].next()
                        Sc.op("act", lambda e: e.activation(out=sg[:], in_=gps[:], func=AF.Silu), reads=[t_g], writes=[t_sg])
                        Sc.op("dve", lambda e: e.tensor_tensor(out=H[:, f_, :], in0=sg[:], in1=ups[:], op=ALU.mult),
                              reads=[t_sg, t_u], writes=[t_H[f_]])
                for half in range(2):
                    ys = [R["y"].next() for _ in range(4)]
                    for fg in range(NFC // 2):
                        wd, t_wd = wdr.next()
                        Sc.dma("sp", [(wd[:], wd_src[:, 2 * fg:2 * fg + 2, half * 512:(half + 1) * 512])], writes=[t_wd])
                        for fl in range(2):
                            f_ = fg * 2 + fl

                            def f(e):
                                for j in range(4):
                                    last = e.matmul(ys[j][0][:], lhsT=wd[:, fl, j * 128:(j + 1) * 128], rhs=H[:, f_, :],
                                                    start=(f_ == 0), stop=(f_ == NFC - 1))
                                return last
                            Sc.op("pe", f, reads=[t_H[f_], t_wd], writes=[y[1] for y in ys])
                    for j in range(4):
                        dc = half * 4 + j
                        Sc.op("dve", lambda e: e.scalar_tensor_tensor(out=xt[:, dc, :], in0=ys[j][0][:], scalar=self.M[0][:, 40 + dc, b:b + 1],
                                                                      in1=xt[:, dc, :], op0=ALU.mult, op1=ALU.add),
                              reads=[ys[j][1], self.t_mod], writes=[t_x[dc]])
                for c in range(8):
                    Sc.dma("sp", [(xdst[:, c, t0:t0 + TT], xt[:, c, :])], reads=[t_x[c]], writes=[Tok()])

    def phase_p4(self):
        nc, Sc, I = self.nc, self.S, self.I
        with ExitStack() as es:
            sb = lambda n, s, d: es.enter_context(nc.sbuf_tensor(_un(n), s, d))
            R = self.mk_rings(es)
            self.mk_stage(es, n=1, w=3072, nb=0)
            w_qkv = sb("w_qkv", [128, 8, 3 * D], BF16)
            w_qkp = sb("w_qkp", [128, 8, 2 * D], BF16)
            t_w = Tok()
            rr = lambda a: a.rearrange("(kc p) n -> p kc n", p=128)
            for kc in range(8):
                self.load_cast(w_qkv[:, kc, :], rr(I["w_qkv"])[:, kc, :], t_w)
                self.load_cast(w_qkp[:, kc, :], rr(I["w_qk_perm"])[:, kc, :], t_w)
            xt = sb("xt", [128, 8, TT], F32)
            t_x = toks(8)
            hT = sb("hT", [128, 8, TT], BF16)
            t_h = toks(8)
            tabr = Ring([(sb(f"tab{i}", [128, 2, TT], F32), Tok()) for i in range(2)])
            q_st, t_qst = sb("q_st", [128, 8, TT], BF16), Tok()
            k_st, t_kst = sb("k_st", [128, 8, TT], BF16), Tok()
            v_st, t_vst = sb("v_st", [128, 4, D], BF16), Tok()
            xsrc = self.scr["x2T"].rearrange("(c p) t -> p c t", p=128)
            q_dst = self.scr["q1T"].rearrange("(c p) t -> p c t", p=128)
            k_dst = self.scr["k1T"].rearrange("(c p) t -> p c t", p=128)
            v_dst = self.scr["v1"].rearrange("(u p) f -> p u f", p=128)
            NT_RUN = int(os.environ.get('K_NT', NT))
            for it in range(NT_RUN):
                b, s0, t0 = it // (S // TT), (it % (S // TT)) * TT, it * TT
                for c in range(8):
                    Sc.dma("sp", [(xt[:, c, :], xsrc[:, c, t0:t0 + TT])], writes=[t_x[c]])
                tab, t_tab = tabr.next()
                Sc.dma("sp", [(tab[:, 0, :], I["c_l1"][:, s0:s0 + TT]), (tab[:, 1, :], I["s_l1"][:, s0:s0 + TT])], writes=[t_tab])
                self.norm_tile(R, xt, t_x, 1, 0, b, hT, t_h)

                def proj(w, c0):
                    ps, t_ps = R["mm"].next()

                    def f(e):
                        for kc in range(8):
                            last = e.matmul(ps[:], lhsT=w[:, kc, c0:c0 + 128], rhs=hT[:, kc, :], start=(kc == 0), stop=(kc == 7))
                        return last
                    Sc.op("pe", f, reads=t_h + [t_w], writes=[t_ps])
                    return ps, t_ps
                for (st, t_st, off, gname) in ((q_st, t_qst, 0, "gq1"), (k_st, t_kst, D, "gk1")):
                    for c in range(8):
                        ps, t_ps = proj(w_qkv, off + 128 * c)
                        pp, t_pp = proj(w_qkp, off + 128 * c)
                        self.headnorm_rope(R, ps, t_ps, pp, t_pp, 128, 64, self.bd_bf[:], gname, tab[:, 0, :], tab[:, 1, :], t_tab,
                                           st[:, c, :], t_st)
                Sc.dma("sp", [(q_dst[:, :, t0:t0 + TT], q_st[:])], reads=[t_qst], writes=[Tok()])
                Sc.dma("sp", [(k_dst[:, :, t0:t0 + TT], k_st[:])], reads=[t_kst], writes=[Tok()])
                for u in range(4):
                    for half in range(2):
                        ps, t_ps = R["mm"].next()

                        def f(e):
                            for kc in range(8):
                                last = e.matmul(ps[:], lhsT=hT[:, kc, u * 128:(u + 1) * 128],
                                                rhs=w_qkv[:, kc, 2 * D + half * 512:2 * D + (half + 1) * 512], start=(kc == 0), stop=(kc == 7))
                            return last
                        Sc.op("pe", f, reads=t_h + [t_w], writes=[t_ps])
                        Sc.op("dve", lambda e: e.tensor_copy(out=v_st[:, u, half * 512:(half + 1) * 512], in_=ps[:]), reads=[t_ps], writes=[t_vst])
                Sc.dma("sp", [(v_dst[:, 4 * it:4 * it + 4, :], v_st[:])], reads=[t_vst], writes=[Tok()])

    def phase_p5(self):
        nc, Sc, I = self.nc, self.S, self.I
        with ExitStack() as es:
            sb = lambda n, s, d: es.enter_context(nc.sbuf_tensor(_un(n), s, d))
            RR = self.mk_attn_rings(es, nS=4, nO=3)
            self.mk_stage(es, n=1, w=2048, nb=0)
            masks = sb("masks", [128, 8, TT], BF16)
            t_mask = Tok()
            for i in range(2):
                self.load_cast(masks[:, 4 * i:4 * i + 4, :].rearrange("p a b -> p (a b)"),
                               I["masks"][:, 4 * i:4 * i + 4, :].rearrange("p a b -> p (a b)"), t_mask)
            er = Ring([(sb(f"e{i}", [128, TT], BF16), Tok()) for i in range(4)])
            q1r = Ring([(sb(f"q1_{i}", [128, S], BF16), Tok()) for i in range(2)])
            k1r = Ring([(sb(f"k1_{i}", [128, S + 128], BF16), Tok()) for i in range(2)])
            q4, t_q4 = sb("q4", [128, 4, 1024], BF16), Tok()
            k4, t_k4 = sb("k4", [128, 4, 1152], BF16), Tok()
            q16, t_q16 = sb("q16", [128, 16, 256], BF16), Tok()
            k16, t_k16 = sb("k16", [128, 16, 384], BF16), Tok()
            zf, t_zf = sb("zf", [128, TT], F32), Tok()
            vr = Ring([((sb(f"v1t{i}", [128, 33, 65], BF16), sb(f"v4t{i}", [128, 4, 9, 65], BF16), sb(f"v16t{i}", [128, 16, 3, 65], BF16)), Tok())
                       for i in range(2)])
            Ur = Ring([(sb(f"U{i}", [128, S], F32), Tok()) for i in range(2)])
            for (bf_, t_bf) in q1r.items + k1r.items + [(q4, t_q4), (k4, t_k4), (q16, t_q16), (k16, t_k16)]:
                fl_ = bf_[:] if len(bf_.shape) == 2 else bf_[:].rearrange("p a b -> p (a b)")
                Sc.op("pool", lambda e: e.memset(fl_, 0.0), writes=[t_bf])
            Sc.op("dve", lambda e: e.memset(zf[:], 0.0), writes=[t_zf])
            for ((a_, b_, c_), t_v) in vr.items:
                for tl in (a_, b_, c_):
                    fl = tl[:].rearrange("p a b -> p (a b)") if len(tl.shape) == 3 else tl[:].rearrange("p a b c -> p (a b c)")
                    Sc.op("dve", lambda e: e.memset(fl, 0.0), writes=[t_v])
                Sc.op("dve", lambda e: e.memset(a_[:, :, 64:65], 1.0), writes=[t_v])
                Sc.op("dve", lambda e: e.memset(b_[:, :, :, 64:65], 1.0), writes=[t_v])
                Sc.op("dve", lambda e: e.memset(c_[:, :, :, 64:65], 1.0), writes=[t_v])
            SCL = 0.125
            NPAIR = int(os.environ.get("K_NP", NB * 8))
            pairs = [(b, hp) for b in range(NB) for hp in range(8)][:NPAIR]
            heads = [(b, hp, hh) for (b, hp) in pairs for hh in range(2)]
            pl, hl = [], []

            def load_pair(i):
                b, hp, hh = heads[i]
                h = 2 * hp + hh
                q1, t_q1 = q1r.next()
                k1, t_k1 = k1r.next()
                Sc.dma("sp", [(q1[0:64, :], self.scr["q1T"][64 * h:64 * h + 64, b * S:(b + 1) * S])], writes=[t_q1])
                Sc.dma("sp", [(k1[0:64, 64:64 + S], self.scr["k1T"][64 * h:64 * h + 64, b * S:(b + 1) * S])], writes=[t_k1])
                pl.append((q1, t_q1, k1, t_k1))

            def load_v(i):
                b, hp, hh = heads[i]
                h = 2 * hp + hh
                (v1t, v4t, v16t), t_v = vr.next()
                vs = self.scr["v1"][b * S:(b + 1) * S, 64 * h:64 * h + 64]
                prs = [(v1t[:, 1:32, 0:64], vs[64:64 + 31 * 128, :].rearrange("(j p) x -> p j x", p=128)),
                       (v1t[64:128, 0, 0:64], vs[0:64, :]), (v1t[0:64, 32, 0:64], vs[S - 64:S, :])]
                v4s = vs.rearrange("(l r) x -> r l x", r=4)
                for r in range(4):
                    prs.append((v4t[:, r, 1:8, 0:64], v4s[r, 64:64 + 7 * 128, :].rearrange("(j p) x -> p j x", p=128)))
                    prs.append((v4t[64:128, r, 0, 0:64], v4s[r, 0:64, :]))
                    prs.append((v4t[0:64, r, 8, 0:64], v4s[r, 960:1024, :]))
                v16s = vs.rearrange("(l r) x -> l r x", r=16)
                prs.append((v16t[64:128, :, 0, 0:64], v16s[0:64, :, :]))
                prs.append((v16t[:, :, 1, 0:64], v16s[64:192, :, :]))
                prs.append((v16t[0:64, :, 2, 0:64], v16s[192:256, :, :]))
                Sc.dma("sp", prs, writes=[t_v])
                hl.append((v1t, v4t, v16t, t_v))

            mk_eng = [0]

            gp = []

            def pv_emit():
                (jr, n, p, t_p, q0, w, v_ap, t_v, o, t_o, fin) = gp.pop(0)
                Sc.op("pe", lambda e: e.matmul(o[0:65, q0:q0 + w], lhsT=v_ap(jr), rhs=p[:, 0:w], start=False, stop=True, skip_group_check=True),
                      reads=[t_p, t_v], writes=[t_o])
                if jr == n - 1:
                    fin()

            def band(hs, k_ap, t_k, q_fn, t_q, nq, v_ap, t_v, first, last, o, t_o, fin):
                n = nq // 128 + 1
                Sc.op("act", lambda e: e.activation(out=o[0:65, 0:nq], in_=zf[0:65, 0:nq], func=AF.Copy), reads=[t_zf], writes=[t_o])
                for jr in range(n):
                    q0 = max(0, 128 * (jr - 1))
                    q1 = min(nq, 128 * (jr + 1))
                    w = q1 - q0
                    m0 = 128 if jr == 0 else 0
                    pat = 1 if (first and jr == 0) else 2 if (last and jr == n - 1) else 0
                    s_, t_s = RR["s"].next()
                    Sc.op("pe", lambda e: e.matmul(s_[:, 0:w], lhsT=k_ap(jr), rhs=q_fn(q0, q1), start=True, stop=True), reads=[t_k, t_q], writes=[t_s])
                    ee, t_e = er.next()
                    Sc.op("act", lambda e: e.activation(out=ee[:, 0:w], in_=s_[:, 0:w], func=AF.Exp, scale=SCL), reads=[t_s], writes=[t_e])
                    p, t_p = RR["pT"].next()
                    mk_eng[0] += 1
                    Sc.op("pool" if mk_eng[0] % 2 else "dve", lambda e: e.tensor_tensor(out=p[:, 0:w], in0=ee[:, 0:w], in1=masks[:, pat, m0:m0 + w], op=ALU.mult),
                          reads=[t_e, t_mask], writes=[t_p])
                    gp.append((jr, n, p, t_p, q0, w, v_ap, t_v, o, t_o, fin))
                    if len(gp) > 3:
                        pv_emit()

            load_pair(0)
            load_v(0)
            hi = 0
            for (b, hp) in pairs:
                for hh in range(2):
                    if hi + 1 < len(heads):
                        load_pair(hi + 1)
                        load_v(hi + 1)
                    hi += 1
                    q1, t_q1, k1, t_k1 = pl.pop(0)
                    Sc.op("act", lambda e: e.activation(out=q4[0:64], in_=q1[0:64, :].rearrange("p (l r) -> p r l", r=4), func=AF.Copy), reads=[t_q1], writes=[t_q4])
                    Sc.op("act", lambda e: e.activation(out=k4[0:64, :, 64:64 + 1024], in_=k1[0:64, 64:64 + S].rearrange("p (l r) -> p r l", r=4), func=AF.Copy),
                          reads=[t_k1], writes=[t_k4])
                    Sc.op("act", lambda e: e.activation(out=q16[0:64], in_=q1[0:64, :].rearrange("p (l r) -> p r l", r=16), func=AF.Copy), reads=[t_q1], writes=[t_q16])
                    Sc.op("act", lambda e: e.activation(out=k16[0:64, :, 64:64 + 256], in_=k1[0:64, 64:64 + S].rearrange("p (l r) -> p r l", r=16), func=AF.Copy),
                          reads=[t_k1], writes=[t_k16])
                    v1t, v4t, v16t, t_v = hl.pop(0)
                    hs = slice(0, 128)
                    U, t_U = Ur.next()
                    for m in range(8):
                        o, t_o = RR["o"].next()
                        band(hs, lambda jr: k1[hs, 128 * (4 * m + jr):128 * (4 * m + jr) + 128], t_k1,
                             lambda a_, b_: q1[hs, m * TT + a_:m * TT + b_], t_q1, TT,
                             lambda jr, m=m, v1t=v1t: v1t[:, 4 * m + jr, :], t_v, m == 0, m == 7, o, t_o,
                             lambda o=o, t_o=t_o, U=U, t_U=t_U, m=m: Sc.op("act", lambda e: e.activation(out=U[0:65, m * TT:(m + 1) * TT], in_=o[0:65, :], func=AF.Copy),
                                                                         reads=[t_o], writes=[t_U]))
                    U4 = U[0:65, :].rearrange("p (l r) -> p r l", r=4)
                    for r in range(4):
                        for m in range(2):
                            o, t_o = RR["o"].next()
                            band(hs, lambda jr: k4[hs, r, 128 * (4 * m + jr):128 * (4 * m + jr) + 128], t_k4,
                                 lambda a_, b_: q4[hs, r, m * TT + a_:m * TT + b_], t_q4, TT,
                                 lambda jr, m=m, r=r, v4t=v4t: v4t[:, r, 4 * m + jr, :], t_v, m == 0, m == 1, o, t_o,
                                 lambda o=o, t_o=t_o, U4=U4, t_U=t_U, m=m, r=r: Sc.op("dve", lambda e: e.tensor_tensor(
                                     out=U4[:, r, m * TT:(m + 1) * TT], in0=U4[:, r, m * TT:(m + 1) * TT], in1=o[0:65, :], op=ALU.add), reads=[t_o], writes=[t_U]))
                    U16 = U[0:65, :].rearrange("p (l r) -> p r l", r=16)
                    for r in range(16):
                        o, t_o = RR["o"].next()
                        band(hs, lambda jr: k16[hs, r, 128 * jr:128 * jr + 128], t_k16, lambda a_, b_: q16[hs, r, a_:b_], t_q16, 256,
                             lambda jr, r=r, v16t=v16t: v16t[:, r, jr, :], t_v, True, True, o, t_o,
                             lambda o=o, t_o=t_o, U16=U16, t_U=t_U, r=r: Sc.op("dve", lambda e: e.tensor_tensor(
                                 out=U16[:, r, :], in0=U16[:, r, :], in1=o[0:65, 0:256], op=ALU.add), reads=[t_o], writes=[t_U]))
                    while gp:
                        pv_emit()
                    h = 2 * hp + hh
                    for nt in range(S // TT):
                        cs = slice(nt * TT, (nt + 1) * TT)
                        rc, t_rc = RR["rec"].next()
                        Sc.op("dve", lambda e: e.reciprocal(out=rc[64:65, :], in_=U[64:65, cs]), reads=[t_U], writes=[t_rc])
                        bc, t_bc = RR["bc"].next()
                        Sc.op("pe", lambda e: e.matmul(bc[0:64, :], lhsT=self.ones_f[64:65, 0:64], rhs=rc[64:65, :], start=True, stop=True),
                              reads=[t_rc, self.t_const], writes=[t_bc])
                        ot, t_ot = RR["outb"].next()
                        Sc.op("dve", lambda e: e.tensor_tensor(out=ot[0:64, :], in0=U[0:64, cs], in1=bc[0:64, :], op=ALU.mult),
                              reads=[t_U, t_bc], writes=[t_ot])
                        Sc.dma("sp", [(self.scr["at1"][64 * h:64 * h + 64, b * S + nt * TT:b * S + (nt + 1) * TT], ot[0:64, :])],
                               reads=[t_ot], writes=[Tok()])

    def phase_p6(self):
        nc, Sc, I = self.nc, self.S, self.I
        NFC = DFE // 128
        with ExitStack() as es:
            sb = lambda n, s, d: es.enter_context(nc.sbuf_tensor(_un(n), s, d))
            R = self.mk_rings(es, nsq=2, nf32=4, nrstd=2, nss=1, nmm=3)
            R["y"] = Ring([(es.enter_context(nc.psum_tensor(_un(f"yps{i}"), [128, TT], F32)), Tok(x=True)) for i in range(4)])
            self.mk_stage(es, n=2, w=1024, nb=0)
            w_out = sb("w_out", [128, 8, D], BF16)
            t_w = Tok()
            rr = lambda a: a.rearrange("(kc p) n -> p kc n", p=128)
            for kc in range(8):
                self.load_cast(w_out[:, kc, :], rr(I["w_out1"])[:, kc, :], t_w)
            router = sb("router", [128, 8, NE], F32)
            ident = sb("ident", [128, 128], F32)
            sel = sb("sel", [8, 1024], F32)
            t_c = Tok()
            Sc.dma("sp", [(router[:], rr(I["router"])), (ident[:], I["ident"][:, :]), (sel[:], I["sel"][:, :])], writes=[t_c])
            xt, t_x = sb("xt", [128, 8, TT], F32), toks(8)
            at, t_at = sb("at", [128, 8, TT], BF16), toks(8)
            hT, t_h = sb("hT", [128, 8, TT], BF16), toks(8)
            h32, t_h32 = sb("h32", [128, 8, TT], F32), toks(8)
            H, t_H = sb("H", [128, NFC, TT], BF16), toks(NFC)
            gb, t_gb = sb("gb", [128, NE, TT], F32), toks(NE)
            lg, t_lg = sb("lg", [128, 4, NE], F32), Tok()
            mx, t_mx = sb("mx", [128, 4, 8], F32), Tok()
            ex, t_ex = sb("ex", [128, 4, NE], F32), Tok()
            mk, t_mk = sb("mk", [128, 4, NE], F32), Tok()
            sm, t_sm = sb("sm", [128, 8], F32), Tok()
            gate, t_gate = sb("gate", [128, 4, NE], F32), Tok()
            gT, t_gT = sb("gT", [8, TT], F32), Tok()
            wgr = Ring([(sb(f"wg{i}", [128, 8, 256], BF16), Tok()) for i in range(3)])
            wur = Ring([(sb(f"wu{i}", [128, 8, 256], BF16), Tok()) for i in range(3)])
            wdr = Ring([(sb(f"wd{i}", [128, 4, 512], BF16), Tok()) for i in range(3)])
            xsrc = self.scr["x2T"].rearrange("(c p) t -> p c t", p=128)
            asrc = self.scr["at1"].rearrange("(c p) t -> p c t", p=128)
            odst = self.out.rearrange("(c p) t -> p c t", p=128)
            NT_RUN = int(os.environ.get('K_NT6', NT))
            for it in range(NT_RUN):
                b, t0 = it // (S // TT), it * TT
                for c in range(8):
                    Sc.dma("sp", [(xt[:, c, :], xsrc[:, c, t0:t0 + TT])], writes=[t_x[c]])
                    Sc.dma("sp", [(at[:, c, :], asrc[:, c, t0:t0 + TT])], writes=[t_at[c]])
                self.outproj_residual(R, w_out, t_w, at, t_at, xt, t_x, 1, b)
                self.norm_tile(R, xt, t_x, 1, 1, b, hT, t_h, h32=h32, t_h32=t_h32)
                lps, t_lps = R["mm"].next()
                for u in range(4):
                    def f(e):
                        for kc in range(8):
                            last = e.matmul(lps[:, u * NE:(u + 1) * NE], lhsT=h32[:, kc, u * 128:(u + 1) * 128], rhs=router[:, kc, :],
                                            start=(kc == 0), stop=(kc == 7))
                        return last
                    Sc.op("pe", f, reads=t_h32 + [t_c], writes=[t_lps])
                Sc.op("dve", lambda e: e.tensor_copy(out=lg[:].rearrange("p u e -> p (u e)"), in_=lps[:, 0:4 * NE]), reads=[t_lps], writes=[t_lg])
                for u in range(4):
                    Sc.op("dve", lambda e: e.max(out=mx[:, u, :], in_=lg[:, u, :]), reads=[t_lg], writes=[t_mx])
                Sc.op("dve", lambda e: e.tensor_scalar(out=sm[:, 0:4], in0=mx[:, :, 0], scalar1=-1.0, scalar2=None, op0=ALU.mult),
                      reads=[t_mx], writes=[t_sm])
                for u in range(4):
                    Sc.op("act", lambda e: e.activation(out=ex[:, u, :], in_=lg[:, u, :], func=AF.Exp, bias=sm[:, u:u + 1], scale=1.0),
                          reads=[t_lg, t_sm], writes=[t_ex])
                    Sc.op("dve", lambda e: e.tensor_scalar(out=mk[:, u, :], in0=lg[:, u, :], scalar1=mx[:, u, 1:2], scalar2=None, op0=ALU.is_ge),
                          reads=[t_lg, t_mx], writes=[t_mk])
                Sc.op("dve", lambda e: e.tensor_tensor(out=mk[:].rearrange("p u e -> p (u e)"), in0=mk[:].rearrange("p u e -> p (u e)"),
                                                       in1=ex[:].rearrange("p u e -> p (u e)"), op=ALU.mult), reads=[t_ex], writes=[t_mk])
                Sc.op("dve", lambda e: e.tensor_reduce(out=sm[:, 4:8], in_=mk[:], axis=AX.X, op=ALU.add), reads=[t_mk], writes=[t_sm])
                Sc.op("dve", lambda e: e.reciprocal(out=sm[:, 4:8], in_=sm[:, 4:8]), reads=[], writes=[t_sm])
                for u in range(4):
                    Sc.op("dve", lambda e: e.tensor_scalar(out=gate[:, u, :], in0=mk[:, u, :], scalar1=sm[:, 4 + u:5 + u], scalar2=None, op0=ALU.mult),
                          reads=[t_mk, t_sm], writes=[t_gate])
                gps, t_gps = R["mm"].next()
                for u in range(4):
                    Sc.op("pe", lambda e: e.matmul(gps[0:NE, u * 128:(u + 1) * 128], lhsT=gate[:, u, :], rhs=ident[:], start=True, stop=True),
                          reads=[t_gate, t_c], writes=[t_gps])
                Sc.op("dve", lambda e: e.tensor_copy(out=gT[:], in_=gps[0:NE, :]), reads=[t_gps], writes=[t_gT])
                for e_ in range(NE):
                    bps, t_bps = R["mm"].next()
                    Sc.op("pe", lambda e: e.matmul(bps[:], lhsT=sel[:, e_ * 128:(e_ + 1) * 128], rhs=gT[:], start=True, stop=True),
                          reads=[t_gT, t_c], writes=[t_bps])
                    Sc.op("act", lambda e: e.activation(out=gb[:, e_, :], in_=bps[:], func=AF.Copy), reads=[t_bps], writes=[t_gb[e_]])
                for e_ in range(NE):
                    wg_src = rr(self.scr["mwg"][e_])
                    wu_src = rr(self.scr["mwu"][e_])
                    wd_src = self.scr["mwd"][e_].rearrange("(fc p) d -> p fc d", p=128)
                    for fg in range(NFC // 2):
                        wg, t_wg = wgr.next()
                        wu, t_wu = wur.next()
                        Sc.dma("sp", [(wg[:], wg_src[:, :, fg * 256:(fg + 1) * 256])], writes=[t_wg])
                        Sc.dma("sp", [(wu[:], wu_src[:, :, fg * 256:(fg + 1) * 256])], writes=[t_wu])
                        for fl in range(2):
                            f_ = fg * 2 + fl
                            gp_, t_g = R["mm"].next()
                            up_, t_u = R["mm"].next()
                            for (pt, tp, w, tw) in ((gp_, t_g, wg, t_wg), (up_, t_u, wu, t_wu)):
                                def f(e):
                                    for kc in range(8):
                                        last = e.matmul(pt[:], lhsT=w[:, kc, fl * 128:(fl + 1) * 128], rhs=hT[:, kc, :],
                                                        start=(kc == 0), stop=(kc == 7))
                                    return last
                                Sc.op("pe", f, reads=t_h + [tw], writes=[tp])
                            sg, t_sg = R["f32"].next()
                            Sc.op("act", lambda e: e.activation(out=sg[:], in_=gp_[:], func=AF.Silu), reads=[t_g], writes=[t_sg])
                            Sc.op("dve", lambda e: e.tensor_tensor(out=H[:, f_, :], in0=sg[:], in1=up_[:], op=ALU.mult),
                                  reads=[t_sg, t_u], writes=[t_H[f_]])
                    for half in range(2):
                        ys = [R["y"].next() for _ in range(4)]
                        for fg in range(NFC // 4):
                            wd, t_wd = wdr.next()
                            Sc.dma("sp", [(wd[:], wd_src[:, 4 * fg:4 * fg + 4, half * 512:(half + 1) * 512])], writes=[t_wd])
                            for fl in range(4):
                                f_ = fg * 4 + fl

                                def f(e):
                                    for j in range(4):
                                        last = e.matmul(ys[j][0][:], lhsT=wd[:, fl, j * 128:(j + 1) * 128], rhs=H[:, f_, :],
                                                        start=(f_ == 0), stop=(f_ == NFC - 1))
                                    return last
                                Sc.op("pe", f, reads=[t_H[f_], t_wd], writes=[y[1] for y in ys])
                        for j in range(4):
                            dc = half * 4 + j
                            if e_ == 0:
                                Sc.op("dve", lambda e: e.tensor_tensor(out=h32[:, dc, :], in0=ys[j][0][:], in1=gb[:, e_, :], op=ALU.mult),
                                      reads=[ys[j][1], t_gb[e_]], writes=[t_h32[dc]])
                            else:
                                tm, t_tm = R["f32"].next()
                                Sc.op("dve", lambda e: e.tensor_tensor(out=tm[:], in0=ys[j][0][:], in1=gb[:, e_, :], op=ALU.mult),
                                      reads=[ys[j][1], t_gb[e_]], writes=[t_tm])
                                Sc.op("pool", lambda e: e.tensor_tensor(out=h32[:, dc, :], in0=h32[:, dc, :], in1=tm[:], op=ALU.add),
                                      reads=[t_tm], writes=[t_h32[dc]])
                for dc in range(8):
                    Sc.op("dve", lambda e: e.scalar_tensor_tensor(out=xt[:, dc, :], in0=h32[:, dc, :], scalar=self.M[1][:, 40 + dc, b:b + 1],
                                                                  in1=xt[:, dc, :], op0=ALU.mult, op1=ALU.add),
                          reads=[t_h32[dc], self.t_mod], writes=[t_x[dc]])
                    Sc.dma("sp", [(odst[:, dc, t0:t0 + TT], xt[:, dc, :])], reads=[t_x[dc]], writes=[Tok()])

    def phase_p6a(self):
        nc, Sc, I = self.nc, self.S, self.I
        with ExitStack() as es:
            sb = lambda n, s, d: es.enter_context(nc.sbuf_tensor(_un(n), s, d))
            R = self.mk_rings(es, nsq=2, nf32=4, nrstd=2, nss=1, nmm=5)
            self.mk_stage(es, n=2, w=1024, nb=0)
            w_out = sb("w_out", [128, 8, D], BF16)
            t_w = Tok()
            rr = lambda a: a.rearrange("(kc p) n -> p kc n", p=128)
            for kc in range(8):
                self.load_cast(w_out[:, kc, :], rr(I["w_out1"])[:, kc, :], t_w)
            router = sb("router", [128, 8, NE], F32)
            ident = sb("ident", [128, 128], F32)
            identb = sb("identb", [128, 128], BF16)
            t_c = Tok()
            Sc.dma("sp", [(router[:], rr(I["router"])), (ident[:], I["ident"][:, :])], writes=[t_c])
            Sc.op("dve", lambda e: e.tensor_copy(out=identb[:], in_=ident[:]), reads=[t_c], writes=[t_c])
            xt, t_x = sb("xt", [128, 8, TT], F32), toks(8)
            at, t_at = sb("at", [128, 8, TT], BF16), toks(8)
            hT, t_h = sb("hT", [128, 8, TT], BF16), toks(8)
            h32, t_h32 = sb("h32", [128, 8, TT], F32), toks(8)
            htr = Ring([(sb(f"htok{i}", [128, 4, D], BF16), Tok()) for i in range(2)])
            lg, t_lg = sb("lg", [128, 4, NE], F32), Tok()
            mx, t_mx = sb("mx", [128, 4, 8], F32), Tok()
            sm, t_sm = sb("sm", [128, 12], F32), Tok()
            zt, t_z = sb("zt", [128, 4, D], BF16), Tok()
            Sc.op("dve", lambda e: e.memset(zt[:].rearrange("p a b -> p (a b)"), 0.0), writes=[t_z])
            Xz = self.scr["Xs"].rearrange("(n p) f -> p n f", p=128)
            for b_ in range(40):
                Sc.dma("sp", [(Xz[:, 4 * b_:4 * b_ + 4, :], zt[:])], reads=[t_z], writes=[Tok()])
            xsrc = self.scr["x2T"].rearrange("(c p) t -> p c t", p=128)
            asrc = self.scr["at1"].rearrange("(c p) t -> p c t", p=128)
            x3dst = self.scr["x3T"].rearrange("(c p) t -> p c t", p=128)
            hdst = self.scr["h_tok"].rearrange("(n p) f -> p n f", p=128)
            for it in range(NT):
                b, t0 = it // (S // TT), it * TT
                for c in range(8):
                    Sc.dma("sp", [(xt[:, c, :], xsrc[:, c, t0:t0 + TT])], writes=[t_x[c]])
                    Sc.dma("sp", [(at[:, c, :], asrc[:, c, t0:t0 + TT])], writes=[t_at[c]])
                self.outproj_residual(R, w_out, t_w, at, t_at, xt, t_x, 1, b)
                for c in range(8):
                    Sc.dma("sp", [(x3dst[:, c, t0:t0 + TT], xt[:, c, :])], reads=[t_x[c]], writes=[Tok()])
                self.norm_tile(R, xt, t_x, 1, 1, b, hT, t_h, h32=h32, t_h32=t_h32)
                lps, t_lps = R["mm"].next()
                for u in range(4):
                    def f(e):
                        for kc in range(8):
                            last = e.matmul(lps[:, u * NE:(u + 1) * NE], lhsT=h32[:, kc, u * 128:(u + 1) * 128], rhs=router[:, kc, :],
                                            start=(kc == 0), stop=(kc == 7))
                        return last
                    Sc.op("pe", f, reads=t_h32 + [t_c], writes=[t_lps])
                Sc.op("dve", lambda e: e.tensor_copy(out=lg[:].rearrange("p u e -> p (u e)"), in_=lps[:, 0:4 * NE]), reads=[t_lps], writes=[t_lg])
                for u in range(4):
                    Sc.op("dve", lambda e: e.max(out=mx[:, u, :], in_=lg[:, u, :]), reads=[t_lg], writes=[t_mx])
                for u in range(4):
                    tt = it * 4 + u
                    Sc.op("dve", lambda e: e.tensor_scalar(out=self.rt_m1[:, tt, :], in0=lg[:, u, :], scalar1=mx[:, u, 0:1], scalar2=None, op0=ALU.is_equal),
                          reads=[t_lg, t_mx], writes=[self.t_rt])
                    Sc.op("dve", lambda e: e.tensor_scalar(out=self.rt_m2[:, tt, :], in0=lg[:, u, :], scalar1=mx[:, u, 1:2], scalar2=None, op0=ALU.is_equal),
                          reads=[t_lg, t_mx], writes=[self.t_rt])
                Sc.op("dve", lambda e: e.tensor_tensor(out=sm[:, 0:4], in0=mx[:, :, 1], in1=mx[:, :, 0], op=ALU.subtract), reads=[t_mx], writes=[t_sm])
                Sc.op("act", lambda e: e.activation(out=sm[:, 4:8], in_=sm[:, 0:4], func=AF.Exp), reads=[t_sm], writes=[t_sm])
                Sc.op("dve", lambda e: e.tensor_scalar(out=sm[:, 8:12], in0=sm[:, 4:8], scalar1=1.0, scalar2=None, op0=ALU.add), reads=[t_sm], writes=[t_sm])
                Sc.op("dve", lambda e: e.reciprocal(out=self.rt_g[:, it * 4:it * 4 + 4, 0], in_=sm[:, 8:12]), reads=[t_sm], writes=[self.t_rt])
                Sc.op("dve", lambda e: e.tensor_tensor(out=self.rt_g[:, it * 4:it * 4 + 4, 1], in0=sm[:, 4:8], in1=self.rt_g[:, it * 4:it * 4 + 4, 0], op=ALU.mult),
                      reads=[t_sm], writes=[self.t_rt])
                htok, t_ht = htr.next()
                k_ = 0
                for u in range(4):
                    for half in range(2):
                        ps, t_ps = R["mm"].next()

                        def f(e):
                            for k4 in range(4):
                                last = e.matmul(ps[:, k4 * 128:(k4 + 1) * 128], lhsT=hT[:, half * 4 + k4, u * 128:(u + 1) * 128], rhs=identb[:],
                                                start=True, stop=True)
                            return last
                        Sc.op("pe", f, reads=t_h + [t_c], writes=[t_ps])
                        eng = "act" if k_ % 2 else "dve"
                        k_ += 1
                        if eng == "act":
                            Sc.op("act", lambda e: e.activation(out=htok[:, u, half * 512:(half + 1) * 512], in_=ps[:], func=AF.Copy), reads=[t_ps], writes=[t_ht])
                        else:
                            Sc.op("dve", lambda e: e.tensor_copy(out=htok[:, u, half * 512:(half + 1) * 512], in_=ps[:]), reads=[t_ps], writes=[t_ht])
                Sc.dma("sp", [(hdst[:, it * 4:it * 4 + 4, :], htok[:])], reads=[t_ht], writes=[Tok()])

    def phase_p6b(self):
        nc, Sc, I = self.nc, self.S, self.I
        I32 = mybir.dt.int32
        with ExitStack() as es:
            sb = lambda n, s, d: es.enter_context(nc.sbuf_tensor(_un(n), s, d))
            pre_ps = es.enter_context(nc.psum_tensor(_un("pre_ps"), [128, TT], F32))
            t_ps = Tok(x=True)
            tri = sb("tri", [128, 128], F32)
            onesF = sb("onesF", [128, 128], F32)
            cst = sb("cst", [128, 64], F32)
            t_c = Tok()
            Sc.dma("sp", [(tri[:], I["tri"][:, :]), (cst[:], I["cst"][:, :])], writes=[t_c])
            Sc.op("dve", lambda e: e.memset(onesF[:], 1.0), writes=[t_c])
            t_a = Tok()
            m = sb("m", [128, 64, NE], F32)
            cA = sb("cA", [128, 64, NE], F32)
            cB = sb("cB", [128, 64, NE], F32)
            slot = sb("slot", [128, 64, NE], F32)
            sv = sb("sv", [128, 64], F32)
            sf = sb("sf", [128, 2, 64], F32)
            eb = sb("eb", [128, 40], F32)
            wf = sb("wf", [128, 40, 14], F32)
            colsum, pre, cnt, nblk, pend, base = (sv[:, 0:8], sv[:, 8:16], sv[:, 16:24], sv[:, 24:32], sv[:, 32:40], sv[:, 40:48])
            fl = lambda t: t[:].rearrange("p t e -> p (t e)")
            D_ = lambda fn, **kw: Sc.op("dve", fn, reads=[t_a, t_c, self.t_rt], writes=[t_a])
            D_(lambda e: e.tensor_tensor(out=fl(m), in0=fl(self.rt_m1), in1=fl(self.rt_m2), op=ALU.add))
            D_(lambda e: e.tensor_reduce(out=colsum, in_=m[:].rearrange("p t e -> p e t"), axis=AX.X, op=ALU.add))
            Sc.op("pe", lambda e: e.matmul(pre_ps[:, 0:8], lhsT=tri[:], rhs=colsum, start=True, stop=True), reads=[t_a, t_c], writes=[t_ps])
            Sc.op("pe", lambda e: e.matmul(pre_ps[:, 8:16], lhsT=onesF[:], rhs=colsum, start=True, stop=True), reads=[t_a, t_c], writes=[t_ps])
            Sc.op("dve", lambda e: e.tensor_copy(out=sv[:, 8:24], in_=pre_ps[:, 0:16]), reads=[t_ps, t_a], writes=[t_a])
            D_(lambda e: e.tensor_scalar(out=nblk, in0=cnt, scalar1=0.0, scalar2=None, op0=ALU.is_gt))
            for k in range(1, 17):
                D_(lambda e: e.scalar_tensor_tensor(out=nblk, in0=cnt, scalar=512.0 * k, in1=nblk, op0=ALU.is_gt, op1=ALU.add))
            D_(lambda e: e.tensor_scalar(out=sv[:, 32:33], in0=sv[:, 24:25], scalar1=512.0, scalar2=None, op0=ALU.mult))
            for e_ in range(1, NE):
                D_(lambda e: e.scalar_tensor_tensor(out=sv[:, 32 + e_:33 + e_], in0=sv[:, 24 + e_:25 + e_], scalar=512.0, in1=sv[:, 31 + e_:32 + e_],
                                                    op0=ALU.mult, op1=ALU.add))
            D_(lambda e: e.scalar_tensor_tensor(out=base, in0=nblk, scalar=-512.0, in1=pend, op0=ALU.mult, op1=ALU.add))
            D_(lambda e: e.tensor_tensor(out=base, in0=base, in1=pre, op=ALU.add))
            D_(lambda e: e.tensor_copy(out=fl(cA), in_=fl(m)))
            src_, dst_ = cA, cB
            for sft in (1, 2, 4, 8, 16, 32):
                D_(lambda e: e.tensor_tensor(out=dst_[:, sft:, :], in0=src_[:, sft:, :], in1=src_[:, 0:64 - sft, :], op=ALU.add))
                D_(lambda e: e.tensor_copy(out=dst_[:, 0:sft, :], in_=src_[:, 0:sft, :]))
                src_, dst_ = dst_, src_
            D_(lambda e: e.tensor_tensor(out=fl(src_), in0=fl(src_), in1=fl(m), op=ALU.subtract))
            for e_ in range(NE):
                D_(lambda e: e.tensor_scalar(out=slot[:, :, e_], in0=src_[:, :, e_], scalar1=sv[:, 40 + e_:41 + e_], scalar2=None, op0=ALU.add))
            for k, mm_ in ((0, self.rt_m1), (1, self.rt_m2)):
                D_(lambda e: e.tensor_tensor(out=fl(dst_), in0=fl(mm_), in1=fl(slot), op=ALU.mult))
                D_(lambda e: e.tensor_reduce(out=sf[:, k, :], in_=dst_[:], axis=AX.X, op=ALU.add))
            Sc.op("dve", lambda e: e.tensor_copy(out=self.rt_s[:].rearrange("p a b -> p (a b)"), in_=sf[:].rearrange("p a b -> p (a b)")),
                  reads=[t_a], writes=[self.t_rt])
            D_(lambda e: e.tensor_scalar(out=eb[:], in0=cst[:, 1:41], scalar1=sv[:, 32:33], scalar2=None, op0=ALU.is_ge))
            for e_ in range(1, NE):
                D_(lambda e: e.scalar_tensor_tensor(out=eb[:], in0=cst[:, 1:41], scalar=sv[:, 32 + e_:33 + e_], in1=eb[:], op0=ALU.is_ge, op1=ALU.add))
            D_(lambda e: e.tensor_scalar(out=eb[:], in0=eb[:], scalar1=float(NE - 1), scalar2=None, op0=ALU.min))
            D_(lambda e: e.tensor_scalar(out=eb[:], in0=eb[:], scalar1=1792.0, scalar2=cst[:, 0:1], op0=ALU.mult, op1=ALU.add))
            for j in range(14):
                D_(lambda e: e.tensor_scalar(out=wf[:, :, j], in0=eb[:], scalar1=128.0 * j, scalar2=None, op0=ALU.add))
            Sc.op("dve", lambda e: e.tensor_copy(out=self.rt_w[:].rearrange("p a b -> p (a b)"), in_=wf[:].rearrange("p a b -> p (a b)")),
                  reads=[t_a], writes=[self.t_rt])
            hr = Ring([(sb(f"hrow{i}", [128, D], BF16), Tok()) for i in range(3)])
            hsrc = self.scr["h_tok"].rearrange("(n p) f -> p n f", p=128)
            Xs = self.scr["Xs"]
            for tt in range(64):
                ht, t_ht = hr.next()
                Sc.dma("sp", [(ht[:], hsrc[:, tt, :])], writes=[t_ht])
                fns = [lambda g, k=k: g.indirect_dma_start(out=Xs[:, :], out_offset=bass.IndirectOffsetOnAxis(ap=self.rt_s[:, k, tt:tt + 1], axis=0),
                                                           in_=ht[:, :], in_offset=None) for k in range(2)]
                Sc.dma_fn("pool", fns, reads=[t_ht, self.t_rt], writes=[Tok()])

    def phase_p7(self):
        nc, Sc, I = self.nc, self.S, self.I
        NFC = DFE // 128
        NBLK = int(os.environ.get("K_NBLK", 39))
        with ExitStack() as es:
            sb = lambda n, s, d: es.enter_context(nc.sbuf_tensor(_un(n), s, d))
            ps = lambda n: es.enter_context(nc.psum_tensor(_un(n), [128, TT], F32))
            mmr = Ring([(ps(f"mm{i}"), Tok(x=True)) for i in range(4)])
            yr = Ring([(ps(f"y{i}"), Tok(x=True)) for i in range(4)])
            ident = sb("ident", [128, 128], F32)
            identb = sb("identb", [128, 128], BF16)
            t_c = Tok()
            Sc.dma("sp", [(ident[:], I["ident"][:, :])], writes=[t_c])
            Sc.op("dve", lambda e: e.tensor_copy(out=identb[:], in_=ident[:]), reads=[t_c], writes=[t_c])
            xkr = Ring([(sb(f"xtok{i}", [128, 4, D], BF16), Tok()) for i in range(2)])
            xTr = Ring([(sb(f"xT{i}", [128, 8, TT], BF16), toks(8)) for i in range(2)])
            wgr = Ring([(sb(f"wg{i}", [128, 8, 256], BF16), Tok()) for i in range(3)])
            wur = Ring([(sb(f"wu{i}", [128, 8, 256], BF16), Tok()) for i in range(3)])
            wdr = Ring([(sb(f"wd{i}", [128, 4, 512], BF16), Tok()) for i in range(3)])
            H, t_H = sb("H", [128, NFC, TT], BF16), toks(NFC)
            sgr = Ring([(sb(f"sg{i}", [128, TT], F32), Tok()) for i in range(3)])
            yTr = Ring([(sb(f"yT{i}", [128, TT], F32), Tok()) for i in range(4)])
            ytok, t_yt = sb("ytok", [128, 4, D], F32), Tok()
            Xs = self.scr["Xs"].rearrange("(n p) f -> p n f", p=128)
            Ys = self.scr["Ys"].rearrange("(n p) f -> p n f", p=128)
            gath = lambda dst, src, idx: (lambda g: g.indirect_dma_start(out=dst, out_offset=None, in_=src,
                                                                         in_offset=bass.IndirectOffsetOnAxis(ap=idx, axis=0)))
            loaded = []

            def load_x(b_):
                xk, t_xk = xkr.next()
                Sc.dma("sp", [(xk[:], Xs[:, 4 * b_:4 * b_ + 4, :])], writes=[t_xk])
                loaded.append((xk, t_xk))
            load_x(0)
            for b_ in range(NBLK):
                if b_ + 1 < NBLK:
                    load_x(b_ + 1)
                xk, t_xk = loaded.pop(0)
                xT, t_xT = xTr.next()
                for kc in range(8):
                    pt, t_pt = mmr.next()

                    def f(e):
                        for u in range(4):
                            last = e.matmul(pt[:, u * 128:(u + 1) * 128], lhsT=xk[:, u, kc * 128:(kc + 1) * 128], rhs=identb[:], start=True, stop=True)
                        return last
                    Sc.op("pe", f, reads=[t_xk, t_c], writes=[t_pt])
                    if kc % 2:
                        Sc.op("act", lambda e: e.activation(out=xT[:, kc, :], in_=pt[:], func=AF.Copy), reads=[t_pt], writes=[t_xT[kc]])
                    else:
                        Sc.op("dve", lambda e: e.tensor_copy(out=xT[:, kc, :], in_=pt[:]), reads=[t_pt], writes=[t_xT[kc]])
                for fg in range(NFC // 2):
                    wg, t_wg = wgr.next()
                    wu, t_wu = wur.next()
                    Sc.dma_fn("pool", [gath(wg[:].rearrange("p k c -> p (k c)"), self.scr["mwg2"][:, :], self.rt_w[:, b_, fg:fg + 1])],
                              reads=[self.t_rt], writes=[t_wg])
                    Sc.dma_fn("pool", [gath(wu[:].rearrange("p k c -> p (k c)"), self.scr["mwu2"][:, :], self.rt_w[:, b_, fg:fg + 1])],
                              reads=[self.t_rt], writes=[t_wu])
                    for fl in range(2):
                        f_ = fg * 2 + fl
                        gp_, t_g = mmr.next()
                        up_, t_u = mmr.next()
                        for (pt, tp, w, tw) in ((gp_, t_g, wg, t_wg), (up_, t_u, wu, t_wu)):
                            def f(e):
                                for kc in range(8):
                                    last = e.matmul(pt[:], lhsT=w[:, kc, fl * 128:(fl + 1) * 128], rhs=xT[:, kc, :], start=(kc == 0), stop=(kc == 7))
                                return last
                            Sc.op("pe", f, reads=t_xT + [tw], writes=[tp])
                        sg, t_sg = sgr.next()
                        Sc.op("act", lambda e: e.activation(out=sg[:], in_=gp_[:], func=AF.Silu), reads=[t_g], writes=[t_sg])
                        Sc.op("dve", lambda e: e.tensor_tensor(out=H[:, f_, :], in0=sg[:], in1=up_[:], op=ALU.mult), reads=[t_sg, t_u], writes=[t_H[f_]])
                for half in range(2):
                    ys = [yr.next() for _ in range(4)]
                    for g4 in range(NFC // 4):
                        wd, t_wd = wdr.next()
                        Sc.dma_fn("pool", [gath(wd[:].rearrange("p k c -> p (k c)"), self.scr["mwd2"][:, :], self.rt_w[:, b_, half * 7 + g4:half * 7 + g4 + 1])],
                                  reads=[self.t_rt], writes=[t_wd])
                        for fl in range(4):
                            f_ = g4 * 4 + fl

                            def f(e):
                                for j in range(4):
                                    last = e.matmul(ys[j][0][:], lhsT=wd[:, fl, j * 128:(j + 1) * 128], rhs=H[:, f_, :], start=(f_ == 0), stop=(f_ == NFC - 1))
                                return last
                            Sc.op("pe", f, reads=[t_H[f_], t_wd], writes=[y[1] for y in ys])
                    yts = []
                    for j in range(4):
                        yT, t_yT = yTr.next()
                        if j % 2:
                            Sc.op("act", lambda e: e.activation(out=yT[:], in_=ys[j][0][:], func=AF.Copy), reads=[ys[j][1]], writes=[t_yT])
                        else:
                            Sc.op("dve", lambda e: e.tensor_copy(out=yT[:], in_=ys[j][0][:]), reads=[ys[j][1]], writes=[t_yT])
                        yts.append((yT, t_yT))
                    for u in range(4):
                        pt, t_pt = mmr.next()

                        def f(e):
                            for j in range(4):
                                last = e.matmul(pt[:, j * 128:(j + 1) * 128], lhsT=yts[j][0][:, u * 128:(u + 1) * 128], rhs=ident[:], start=True, stop=True)
                            return last
                        Sc.op("pe", f, reads=[y[1] for y in yts] + [t_c], writes=[t_pt])
                        if u % 2:
                            Sc.op("act", lambda e: e.activation(out=ytok[:, u, half * 512:(half + 1) * 512], in_=pt[:], func=AF.Copy), reads=[t_pt], writes=[t_yt])
                        else:
                            Sc.op("dve", lambda e: e.tensor_copy(out=ytok[:, u, half * 512:(half + 1) * 512], in_=pt[:]), reads=[t_pt], writes=[t_yt])
                Sc.dma("sp", [(Ys[:, 4 * b_:4 * b_ + 4, :], ytok[:])], reads=[t_yt], writes=[Tok()])

    def phase_p8(self):
        nc, Sc, I = self.nc, self.S, self.I
        with ExitStack() as es:
            sb = lambda n, s, d: es.enter_context(nc.sbuf_tensor(_un(n), s, d))
            ps = lambda n: es.enter_context(nc.psum_tensor(_un(n), [128, TT], F32))
            pr = Ring([(ps(f"pp{i}"), Tok(x=True)) for i in range(8)])
            ident = sb("ident", [128, 128], F32)
            t_c = Tok()
            Sc.dma("sp", [(ident[:], I["ident"][:, :])], writes=[t_c])
            y1r = Ring([(sb(f"y1_{i}", [128, D], F32), Tok()) for i in range(3)])
            y2r = Ring([(sb(f"y2_{i}", [128, D], F32), Tok()) for i in range(3)])
            ycr = Ring([(sb(f"yc{i}", [128, 4, D], F32), toks(4)) for i in range(2)])
            xr = Ring([(sb(f"x3_{i}", [128, 8, TT], F32), toks(8)) for i in range(2)])
            x3src = self.scr["x3T"].rearrange("(c p) t -> p c t", p=128)
            odst = self.out.rearrange("(c p) t -> p c t", p=128)
            Ysf = self.scr["Ys"]
            NT_RUN = int(os.environ.get('K_NT8', NT))
            for it in range(NT_RUN):
                b, t0 = it // (S // TT), it * TT
                xt, t_x = xr.next()
                for c in range(8):
                    Sc.dma("sp", [(xt[:, c, :], x3src[:, c, t0:t0 + TT])], writes=[t_x[c]])
                yc, t_yc = ycr.next()
                for u in range(4):
                    tt = it * 4 + u
                    y1, t_y1 = y1r.next()
                    y2, t_y2 = y2r.next()
                    for (yy, ty, k) in ((y1, t_y1, 0), (y2, t_y2, 1)):
                        Sc.dma_fn("pool", [lambda g: g.indirect_dma_start(out=yy[:, :], out_offset=None, in_=Ysf[:, :],
                                                                           in_offset=bass.IndirectOffsetOnAxis(ap=self.rt_s[:, k, tt:tt + 1], axis=0))],
                                  reads=[self.t_rt], writes=[ty])
                    Sc.op("dve", lambda e: e.tensor_scalar(out=yc[:, u, :], in0=y1[:], scalar1=self.rt_g[:, tt, 0:1], scalar2=None, op0=ALU.mult),
                          reads=[t_y1, self.t_rt], writes=[t_yc[u]])
                    Sc.op("pool", lambda e: e.scalar_tensor_tensor(out=yc[:, u, :], in0=y2[:], scalar=self.rt_g[:, tt, 1:2], in1=yc[:, u, :],
                                                                   op0=ALU.mult, op1=ALU.add), reads=[t_y2, self.t_rt], writes=[t_yc[u]]) \
                        if False else \
                        Sc.op("dve", lambda e: e.scalar_tensor_tensor(out=yc[:, u, :], in0=y2[:], scalar=self.rt_g[:, tt, 1:2], in1=yc[:, u, :],
                                                                      op0=ALU.mult, op1=ALU.add), reads=[t_y2, self.t_rt], writes=[t_yc[u]])
                for dc in range(8):
                    pt, t_pt = pr.next()

                    def f(e):
                        for u in range(4):
                            last = e.matmul(pt[:, u * 128:(u + 1) * 128], lhsT=yc[:, u, dc * 128:(dc + 1) * 128], rhs=ident[:], start=True, stop=True)
                        return last
                    Sc.op("pe", f, reads=t_yc + [t_c], writes=[t_pt])
                    Sc.op("dve", lambda e: e.scalar_tensor_tensor(out=xt[:, dc, :], in0=pt[:], scalar=self.M[1][:, 40 + dc, b:b + 1],
                                                                  in1=xt[:, dc, :], op0=ALU.mult, op1=ALU.add),
                          reads=[t_pt, self.t_mod], writes=[t_x[dc]])
                    Sc.dma("sp", [(odst[:, dc, t0:t0 + TT], xt[:, dc, :])], reads=[t_x[dc]], writes=[Tok()])

    def mk_stage(self, es, n=2, w=3584, nb=None):
        nc = self.nc
        nb = n if nb is None else nb
        self.stg = Ring([(es.enter_context(nc.sbuf_tensor(_un(f"stg{i}"), [128, w], F32)), Tok()) for i in range(n)])
        self.stgb = Ring([(es.enter_context(nc.sbuf_tensor(_un(f"stgb{i}"), [128, w], BF16)), Tok()) for i in range(nb)])

    def pump_casts(self, n, eng="pool"):
        if not hasattr(self, "pump_inflight"):
            self.pump_inflight = []
        for _ in range(n):
            if self.cast_jobs:
                job = self.cast_jobs.pop(0)
                st, t_st = self.stg.next()
                w = job[1].shape[-1]
                self.S.dma("pool", [(st[:, 0:w], job[1])], writes=[t_st])
                self.pump_inflight.append((job, st, t_st, w))
            if self.pump_inflight and (len(self.pump_inflight) > 1 or not self.cast_jobs):
                job, st, t_st, w = self.pump_inflight.pop(0)
                sb_, t_sb = self.stgb.next()
                self.S.op(eng, lambda e: e.tensor_copy(out=sb_[:, 0:w], in_=st[:, 0:w]), reads=[t_st], writes=[t_sb])
                src_sb = sb_[:, 0:w]
                if len(job) > 2:
                    src_sb = src_sb.rearrange("p (a b) -> p a b", a=job[2][0])
                self.S.dma("pool", [(job[0], src_sb)], reads=[t_sb], writes=[Tok()])

    def load_cast(self, dst, src, t_dst, eng="pool"):
        w = src.shape[-1]
        st, t_st = self.stg.next()
        self.S.dma("sp", [(st[:, 0:w], src)], writes=[t_st])
        self.S.op(eng, lambda e: e.tensor_copy(out=dst, in_=st[:, 0:w]), reads=[t_st], writes=[t_dst])


ALL_PHASES = ["p0", "p1", "p2", "p3", "p4", "p5", "p6"] if os.environ.get("K_DENSE") else ["p0", "p1", "p2", "p3", "p4", "p5", "p6a", "p6b", "p7", "p8"]


def make_in_maps(inputs, cores=range(NCORES)):
    inp = {k: np.asarray(v) for k, v in inputs.items()}
    sh = _shared_inputs(inp)
    maps = []
    for c in cores:
        xs = inp["x"][NB * c:NB * (c + 1)]
        xT = np.ascontiguousarray(xs.reshape(T, D).T)
        cs = inp["c"][NB * c:NB * (c + 1)]
        cT = np.ascontiguousarray(cs.reshape(NB, 8, 128).transpose(2, 1, 0))
        m = dict(sh)
        m["xT"] = xT
        m["cT"] = cT
        maps.append(m)
    return maps, sh["vecs"].shape[1]


def kernel(**inputs):
    maps, nvec = make_in_maps(inputs)
    prog = Prog(nvec=nvec)
    prog.build(ALL_PHASES)
    res = run_bass_kernel_spmd(prog.nc, maps, core_ids=list(range(NCORES)))
    out = np.empty((NCORES * NB, S, D), np.float32)
    for c in range(NCORES):
        out[NB * c:NB * (c + 1)] = res.results[c]["outT"].T.reshape(NB, S, D)
    return out
```

```python
import os
import numpy as np
from contextlib import ExitStack
import concourse.bass as bass
import concourse.mybir as mybir
from concourse.bass_utils import run_bass_kernel_spmd

F32 = mybir.dt.float32
BF16 = mybir.dt.bfloat16
AF = mybir.ActivationFunctionType
ALU = mybir.AluOpType
AX = mybir.AxisListType

NCORES = 8
D = 1024
S = 4096
NB = 2
T = NB * S
TT = 512
NT = T // TT
EPS = 1e-6
DFF = 2816
NE = 8
NE_IN = 1 if os.environ.get('K_SMALLMOE') else 8
DFE = 3584
THETA = 10000.0


_UN = [0]


def _un(n):
    _UN[0] += 1
    return f"t{_UN[0]}_{n}"


class Tok:
    __slots__ = ("name", "w", "r", "x")

    def __init__(self, name="", x=False):
        self.name = name
        self.w = []
        self.r = []
        self.x = x


def toks(n, name=""):
    return [Tok(f"{name}{i}") for i in range(n)]


class Ring:
    def __init__(self, items):
        self.items = list(items)
        self.i = 0

    def next(self):
        it = self.items[self.i % len(self.items)]
        self.i += 1
        return it


class Sched:
    ROT = 60000

    def __init__(self, nc, es):
        self.nc = nc
        self.es = es
        self.eng = {"pe": nc.tensor, "act": nc.scalar, "dve": nc.vector, "pool": nc.gpsimd, "sp": nc.sync}
        self.esem = {}
        self.known = {e: {} for e in self.eng}
        self.nsem = 0
        for e in self.eng:
            self.esem[e] = [self._newsem(e), 0]
        self.dq = {}
        for q, n in (("sp", 8), ("pool", 6), ("act", 2)):
            self.dq[q] = [[self._newsem("d" + q), 0] for _ in range(n)]
        self.dqi = {q: 0 for q in self.dq}
        self.all_dsems = [s for q in self.dq for s in self.dq[q]]
        self.old_esems = []

    def _newsem(self, name):
        self.nsem += 1
        return self.es.enter_context(self.nc.semaphore(f"s_{name}_{self.nsem}"))

    def _wait(self, e, evs):
        k = self.known[e]
        h = self.eng[e]
        own = self.esem[e][0] if e == "pe" else None
        for (sem, val) in evs:
            if val <= 0 or sem is own:
                continue
            key = id(sem)
            if k.get(key, 0) >= val:
                continue
            h.wait_ge(sem, val)
            k[key] = val

    def _deps(self, reads, writes, e=None):
        evs = []
        own = self.esem[e][0] if e in self.esem else None
        for t in reads:
            evs += t.w
            if t.x:
                evs += [ev for ev in t.r if ev[0] is not own]
        for t in writes:
            evs += t.w
            evs += t.r
        return evs

    @staticmethod
    def _record(ev, reads, writes):
        for t in reads:
            t.r = [x for x in t.r if x[0] is not ev[0]] + [ev]
        for t in writes:
            t.w = [ev]
            t.r = []

    def op(self, e, fn, reads=(), writes=()):
        self._wait(e, self._deps(reads, writes, e))
        inst = fn(self.eng[e])
        st = self.esem[e]
        if st[1] >= self.ROT:
            self.old_esems.append((st[0], st[1]))
            st[0] = self._newsem(e)
            st[1] = 0
        st[1] += 1
        inst.then_inc(st[0], 1)
        self._record((st[0], st[1]), reads, writes)

    def dma(self, q, pairs, reads=(), writes=()):
        if os.environ.get("K_NOSTORE") and len(reads) > 0:
            return
        evs = self._deps(reads, writes)
        slot = self.dq[q][self.dqi[q] % len(self.dq[q])]
        self.dqi[q] += 1
        evs.append((slot[0], slot[1]))
        self._wait(q, evs)
        h = self.eng[q]
        for (o, i) in pairs:
            h.dma_start(out=o, in_=i).then_inc(slot[0], 16)
        slot[1] += 16 * len(pairs)
        self._record((slot[0], slot[1]), reads, writes)

    def dma_fn(self, q, fns, reads=(), writes=()):
        evs = self._deps(reads, writes)
        slot = self.dq[q][self.dqi[q] % len(self.dq[q])]
        self.dqi[q] += 1
        evs.append((slot[0], slot[1]))
        self._wait(q, evs)
        for fn in fns:
            fn(self.eng[q]).then_inc(slot[0], 16)
        slot[1] += 16 * len(fns)
        self._record((slot[0], slot[1]), reads, writes)

    def barrier(self, engines=None):
        evs = [(st[0], st[1]) for st in self.esem.values()]
        evs += list(self.old_esems)
        evs += [(s[0], s[1]) for s in self.all_dsems]
        for e in (engines or self.eng):
            self._wait(e, evs)


def _perm_rot(n, half):
    i = np.arange(n)
    return np.where(i % (2 * half) < half, i + half, i - half)


PERM_MLA32 = _perm_rot(32, 16)
PERM_AX64 = _perm_rot(64, 16)
PERM_L164 = _perm_rot(64, 32)


def _rope_tables():
    pos = np.arange(S, dtype=np.float32)
    row = (np.arange(S) // 64).astype(np.float32)
    col = (np.arange(S) % 64).astype(np.float32)

    def inv(dh):
        return (THETA ** (-np.arange(0, dh, 2, dtype=np.float32) / dh)).astype(np.float32)

    c_mla = np.ones((96, S), np.float32)
    s_mla = np.zeros((96, S), np.float32)
    iv = inv(32)
    for i in range(32):
        ang = pos * iv[i % 16]
        c_mla[64 + i] = np.cos(ang)
        s_mla[64 + i] = (-1.0 if i < 16 else 1.0) * np.sin(ang)
    c_ax = np.zeros((128, S), np.float32)
    s_ax = np.zeros((128, S), np.float32)
    for p in range(128):
        i = p % 64
        blk, ii = i // 32, i % 32
        ang = (row if blk == 0 else col) * iv[ii % 16]
        c_ax[p] = np.cos(ang)
        s_ax[p] = (-1.0 if ii < 16 else 1.0) * np.sin(ang)
    c_l1 = np.zeros((128, S), np.float32)
    s_l1 = np.zeros((128, S), np.float32)
    iv64 = inv(64)
    for p in range(128):
        i = p % 64
        ang = pos * iv64[i % 32]
        c_l1[p] = np.cos(ang)
        s_l1[p] = (-1.0 if i < 32 else 1.0) * np.sin(ang)
    return c_mla, s_mla, c_ax, s_ax, c_l1, s_l1


def _masks():
    m = np.zeros((128, 8, 512), np.float32)
    i = np.arange(128)[:, None]
    v = np.arange(256)[None, :]
    mg = ((v >= i) & (v <= i + 128)).astype(np.float32)
    m[:, 0, 0:256] = mg
    m[:, 1, 0:256] = mg * (i >= 64)
    m[:, 2, 0:256] = mg * (i < 64)
    return m


VCOLS = {}


def _pack_vecs(inp):
    cols = []

    def add(name, arr):
        VCOLS[name] = (sum(c.shape[1] for c in cols), arr.shape[1])
        cols.append(np.ascontiguousarray(arr, dtype=np.float32))

    def chunked(v):
        return v.reshape(-1, 128).T

    def pad128(v):
        o = np.zeros((128,), np.float32)
        o[: v.shape[0]] = v
        return o[:, None]

    add("g_mix0", chunked(inp["norm_even_mix"][0]))
    add("g_ffn0", chunked(inp["norm_even_ffn"][0]))
    add("g_mix1", chunked(inp["norm_odd_mix"][0]))
    add("g_ffn1", chunked(inp["norm_odd_ffn"][0]))
    add("qn", chunked(inp["mla_q_norm"][0]))
    add("kvn", chunked(inp["mla_kv_norm"][0]))
    gq = inp["mla_q_gain"][0]
    gk = inp["mla_k_gain"][0]
    p96 = np.concatenate([np.arange(64), 64 + PERM_MLA32])
    add("gqa96", pad128(gq))
    add("gqa96p", pad128(gq[p96]))
    add("gka96", pad128(gk))
    add("gka96p", pad128(gk[p96]))
    for nm, key, perm in (("gqb", "gqa_q_gain", PERM_AX64), ("gkb", "gqa_k_gain", PERM_AX64),
                          ("gq1", "dil_q_gain", PERM_L164), ("gk1", "dil_k_gain", PERM_L164)):
        g = inp[key][0]
        add(nm, np.tile(g, 2)[:, None])
        add(nm + "p", np.tile(g[perm], 2)[:, None])
    add("b0", chunked(inp["ada_even_b"][0]))
    add("b1", chunked(inp["ada_odd_b"][0]))
    return np.concatenate(cols, axis=1)


_CONST_CACHE = {}


def _consts():
    if not _CONST_CACHE:
        c_mla, s_mla, c_ax, s_ax, c_l1, s_l1 = _rope_tables()
        sel = np.zeros((8, 1024), np.float32)
        for e in range(8):
            sel[e, e * 128:(e + 1) * 128] = 1.0
        tri = np.triu(np.ones((128, 128), np.float32), k=1)
        cst = np.zeros((128, 64), np.float32)
        cst[:, 0] = np.arange(128)
        cst[:, 1:41] = 512.0 * np.arange(40)[None, :]
        _CONST_CACHE.update(dict(tri=tri, cst=cst))
        _CONST_CACHE.update(dict(c_mla=c_mla, s_mla=s_mla, c_ax=c_ax, s_ax=s_ax, c_l1=c_l1, s_l1=s_l1,
                                 masks=_masks(), ident=np.eye(128, dtype=np.float32), sel=sel))
    return _CONST_CACHE


def _shared_inputs(inp):
    f = lambda a: np.ascontiguousarray(a, dtype=np.float32)
    w_in = inp["even_w_in"][0]
    qb0, kb0 = 416, 928
    pq = np.concatenate([qb0 + 64 * h + PERM_AX64 for h in range(8)])
    pk = np.concatenate([kb0 + 64 * h + PERM_AX64 for h in range(2)])
    w_in_perm = w_in[:, np.concatenate([pq, pk])]
    w_kpe2 = np.concatenate([w_in[:, 320:416], w_in[:, 320:384], w_in[:, 384 + PERM_MLA32]], axis=1)
    w_uq = inp["mla_w_uq"][0]
    puq = np.concatenate([np.concatenate([96 * h + np.arange(64), 96 * h + 64 + PERM_MLA32]) for h in range(8)])
    w_qkv = inp["dil_w_qkv"][0]
    pqk = np.concatenate([64 * h + PERM_L164 for h in range(32)])
    sh = dict(
        vecs=_pack_vecs(inp),
        ada_w0=f(inp["ada_even_w"][0]), ada_w1=f(inp["ada_odd_w"][0]),
        w_in=f(w_in), w_in_perm=f(w_in_perm), w_kpe2=f(w_kpe2),
        w_uq=f(w_uq), w_uq_perm=f(w_uq[:, puq]), w_ukv=f(inp["mla_w_ukv"][0]),
        w_out0=f(inp["even_w_out"][0]),
        wg0=f(inp["ffn_w_gate"][0]), wu0=f(inp["ffn_w_up"][0]), wd0=f(inp["ffn_w_down"][0]),
        w_qkv=f(w_qkv), w_qk_perm=f(w_qkv[:, pqk]), w_out1=f(inp["dil_w_out"][0]),
        router=f(inp["moe_router"][0]),
        moe_wg=f(inp["moe_w_gate"][0][:NE_IN]), moe_wu=f(inp["moe_w_up"][0][:NE_IN]), moe_wd=f(inp["moe_w_down"][0][:NE_IN]),
    )
    sh.update(_consts())
    return sh


INPUT_SHAPES = dict(
    xT=[D, T], cT=[128, 8, NB], vecs=None,
    ada_w0=[D, 6 * D], ada_w1=[D, 6 * D],
    w_in=[D, 1184], w_in_perm=[D, 640], w_kpe2=[D, 192],
    w_uq=[256, 768], w_uq_perm=[256, 768], w_ukv=[128, 1024],
    w_out0=[D, D], wg0=[D, DFF], wu0=[D, DFF], wd0=[DFF, D],
    w_qkv=[D, 3 * D], w_qk_perm=[D, 2 * D], w_out1=[D, D],
    router=[D, NE], moe_wg=[NE_IN, D, DFE], moe_wu=[NE_IN, D, DFE], moe_wd=[NE_IN, DFE, D],
    c_mla=[96, S], s_mla=[96, S], c_ax=[128, S], s_ax=[128, S], c_l1=[128, S], s_l1=[128, S],
    masks=[128, 8, 512], ident=[128, 128], sel=[8, 1024], tri=[128, 128], cst=[128, 64],
)


class Prog:
    def __init__(self, dbg=(), nvec=None):
        self.nc = nc = bass.Bass("TRN2", target_bir_lowering=False)
        self.dbg = set(dbg)
        self.I = {}
        for name, shp in INPUT_SHAPES.items():
            if shp is None:
                shp = [128, nvec]
            self.I[name] = nc.dram_tensor(name, list(shp), F32, kind="ExternalInput").ap()
        self.out = nc.dram_tensor("outT", [D, T], F32, kind="ExternalOutput").ap()
        self.scr = {}

    def scratch(self, name, shape, dtype):
        kind = "ExternalOutput" if name in self.dbg else "Internal"
        ap = self.nc.dram_tensor(name, list(shape), dtype, kind=kind).ap()
        self.scr[name] = ap
        return ap

    def vcol(self, name, j=0, n=1):
        o, w = VCOLS[name]
        return self.vecs[:, o + j:o + j + n]

    def build(self, phases):
        nc = self.nc
        with ExitStack() as es:
            self.es = es
            self.S = Sc = Sched(nc, es)
            I_ = self.I
            sb = lambda n, s, d: es.enter_context(nc.sbuf_tensor(_un(n), s, d))
            nvec = self.I["vecs"].shape[1]
            self.vecs = sb("vecs", [128, nvec], F32)
            self.t_vecs = Tok("vecs")
            Sc.dma("sp", [(self.vecs[:], self.I["vecs"][:, :])], writes=[self.t_vecs])
            self.ones_bf = sb("ones_bf", [128, 128], BF16)
            self.bd_bf = sb("bd_bf", [128, 128], BF16)
            self.ones_f = sb("ones_f", [128, 64], F32)
            self.eps_t = sb("eps_t", [128, 1], F32)
            self.t_const = Tok("const")
            Sc.op("dve", lambda e: e.memset(self.ones_bf[:], 1.0), writes=[self.t_const])
            Sc.op("dve", lambda e: e.memset(self.bd_bf[:], 0.0), writes=[self.t_const])
            Sc.op("dve", lambda e: e.memset(self.bd_bf[0:64, 0:64], 1.0), writes=[self.t_const])
            Sc.op("dve", lambda e: e.memset(self.bd_bf[64:128, 64:128], 1.0), writes=[self.t_const])
            Sc.op("dve", lambda e: e.memset(self.ones_f[:], 1.0), writes=[self.t_const])
            Sc.op("dve", lambda e: e.memset(self.eps_t[:], EPS), writes=[self.t_const])
            self.M = [sb(f"M{l}", [128, 48, NB], F32) for l in range(2)]
            self.A = [sb(f"A{l}", [128, 2, 8, NB], F32) for l in range(2)]
            self.t_mod = Tok("mod")
            self.scratch("qaT", [768, T], BF16)
            self.scratch("kaT", [768, T], BF16)
            self.scratch("vA", [T, 512], BF16)
            self.scratch("qbT", [512, T], BF16)
            self.scratch("kbT", [128, T], BF16)
            self.scratch("vB", [T, 128], BF16)
            self.scratch("at0", [D, T], BF16)
            self.scratch("x2T", [D, T], F32)
            self.scratch("q1T", [D, T], BF16)
            self.scratch("k1T", [D, T], BF16)
            self.scratch("v1", [T, D], BF16)
            self.scratch("at1", [D, T], BF16)
            self.scratch("wg0b", [D, DFF], BF16)
            self.scratch("wu0b", [D, DFF], BF16)
            self.scratch("wd0b", [DFF, D], BF16)
            self.scratch("mwg", [NE, D, DFE], BF16)
            self.scratch("mwu", [NE, D, DFE], BF16)
            self.scratch("mwd", [NE, DFE, D], BF16)
            NBLK = 40
            self.scratch("x3T", [D, T], F32)
            self.scratch("h_tok", [T, D], BF16)
            self.scratch("Xs", [NBLK * 512, D], BF16)
            self.scratch("Ys", [NBLK * 512, D], F32)
            self.scratch("mwg2", [NE * 14 * 128, 2048], BF16)
            self.scratch("mwu2", [NE * 14 * 128, 2048], BF16)
            self.scratch("mwd2", [NE * 14 * 128, 2048], BF16)
            self.rt_m1 = sb("rt_m1", [128, 64, NE], F32)
            self.rt_m2 = sb("rt_m2", [128, 64, NE], F32)
            self.rt_g = sb("rt_g", [128, 64, 2], F32)
            self.rt_s = sb("rt_s", [128, 2, 64], mybir.dt.int32)
            self.rt_w = sb("rt_w", [128, NBLK, 14], mybir.dt.int32)
            self.t_rt = Tok("rt")
            self.cast_jobs = []
            for dn, sn, rows in (("wg0b", "wg0", D), ("wu0b", "wu0", D), ("wd0b", "wd0", DFF)):
                for r in range(0, rows, 128):
                    self.cast_jobs.append((self.scr[dn][r:r + 128, :], I_[sn][r:r + 128, :]))
            SORTED = not os.environ.get("K_DENSE")
            for e_ in range(NE_IN):
                if not SORTED:
                    for dn, sn, rows in (("mwg", "moe_wg", D), ("mwu", "moe_wu", D), ("mwd", "moe_wd", DFE)):
                        for r in range(0, rows, 128):
                            self.cast_jobs.append((self.scr[dn][e_, r:r + 128, :], I_[sn][e_, r:r + 128, :]))
                    continue
                for dn, sn in (("mwg2", "moe_wg"), ("mwu2", "moe_wu")):
                    dv = self.scr[dn].rearrange("(e fg p) (kc c) -> e kc p fg c", e=NE, fg=14, p=128, kc=8)
                    for kc in range(8):
                        self.cast_jobs.append((dv[e_, kc], I_[sn][e_, kc * 128:(kc + 1) * 128, :], (14, 256)))
                dv = self.scr["mwd2"].rearrange("(e h g p) (fl c) -> e g fl p h c", e=NE, h=2, g=7, p=128, fl=4)
                for fc in range(28):
                    self.cast_jobs.append((dv[e_, fc // 4, fc % 4], I_["moe_wd"][e_, fc * 128:(fc + 1) * 128, :], (2, 512)))
            for ph in phases:
                getattr(self, "phase_" + ph)()
                Sc.barrier()
            Sc.barrier()

    def rstd_from_ss(self, ss_ps, t_ss, np_, dim, tmp, t_tmp, out, t_out, n=TT):
        Sc = self.S
        Sc.op("act", lambda e: e.activation(out=tmp[0:np_, 0:n], in_=ss_ps[0:np_, 0:n], func=AF.Ln,
                                            bias=self.eps_t[0:np_, :], scale=1.0 / dim),
              reads=[t_ss, self.t_const], writes=[t_tmp])
        Sc.op("act", lambda e: e.activation(out=out[0:np_, 0:n], in_=tmp[0:np_, 0:n], func=AF.Exp, scale=-0.5),
              reads=[t_tmp], writes=[t_out])

    def norm_tile(self, R, xt, t_x, l, n, b, hT, t_h, h32=None, t_h32=None):
        Sc = self.S
        ss, t_ss = R["ss"].next()
        for c in range(8):
            sq, t_sq = R["sq"].next()
            Sc.op("act", lambda e: e.activation(out=sq[:], in_=xt[:, c, :], func=AF.Square), reads=[t_x[c]], writes=[t_sq])
            Sc.op("pe", lambda e: e.matmul(ss[:], lhsT=self.ones_bf[:], rhs=sq[:], start=(c == 0), stop=(c == 7)),
                  reads=[t_sq, self.t_const], writes=[t_ss])
        tmp, t_tmp = R["f32"].next()
        rs, t_rs = R["rstd"].next()
        self.rstd_from_ss(ss, t_ss, 128, float(D), tmp, t_tmp, rs, t_rs)
        for c in range(8):
            tm, t_tm = R["f32"].next()
            Sc.op("dve", lambda e: e.tensor_tensor(out=tm[:], in0=xt[:, c, :], in1=rs[:], op=ALU.mult),
                  reads=[t_x[c], t_rs], writes=[t_tm])
            if h32 is not None:
                Sc.op("act", lambda e: e.activation(out=h32[:, c, :], in_=tm[:], func=AF.Identity,
                                                    bias=self.M[l][:, (24 if n else 0) + c, b:b + 1],
                                                    scale=self.A[l][:, n, c, b:b + 1]),
                      reads=[t_tm, self.t_mod], writes=[t_h32[c]])
                Sc.op("pool", lambda e: e.tensor_copy(out=hT[:, c, :], in_=h32[:, c, :]), reads=[t_h32[c]], writes=[t_h[c]])
            else:
                Sc.op("act", lambda e: e.activation(out=hT[:, c, :], in_=tm[:], func=AF.Identity,
                                                    bias=self.M[l][:, (24 if n else 0) + c, b:b + 1],
                                                    scale=self.A[l][:, n, c, b:b + 1]),
                      reads=[t_tm, self.t_mod], writes=[t_h[c]])

    def headnorm_rope(self, R, ps, t_ps, psp, t_psp, np_, dim, ones_l, gname, ctab, stab, t_tab, out_ap, t_out):
        Sc = self.S
        SUB = int(os.environ.get("K_SUB", 99))
        if SUB <= 1:
            return
        sq, t_sq = R["sq"].next()
        Sc.op("act", lambda e: e.activation(out=sq[0:np_, :], in_=ps[0:np_, :], func=AF.Square), reads=[t_ps], writes=[t_sq])
        ss, t_ss = R["ss"].next()
        Sc.op("pe", lambda e: e.matmul(ss[0:np_, :], lhsT=ones_l, rhs=sq[0:np_, :], start=True, stop=True),
              reads=[t_sq, self.t_const], writes=[t_ss])
        tmp, t_tmp = R["f32"].next()
        rs, t_rs = R["rstd"].next()
        self.rstd_from_ss(ss, t_ss, np_, float(dim), tmp, t_tmp, rs, t_rs)
        if SUB <= 2:
            return
        t1, t_t1 = R["f32"].next()
        t2, t_t2 = R["f32"].next()
        Sc.op("dve", lambda e: e.scalar_tensor_tensor(out=t1[0:np_, :], in0=ps[0:np_, :], scalar=self.vcol(gname)[0:np_, :],
                                                      in1=ctab[0:np_, :], op0=ALU.mult, op1=ALU.mult),
              reads=[t_ps, t_tab, self.t_vecs], writes=[t_t1])
        Sc.op("dve", lambda e: e.scalar_tensor_tensor(out=t2[0:np_, :], in0=psp[0:np_, :], scalar=self.vcol(gname + "p")[0:np_, :],
                                                      in1=stab[0:np_, :], op0=ALU.mult, op1=ALU.mult),
              reads=[t_psp, t_tab, self.t_vecs], writes=[t_t2])
        if SUB <= 3:
            return
        Sc.op("pool", lambda e: e.tensor_tensor(out=t1[0:np_, :], in0=t1[0:np_, :], in1=t2[0:np_, :], op=ALU.add),
              reads=[t_t2], writes=[t_t1])
        Sc.op(os.environ.get("K_FIN", "pool"), lambda e: e.tensor_tensor(out=out_ap, in0=t1[0:np_, :], in1=rs[0:np_, :], op=ALU.mult),
              reads=[t_t1, t_rs], writes=[t_out])

    def mk_rings(self, es, nsq=4, nf32=6, nrstd=3, nss=2, nmm=int(os.environ.get("K_NMM", 6))):
        nc = self.nc
        R = {}
        R["sq"] = Ring([(es.enter_context(nc.sbuf_tensor(_un(f"sq{i}"), [128, TT], BF16)), Tok()) for i in range(nsq)])
        R["f32"] = Ring([(es.enter_context(nc.sbuf_tensor(_un(f"f32_{i}"), [128, TT], F32)), Tok()) for i in range(nf32)])
        R["rstd"] = Ring([(es.enter_context(nc.sbuf_tensor(_un(f"rstd{i}"), [128, TT], F32)), Tok()) for i in range(nrstd)])
        R["ss"] = Ring([(es.enter_context(nc.psum_tensor(_un(f"ssps{i}"), [128, TT], F32)), Tok(x=True)) for i in range(nss)])
        R["mm"] = Ring([(es.enter_context(nc.psum_tensor(_un(f"mmps{i}"), [128, TT], F32)), Tok(x=True)) for i in range(nmm)])
        return R

    def phase_p0(self):
        nc, Sc, I = self.nc, self.S, self.I
        with ExitStack() as es:
            sb = lambda n, s, d: es.enter_context(nc.sbuf_tensor(_un(n), s, d))
            cT_t = sb("cT_t", [128, 8, NB], F32)
            sc_t = sb("sc_t", [128, 8, NB], F32)
            t_c, t_sc = Tok(), Tok()
            Sc.dma("sp", [(cT_t[:], I["cT"][:, :, :])], writes=[t_c])
            Sc.op("act", lambda e: e.activation(out=sc_t[:], in_=cT_t[:], func=AF.Silu), reads=[t_c], writes=[t_sc])
            wr = Ring([(sb(f"adaw{i}", [128, 8, 768], F32), Tok()) for i in range(2)])
            mps = es.enter_context(nc.psum_tensor(_un("mps"), [128, 48, NB], F32))
            t_mps = Tok()
            for l in range(2):
                wsrc = I["ada_w%d" % l].rearrange("(kc p) n -> p kc n", p=128)
                for g in range(8):
                    wt, t_w = wr.next()
                    Sc.dma("sp", [(wt[:], wsrc[:, :, g * 768:(g + 1) * 768])], writes=[t_w])
                    for j in range(6):
                        ch = g * 6 + j

                        def f(e):
                            for kc in range(8):
                                last = e.matmul(mps[:, ch, :], lhsT=wt[:, kc, j * 128:(j + 1) * 128], rhs=sc_t[:, kc, :],
                                                start=(kc == 0), stop=(kc == 7))
                            return last
                        Sc.op("pe", f, reads=[t_w, t_sc], writes=[t_mps])
                bo = VCOLS["b%d" % l][0]
                for b in range(NB):
                    Sc.op("dve", lambda e: e.tensor_tensor(out=self.M[l][:, :, b], in0=mps[:, :, b],
                                                           in1=self.vecs[:, bo:bo + 48], op=ALU.add),
                          reads=[t_mps, self.t_vecs], writes=[self.t_mod])
                for n in range(2):
                    go = VCOLS[("g_mix%d" if n == 0 else "g_ffn%d") % l][0]
                    for b in range(NB):
                        Sc.op("dve", lambda e: e.scalar_tensor_tensor(
                            out=self.A[l][:, n, :, b], in0=self.M[l][:, (8 if n == 0 else 32):(16 if n == 0 else 40), b],
                            scalar=1.0, in1=self.vecs[:, go:go + 8], op0=ALU.add, op1=ALU.mult),
                            reads=[self.t_mod, self.t_vecs], writes=[self.t_mod])

    def phase_p1(self):
        nc, Sc, I = self.nc, self.S, self.I
        with ExitStack() as es:
            sb = lambda n, s, d: es.enter_context(nc.sbuf_tensor(_un(n), s, d))
            R = self.mk_rings(es)
            w_in = sb("w_in", [128, 8, 1184], BF16)
            w_inp = sb("w_inp", [128, 8, 640], BF16)
            w_kpe = sb("w_kpe", [128, 8, 192], BF16)
            w_uq = sb("w_uq", [128, 2, 768], BF16)
            w_uqp = sb("w_uqp", [128, 2, 768], BF16)
            w_ukv = sb("w_ukv", [128, 1024], BF16)
            w_ukvv = sb("w_ukvv", [128, 8, 64], BF16)
            t_w = Tok()
            self.mk_stage(es, n=2, w=1184, nb=0)
            rr = lambda a: a.rearrange("(kc p) n -> p kc n", p=128)
            for kc in range(8):
                self.load_cast(w_in[:, kc, :], rr(I["w_in"])[:, kc, :], t_w)
                self.load_cast(w_inp[:, kc, :], rr(I["w_in_perm"])[:, kc, :], t_w)
                self.load_cast(w_kpe[:, kc, :], rr(I["w_kpe2"])[:, kc, :], t_w)
            for kc in range(2):
                self.load_cast(w_uq[:, kc, :], rr(I["w_uq"])[:, kc, :], t_w)
                self.load_cast(w_uqp[:, kc, :], rr(I["w_uq_perm"])[:, kc, :], t_w)
            self.load_cast(w_ukv[:], I["w_ukv"][:, :], t_w)
            for h in range(8):
                self.load_cast(w_ukvv[:, h, :], I["w_ukv"][:, 128 * h + 64:128 * h + 128], t_w)
            xr = Ring([(sb(f"xt{i}", [128, 8, TT], F32), toks(8)) for i in range(2)])
            hr = Ring([(sb(f"hT{i}", [128, 8, TT], BF16), toks(8)) for i in range(2)])
            tabr = Ring([(sb(f"tab{i}", [128, 4, TT], F32), Tok()) for i in range(2)])
            cqn = sb("cqn", [128, 2, TT], BF16)
            t_cqn = toks(2)
            ckvn = sb("ckvn", [128, TT], BF16)
            t_ckvn = Tok()
            sqpe = sb("sqpe", [128, TT], BF16)
            t_sqpe = Tok()
            Rpe = sb("Rpe", [128, TT], F32)
            t_Rpe = Tok()
            qa_st = Ring([(sb(f"qa_st{i}", [96, 8, TT], BF16), Tok()) for i in range(2)])
            ka_st = Ring([(sb(f"ka_st{i}", [96, 8, TT], BF16), Tok()) for i in range(2)])
            qb_st = Ring([(sb(f"qb_st{i}", [128, 4, TT], BF16), Tok()) for i in range(2)])
            kb_st = Ring([(sb(f"kb_st{i}", [128, TT], BF16), Tok()) for i in range(2)])
            va_st = Ring([(sb(f"va_st{i}", [128, 4, 512], BF16), Tok()) for i in range(2)])
            vb_st = Ring([(sb(f"vb_st{i}", [128, 4, 128], BF16), Tok()) for i in range(2)])
            xsrc = I["xT"].rearrange("(c p) t -> p c t", p=128)
            qa_dst = self.scr["qaT"].rearrange("(h p) t -> p h t", p=96)
            ka_dst = self.scr["kaT"].rearrange("(h p) t -> p h t", p=96)
            qb_dst = self.scr["qbT"].rearrange("(c p) t -> p c t", p=128)
            va_dst = self.scr["vA"].rearrange("(u p) f -> p u f", p=128)
            vb_dst = self.scr["vB"].rearrange("(u p) f -> p u f", p=128)
            t_scr = Tok()

            LIM = int(os.environ.get('K_LIM', 99))
            NT_RUN = int(os.environ.get('K_NT', NT))
            loaded = []

            def p1_loads(j):
                s0_, t0_ = (j % (S // TT)) * TT, j * TT
                xt_, t_x_ = xr.next()
                for c in range(8):
                    Sc.dma("sp", [(xt_[:, c, :], xsrc[:, c, t0_:t0_ + TT])], writes=[t_x_[c]])
                tab_, t_tab_ = tabr.next()
                Sc.dma("sp", [(tab_[0:96, 0, :], I["c_mla"][:, s0_:s0_ + TT]), (tab_[0:96, 1, :], I["s_mla"][:, s0_:s0_ + TT]),
                              (tab_[:, 2, :], I["c_ax"][:, s0_:s0_ + TT]), (tab_[:, 3, :], I["s_ax"][:, s0_:s0_ + TT])],
                       writes=[t_tab_])
                loaded.append((xt_, t_x_, tab_, t_tab_))

            normed = []

            def p1_norm(j):
                xt_, t_x_, tab_, t_tab_ = loaded.pop(0)
                hT_, t_h_ = hr.next()
                self.norm_tile(R, xt_, t_x_, 0, 0, j // (S // TT), hT_, t_h_)
                normed.append((hT_, t_h_, tab_, t_tab_))

            for it in range(NT_RUN):
                b, s0, t0 = it // (S // TT), (it % (S // TT)) * TT, it * TT
                if it == 0:
                    p1_loads(0)
                    p1_norm(0)
                if it + 1 < NT_RUN:
                    p1_loads(it + 1)
                    p1_norm(it + 1)
                hT, t_h, tab, t_tab = normed.pop(0)

                def proj(w, c0, m, po=0):
                    ps, t_ps = R["mm"].next()

                    def f(e):
                        for kc in range(8):
                            last = e.matmul(ps[po:po + m, :], lhsT=w[:, kc, c0:c0 + m], rhs=hT[:, kc, :],
                                            start=(kc == 0), stop=(kc == 7))
                        return last
                    Sc.op("pe", f, reads=t_h + [t_w], writes=[t_ps])
                    return ps, t_ps

                if LIM <= 1:
                    continue
                cq = [proj(w_in, 128 * c, 128) for c in range(2)]
                ss, t_ss = R["ss"].next()
                for c in range(2):
                    sq, t_sq = R["sq"].next()
                    Sc.op("act", lambda e: e.activation(out=sq[:], in_=cq[c][0][:], func=AF.Square), reads=[cq[c][1]], writes=[t_sq])
                    Sc.op("pe", lambda e: e.matmul(ss[:], lhsT=self.ones_bf[:], rhs=sq[:], start=(c == 0), stop=(c == 1)),
                          reads=[t_sq, self.t_const], writes=[t_ss])
                tmp, t_tmp = R["f32"].next()
                rs, t_rs = R["rstd"].next()
                self.rstd_from_ss(ss, t_ss, 128, 256.0, tmp, t_tmp, rs, t_rs)
                for c in range(2):
                    Sc.op("dve", lambda e: e.scalar_tensor_tensor(out=cqn[:, c, :], in0=cq[c][0][:], scalar=self.vcol("qn", c),
                                                                  in1=rs[:], op0=ALU.mult, op1=ALU.mult),
                          reads=[cq[c][1], t_rs, self.t_vecs], writes=[t_cqn[c]])
                ckv, t_ckv = proj(w_in, 256, 128)
                sq, t_sq = R["sq"].next()
                Sc.op("act", lambda e: e.activation(out=sq[:], in_=ckv[:], func=AF.Square), reads=[t_ckv], writes=[t_sq])
                ss, t_ss = R["ss"].next()
                Sc.op("pe", lambda e: e.matmul(ss[:], lhsT=self.ones_bf[:], rhs=sq[:], start=True, stop=True),
                      reads=[t_sq, self.t_const], writes=[t_ss])
                tmp, t_tmp = R["f32"].next()
                rs, t_rs = R["rstd"].next()
                self.rstd_from_ss(ss, t_ss, 128, 128.0, tmp, t_tmp, rs, t_rs)
                Sc.op("dve", lambda e: e.scalar_tensor_tensor(out=ckvn[:], in0=ckv[:], scalar=self.vcol("kvn"),
                                                              in1=rs[:], op0=ALU.mult, op1=ALU.mult),
                      reads=[t_ckv, t_rs, self.t_vecs], writes=[t_ckvn])
                if LIM <= 2:
                    continue
                kpe, t_kpe = proj(w_kpe, 0, 96)
                kpp, t_kpp = proj(w_kpe, 96, 96)
                Sc.op("act", lambda e: e.activation(out=sqpe[64:96, :], in_=kpe[64:96, :], func=AF.Square), reads=[t_kpe], writes=[t_sqpe])
                t1, t_t1 = R["f32"].next()
                Sc.op("dve", lambda e: e.scalar_tensor_tensor(out=Rpe[64:96, :], in0=kpe[64:96, :], scalar=self.vcol("gka96")[64:96, :],
                                                              in1=tab[64:96, 0, :], op0=ALU.mult, op1=ALU.mult),
                      reads=[t_kpe, t_tab, self.t_vecs], writes=[t_Rpe])
                Sc.op("dve", lambda e: e.scalar_tensor_tensor(out=t1[64:96, :], in0=kpp[64:96, :], scalar=self.vcol("gka96p")[64:96, :],
                                                              in1=tab[64:96, 1, :], op0=ALU.mult, op1=ALU.mult),
                      reads=[t_kpp, t_tab, self.t_vecs], writes=[t_t1])
                Sc.op("pool", lambda e: e.tensor_tensor(out=Rpe[64:96, :], in0=Rpe[64:96, :], in1=t1[64:96, :], op=ALU.add),
                      reads=[t_t1], writes=[t_Rpe])
                if LIM <= 3:
                    continue
                qbs, t_qbs = qb_st.next()
                for c in range(4):
                    ps, t_ps = proj(w_in, 416 + 128 * c, 128)
                    pp, t_pp = proj(w_inp, 128 * c, 128)
                    self.headnorm_rope(R, ps, t_ps, pp, t_pp, 128, 64, self.bd_bf[:], "gqb", tab[:, 2, :], tab[:, 3, :], t_tab,
                                       qbs[:, c, :], t_qbs)
                Sc.dma("sp", [(qb_dst[:, :, t0:t0 + TT], qbs[:])], reads=[t_qbs], writes=[Tok()])
                kbs, t_kbs = kb_st.next()
                ps, t_ps = proj(w_in, 928, 128)
                pp, t_pp = proj(w_inp, 512, 128)
                self.headnorm_rope(R, ps, t_ps, pp, t_pp, 128, 64, self.bd_bf[:], "gkb", tab[:, 2, :], tab[:, 3, :], t_tab,
                                   kbs[:], t_kbs)
                Sc.dma("sp", [(self.scr["kbT"][:, t0:t0 + TT], kbs[:])], reads=[t_kbs], writes=[Tok()])
                if LIM <= 4:
                    continue
                vbs, t_vbs = vb_st.next()
                ps, t_ps = R["mm"].next()
                for u in range(4):
                    def f(e):
                        for kc in range(8):
                            last = e.matmul(ps[:, u * 128:(u + 1) * 128], lhsT=hT[:, kc, u * 128:(u + 1) * 128],
                                            rhs=w_in[:, kc, 1056:1184], start=(kc == 0), stop=(kc == 7))
                        return last
                    Sc.op("pe", f, reads=t_h + [t_w], writes=[t_ps])
                Sc.op("dve", lambda e: e.tensor_copy(out=vbs[:].rearrange("p u f -> p (u f)"), in_=ps[:]), reads=[t_ps], writes=[t_vbs])
                Sc.dma("sp", [(vb_dst[:, 4 * it:4 * it + 4, :], vbs[:])], reads=[t_vbs], writes=[Tok()])
                if LIM <= 5:
                    continue
                qas, t_qas = qa_st.next()
                for h in range(8):
                    ps, t_ps = R["mm"].next()
                    pp, t_pp = R["mm"].next()
                    for (pt, tp, w) in ((ps, t_ps, w_uq), (pp, t_pp, w_uqp)):
                        def f(e):
                            for kc in range(2):
                                last = e.matmul(pt[0:96, :], lhsT=w[:, kc, 96 * h:96 * h + 96], rhs=cqn[:, kc, :],
                                                start=(kc == 0), stop=(kc == 1))
                            return last
                        Sc.op("pe", f, reads=t_cqn + [t_w], writes=[tp])
                    self.headnorm_rope(R, ps, t_ps, pp, t_pp, 96, 96, self.ones_bf[0:96, 0:96], "gqa96",
                                       tab[:, 0, :], tab[:, 1, :], t_tab, qas[0:96, h, :], t_qas)
                Sc.dma("sp", [(qa_dst[:, :, t0:t0 + TT], qas[:])], reads=[t_qas], writes=[Tok()])
                if LIM <= 6:
                    continue
                kas, t_kas = ka_st.next()
                for h in range(8):
                    ps, t_ps = R["mm"].next()
                    Sc.op("pe", lambda e: e.matmul(ps[0:64, :], lhsT=w_ukv[:, 128 * h:128 * h + 64], rhs=ckvn[:], start=True, stop=True),
                          reads=[t_ckvn, t_w], writes=[t_ps])
                    sq, t_sq = R["sq"].next()
                    Sc.op("act", lambda e: e.activation(out=sq[0:64, :], in_=ps[0:64, :], func=AF.Square), reads=[t_ps], writes=[t_sq])
                    ss, t_ss = R["ss"].next()

                    def f(e):
                        e.matmul(ss[0:96, :], lhsT=self.ones_bf[0:64, 0:96], rhs=sq[0:64, :], start=True, stop=False)
                        return e.matmul(ss[0:96, :], lhsT=self.ones_bf[64:96, 0:96], rhs=sqpe[64:96, :], start=False, stop=True)
                    Sc.op("pe", f, reads=[t_sq, t_sqpe, self.t_const], writes=[t_ss])
                    tmp, t_tmp = R["f32"].next()
                    rs, t_rs = R["rstd"].next()
                    self.rstd_from_ss(ss, t_ss, 96, 96.0, tmp, t_tmp, rs, t_rs)
                    Sc.op("dve", lambda e: e.scalar_tensor_tensor(out=kas[0:64, h, :], in0=ps[0:64, :], scalar=self.vcol("gka96")[0:64, :],
                                                                  in1=rs[0:64, :], op0=ALU.mult, op1=ALU.mult),
                          reads=[t_ps, t_rs, self.t_vecs], writes=[t_kas])
                    Sc.op("pool", lambda e: e.tensor_tensor(out=kas[64:96, h, :], in0=Rpe[64:96, :], in1=rs[64:96, :], op=ALU.mult),
                          reads=[t_Rpe, t_rs], writes=[t_kas])
                Sc.dma("sp", [(ka_dst[:, :, t0:t0 + TT], kas[:])], reads=[t_kas], writes=[Tok()])
                if LIM <= 7:
                    continue
                vas, t_vas = va_st.next()
                for u in range(4):
                    ps, t_ps = R["mm"].next()
                    Sc.op("pe", lambda e: e.matmul(ps[:], lhsT=ckvn[:, u * 128:(u + 1) * 128], rhs=w_ukvv[:].rearrange("k h x -> k (h x)"),
                                                   start=True, stop=True), reads=[t_ckvn, t_w], writes=[t_ps])
                    Sc.op("dve", lambda e: e.tensor_copy(out=vas[:, u, :], in_=ps[:]), reads=[t_ps], writes=[t_vas])
                Sc.dma("sp", [(va_dst[:, 4 * it:4 * it + 4, :], vas[:])], reads=[t_vas], writes=[Tok()])


    def attn_epilogue(self, o, t_o, RR, dst_ap):
        Sc = self.S
        rc, t_rc = RR["rec"].next()
        Sc.op("dve", lambda e: e.reciprocal(out=rc[64:65, :], in_=o[64:65, :]), reads=[t_o], writes=[t_rc])
        bc, t_bc = RR["bc"].next()
        Sc.op("pe", lambda e: e.matmul(bc[0:64, :], lhsT=self.ones_f[64:65, 0:64], rhs=rc[64:65, :], start=True, stop=True),
              reads=[t_rc, self.t_const], writes=[t_bc])
        ob, t_ob = RR["osb"].next()
        Sc.op("dve", lambda e: e.tensor_copy(out=ob[0:64, :], in_=o[0:64, :]), reads=[t_o], writes=[t_ob])
        ot, t_ot = RR["outb"].next()
        Sc.op("dve", lambda e: e.tensor_tensor(out=ot[0:64, :], in0=ob[0:64, :], in1=bc[0:64, :], op=ALU.mult),
              reads=[t_ob, t_bc], writes=[t_ot])
        Sc.dma("sp", [(dst_ap, ot[0:64, :])], reads=[t_ot], writes=[Tok()])

    def mk_attn_rings(self, es, nS=4, nO=2):
        nc = self.nc
        sb = lambda n, s, d: es.enter_context(nc.sbuf_tensor(_un(n), s, d))
        ps = lambda n: es.enter_context(nc.psum_tensor(_un(n), [128, TT], F32))
        RR = {}
        RR["s"] = Ring([(ps(f"sps{i}"), Tok(x=True)) for i in range(nS)])
        RR["o"] = Ring([(ps(f"ops{i}"), Tok(x=True)) for i in range(nO)])
        RR["bc"] = Ring([(ps("bcps"), Tok(x=True))])
        RR["pT"] = Ring([(sb(f"pT{i}", [128, TT], BF16), Tok()) for i in range(nS + 1)])
        RR["rec"] = Ring([(sb(f"rec{i}", [128, TT], F32), Tok()) for i in range(2)])
        RR["osb"] = Ring([(sb(f"osb{i}", [128, TT], F32), Tok()) for i in range(2)])
        RR["outb"] = Ring([(sb(f"outb{i}", [128, TT], BF16), Tok()) for i in range(2)])
        return RR

    def phase_p2(self):
        nc, Sc, I = self.nc, self.S, self.I
        with ExitStack() as es:
            sb = lambda n, s, d: es.enter_context(nc.sbuf_tensor(_un(n), s, d))
            self.mk_stage(es, n=2, w=3584)
            RR = self.mk_attn_rings(es, nS=5, nO=2)
            qr = Ring([(sb(f"qT{i}", [96, S], BF16), Tok()) for i in range(2)])
            kr = Ring([(sb(f"kT{i}", [96, S], BF16), Tok()) for i in range(2)])
            qrB = Ring([(sb(f"qTb{i}", [128, S], BF16), Tok()) for i in range(2)])
            krB = Ring([(sb(f"kTb{i}", [128, S], BF16), Tok()) for i in range(2)])
            for (bf_, t_bf) in qrB.items + krB.items:
                Sc.op("pool", lambda e: e.memset(bf_[:], 0.0), writes=[t_bf])
            vr = Ring([(sb(f"vt{i}", [128, 32, 65], BF16), Tok()) for i in range(2)])
            for vt, t_v in vr.items:
                Sc.op("dve", lambda e: e.memset(vt[:, :, 64:65], 1.0), writes=[t_v])
            vA = self.scr["vA"].rearrange("(n p) f -> p n f", p=128)
            vB = self.scr["vB"].rearrange("(n p) f -> p n f", p=128)
            items = []
            for b in range(NB):
                tb = b * S
                for h in range(8):
                    items.append(dict(b=b, kv=("a", b, h), dq=96, dk=96, sc=96.0 ** -0.5, orow=64 * h,
                                      k=self.scr["kaT"][96 * h:96 * h + 96, tb:tb + S],
                                      q=self.scr["qaT"][96 * h:96 * h + 96, tb:tb + S],
                                      v=vA[:, 32 * b:32 * b + 32, 64 * h:64 * h + 64]))
                for h in range(8):
                    g = h // 4
                    items.append(dict(b=b, kv=("b", b, g), dq=64, dk=128, sc=64.0 ** -0.5, orow=512 + 64 * h,
                                      k=self.scr["kbT"][64 * g:64 * g + 64, tb:tb + S],
                                      q=self.scr["qbT"][64 * h:64 * h + 64, tb:tb + S],
                                      v=vB[:, 32 * b:32 * b + 32, 64 * g:64 * g + 64]))
            NIT = int(os.environ.get("K_NI", len(items)))
            items = items[:NIT]
            cur = {"kv": None}

            def loads(i):
                it = items[i]
                dq = it["dq"]
                if it["kv"] != cur["kv"]:
                    cur["kv"] = it["kv"]
                    kT, t_k = (krB if it["dk"] == 128 else kr).next()
                    vt, t_v = vr.next()
                    Sc.dma("sp", [(kT[0:dq, :], it["k"])], writes=[t_k])
                    Sc.dma("sp", [(vt[:, :, 0:64], it["v"])], writes=[t_v])
                    cur["k"] = (kT, t_k, vt, t_v)
                qT, t_q = (qrB if it["dk"] == 128 else qr).next()
                Sc.dma("sp", [(qT[0:dq, :], it["q"])], writes=[t_q])
                it["bufs"] = cur["k"] + (qT, t_q)

            loads(0)
            deferred = []
            for i, it in enumerate(items):
                if i + 1 < len(items):
                    loads(i + 1)
                kT, t_k, vt, t_v, qT, t_q = it["bufs"]
                dq, scl, tb = it["dk"], it["sc"], it["b"] * S
                for qt in range(S // TT):
                    self.pump_casts(2)
                    o, t_o = RR["o"].next()
                    pend = []

                    def pv(kt, p, t_p):
                        Sc.op("pe", lambda e: e.matmul(o[0:65, :], lhsT=vt[:, kt, :], rhs=p[:], start=(kt == 0), stop=(kt == 31)),
                              reads=[t_p, t_v], writes=[t_o])
                    for kt in range(32):
                        if kt == 14 and deferred:
                            deferred.pop(0)()
                        s_, t_s = RR["s"].next()
                        Sc.op("pe", lambda e: e.matmul(s_[:], lhsT=kT[0:dq, kt * 128:(kt + 1) * 128], rhs=qT[0:dq, qt * TT:(qt + 1) * TT],
                                                       start=True, stop=True), reads=[t_k, t_q], writes=[t_s])
                        p, t_p = RR["pT"].next()
                        Sc.op("act", lambda e: e.activation(out=p[:], in_=s_[:], func=AF.Exp, scale=scl), reads=[t_s], writes=[t_p])
                        pend.append((kt, p, t_p))
                        if len(pend) > 3:
                            pv(*pend.pop(0))
                    while pend:
                        pv(*pend.pop(0))
                    deferred.append(lambda o=o, t_o=t_o, dst=self.scr["at0"][it["orow"]:it["orow"] + 64, tb + qt * TT:tb + (qt + 1) * TT]:
                                    self.attn_epilogue(o, t_o, RR, dst))
            while deferred:
                deferred.pop(0)()
            while self.cast_jobs or getattr(self, "pump_inflight", None):
                self.pump_casts(1)

    def outproj_residual(self, R, w_out, t_w, at, t_at, xt, t_x, l, b):
        Sc = self.S
        for dc in range(8):
            ps, t_ps = R["mm"].next()

            def f(e):
                for kc in range(8):
                    last = e.matmul(ps[:], lhsT=w_out[:, kc, dc * 128:(dc + 1) * 128], rhs=at[:, kc, :], start=(kc == 0), stop=(kc == 7))
                return last
            Sc.op("pe", f, reads=t_at + [t_w], writes=[t_ps])
            Sc.op("dve", lambda e: e.scalar_tensor_tensor(out=xt[:, dc, :], in0=ps[:], scalar=self.M[l][:, 16 + dc, b:b + 1],
                                                          in1=xt[:, dc, :], op0=ALU.mult, op1=ALU.add),
                  reads=[t_ps, self.t_mod], writes=[t_x[dc]])

    def phase_p3(self):
        nc, Sc, I = self.nc, self.S, self.I
        NFC = DFF // 128
        with ExitStack() as es:
            sb = lambda n, s, d: es.enter_context(nc.sbuf_tensor(_un(n), s, d))
            R = self.mk_rings(es, nss=1, nmm=3)
            R["y"] = Ring([(es.enter_context(nc.psum_tensor(_un(f"yps{i}"), [128, TT], F32)), Tok(x=True)) for i in range(4)])
            self.mk_stage(es, n=2, w=1024, nb=0)
            w_out = sb("w_out", [128, 8, D], BF16)
            t_w = Tok()
            rr = lambda a: a.rearrange("(kc p) n -> p kc n", p=128)
            for kc in range(8):
                self.load_cast(w_out[:, kc, :], rr(I["w_out0"])[:, kc, :], t_w)
            xr = Ring([(sb(f"xt{i}", [128, 8, TT], F32), toks(8)) for i in range(2)])
            ar = Ring([(sb(f"at{i}", [128, 8, TT], BF16), toks(8)) for i in range(2)])
            hT = sb("hT", [128, 8, TT], BF16)
            t_h = toks(8)
            H = sb("H", [128, NFC, TT], BF16)
            t_H = toks(NFC)
            wgr = Ring([(sb(f"wg{i}", [128, 8, 256], BF16), Tok()) for i in range(3)])
            wur = Ring([(sb(f"wu{i}", [128, 8, 256], BF16), Tok()) for i in range(3)])
            wdr = Ring([(sb(f"wd{i}", [128, 2, 512], BF16), Tok()) for i in range(3)])
            xsrc = I["xT"].rearrange("(c p) t -> p c t", p=128)
            asrc = self.scr["at0"].rearrange("(c p) t -> p c t", p=128)
            xdst = self.scr["x2T"].rearrange("(c p) t -> p c t", p=128)
            wg_src = rr(self.scr["wg0b"])
            wu_src = rr(self.scr["wu0b"])
            wd_src = self.scr["wd0b"].rearrange("(fc p) d -> p fc d", p=128)
            NT_RUN = int(os.environ.get('K_NT', NT))
            loaded = []

            def loads(j):
                t0_ = j * TT
                xt_, t_x_ = xr.next()
                at_, t_at_ = ar.next()
                for c in range(8):
                    Sc.dma("sp", [(xt_[:, c, :], xsrc[:, c, t0_:t0_ + TT])], writes=[t_x_[c]])
                    Sc.dma("sp", [(at_[:, c, :], asrc[:, c, t0_:t0_ + TT])], writes=[t_at_[c]])
                loaded.append((xt_, t_x_, at_, t_at_))

            loads(0)
            for it in range(NT_RUN):
                b, t0 = it // (S // TT), it * TT
                if it + 1 < NT_RUN:
                    loads(it + 1)
                xt, t_x, at, t_at = loaded.pop(0)
                self.outproj_residual(R, w_out, t_w, at, t_at, xt, t_x, 0, b)
                self.norm_tile(R, xt, t_x, 0, 1, b, hT, t_h)
                for fg in range(NFC // 2):
                    wg, t_wg = wgr.next()
                    wu, t_wu = wur.next()
                    Sc.dma("sp", [(wg[:], wg_src[:, :, fg * 256:(fg + 1) * 256])], writes=[t_wg])
                    Sc.dma("sp", [(wu[:], wu_src[:, :, fg * 256:(fg + 1) * 256])], writes=[t_wu])
                    for fl in range(2):
                        f_ = fg * 2 + fl
                        gps, t_g = R["mm"].next()
                        ups, t_u = R["mm"].next()
                        for (pt, tp, w, tw) in ((gps, t_g, wg, t_wg), (ups, t_u, wu, t_wu)):
                            def f(e):
                                for kc in range(8):
                                    last = e.matmul(pt[:], lhsT=w[:, kc, fl * 128:(fl + 1) * 128], rhs=hT[:, kc, :],
                                                    start=(kc == 0), stop=(kc == 7))
                                return last
                            Sc.op("pe", f, reads=t_h + [tw], writes=[tp])
                        sg, t_sg = R["f32"].next()
                        Sc.op("act", lambda e: e.activation(out=sg[:], in_=gps[:], func=AF.Silu), reads=[t_g], writes=[t_sg])
                        Sc.op("dve", lambda e: e.tensor_tensor(out=H[:, f_, :], in0=sg[:], in1=ups[:], op=ALU.mult),
                              reads=[t_sg, t_u], writes=[t_H[f_]])
                for half in range(2):
                    ys = [R["y"].next() for _ in range(4)]
                    for fg in range(NFC // 2):
                        wd, t_wd = wdr.next()
                        Sc.dma("sp", [(wd[:], wd_src[:, 2 * fg:2 * fg + 2, half * 512:(half + 1) * 512])], writes=[t_wd])
                        for fl in range(2):
                            f_ = fg * 2 + fl

                            def f(e):
                                for j in range(4):
                                    last = e.matmul(ys[j][0][:], lhsT=wd[:, fl, j * 128:(j + 1) * 128], rhs=H[:, f_, :],
                                                    start=(f_ == 0), stop=(f_ == NFC - 1))
                                return last
                            Sc.op("pe", f, reads=[t_H[f_], t_wd], writes=[y[1] for y in ys])
                    for j in range(4):
                        dc = half * 4 + j
                        Sc.op("dve", lambda e: e.scalar_tensor_tensor(out=xt[:, dc, :], in0=ys[j][0][:], scalar=self.M[0][:, 40 + dc, b:b + 1],
                                                                      in1=xt[:, dc, :], op0=ALU.mult, op1=ALU.add),
                              reads=[ys[j][1], self.t_mod], writes=[t_x[dc]])
                for c in range(8):
                    Sc.dma("sp", [(xdst[:, c, t0:t0 + TT], xt[:, c, :])], reads=[t_x[c]], writes=[Tok()])

    def phase_p4(self):
        nc, Sc, I = self.nc, self.S, self.I
        with ExitStack() as es:
            sb = lambda n, s, d: es.enter_context(nc.sbuf_tensor(_un(n), s, d))
            R = self.mk_rings(es)
            self.mk_stage(es, n=1, w=3072, nb=0)
            w_qkv = sb("w_qkv", [128, 8, 3 * D], BF16)
            w_qkp = sb("w_qkp", [128, 8, 2 * D], BF16)
            t_w = Tok()
            rr = lambda a: a.rearrange("(kc p) n -> p kc n", p=128)
            for kc in range(8):
                self.load_cast(w_qkv[:, kc, :], rr(I["w_qkv"])[:, kc, :], t_w)
                self.load_cast(w_qkp[:, kc, :], rr(I["w_qk_perm"])[:, kc, :], t_w)
            xt = sb("xt", [128, 8, TT], F32)
            t_x = toks(8)
            hr = Ring([(sb(f"hT{i}", [128, 8, TT], BF16), toks(8)) for i in range(2)])
            tabr = Ring([(sb(f"tab{i}", [128, 2, TT], F32), Tok()) for i in range(2)])
            q_st, t_qst = sb("q_st", [128, 8, TT], BF16), Tok()
            k_st, t_kst = sb("k_st", [128, 8, TT], BF16), Tok()
            v_st, t_vst = sb("v_st", [128, 4, D], BF16), Tok()
            xsrc = self.scr["x2T"].rearrange("(c p) t -> p c t", p=128)
            q_dst = self.scr["q1T"].rearrange("(c p) t -> p c t", p=128)
            k_dst = self.scr["k1T"].rearrange("(c p) t -> p c t", p=128)
            v_dst = self.scr["v1"].rearrange("(u p) f -> p u f", p=128)
            NT_RUN = int(os.environ.get('K_NT', NT))
            normed = []

            def p4_front(j):
                s0_, t0_ = (j % (S // TT)) * TT, j * TT
                for c in range(8):
                    Sc.dma("sp", [(xt[:, c, :], xsrc[:, c, t0_:t0_ + TT])], writes=[t_x[c]])
                tab_, t_tab_ = tabr.next()
                Sc.dma("sp", [(tab_[:, 0, :], I["c_l1"][:, s0_:s0_ + TT]), (tab_[:, 1, :], I["s_l1"][:, s0_:s0_ + TT])], writes=[t_tab_])
                hT_, t_h_ = hr.next()
                self.norm_tile(R, xt, t_x, 1, 0, j // (S // TT), hT_, t_h_)
                normed.append((hT_, t_h_, tab_, t_tab_))

            for it in range(NT_RUN):
                b, s0, t0 = it // (S // TT), (it % (S // TT)) * TT, it * TT
                if it == 0:
                    p4_front(0)
                if it + 1 < NT_RUN:
                    p4_front(it + 1)
                hT, t_h, tab, t_tab = normed.pop(0)

                def proj(w, c0):
                    ps, t_ps = R["mm"].next()

                    def f(e):
                        for kc in range(8):
                            last = e.matmul(ps[:], lhsT=w[:, kc, c0:c0 + 128], rhs=hT[:, kc, :], start=(kc == 0), stop=(kc == 7))
                        return last
                    Sc.op("pe", f, reads=t_h + [t_w], writes=[t_ps])
                    return ps, t_ps
                for (st, t_st, off, gname) in ((q_st, t_qst, 0, "gq1"), (k_st, t_kst, D, "gk1")):
                    for c in range(8):
                        ps, t_ps = proj(w_qkv, off + 128 * c)
                        pp, t_pp = proj(w_qkp, off + 128 * c)
                        self.headnorm_rope(R, ps, t_ps, pp, t_pp, 128, 64, self.bd_bf[:], gname, tab[:, 0, :], tab[:, 1, :], t_tab,
                                           st[:, c, :], t_st)
                Sc.dma("sp", [(q_dst[:, :, t0:t0 + TT], q_st[:])], reads=[t_qst], writes=[Tok()])
                Sc.dma("sp", [(k_dst[:, :, t0:t0 + TT], k_st[:])], reads=[t_kst], writes=[Tok()])
                for u in range(4):
                    for half in range(2):
                        ps, t_ps = R["mm"].next()

                        def f(e):
                            for kc in range(8):
                                last = e.matmul(ps[:], lhsT=hT[:, kc, u * 128:(u + 1) * 128],
                                                rhs=w_qkv[:, kc, 2 * D + half * 512:2 * D + (half + 1) * 512], start=(kc == 0), stop=(kc == 7))
                            return last
                        Sc.op("pe", f, reads=t_h + [t_w], writes=[t_ps])
                        Sc.op("dve", lambda e: e.tensor_copy(out=v_st[:, u, half * 512:(half + 1) * 512], in_=ps[:]), reads=[t_ps], writes=[t_vst])
                Sc.dma("sp", [(v_dst[:, 4 * it:4 * it + 4, :], v_st[:])], reads=[t_vst], writes=[Tok()])

    def phase_p5(self):
        nc, Sc, I = self.nc, self.S, self.I
        with ExitStack() as es:
            sb = lambda n, s, d: es.enter_context(nc.sbuf_tensor(_un(n), s, d))
            RR = self.mk_attn_rings(es, nS=4, nO=3)
            self.mk_stage(es, n=1, w=2048, nb=0)
            masks = sb("masks", [128, 8, TT], BF16)
            t_mask = Tok()
            for i in range(2):
                self.load_cast(masks[:, 4 * i:4 * i + 4, :].rearrange("p a b -> p (a b)"),
                               I["masks"][:, 4 * i:4 * i + 4, :].rearrange("p a b -> p (a b)"), t_mask)
            er = Ring([(sb(f"e{i}", [128, TT], BF16), Tok()) for i in range(4)])
            q1r = Ring([(sb(f"q1_{i}", [128, S], BF16), Tok()) for i in range(2)])
            k1r = Ring([(sb(f"k1_{i}", [128, S + 128], BF16), Tok()) for i in range(2)])
            q4, t_q4 = sb("q4", [128, 4, 1024], BF16), Tok()
            k4, t_k4 = sb("k4", [128, 4, 1152], BF16), Tok()
            q16, t_q16 = sb("q16", [128, 16, 256], BF16), Tok()
            k16, t_k16 = sb("k16", [128, 16, 384], BF16), Tok()
            zf, t_zf = sb("zf", [128, TT], F32), Tok()
            vr = Ring([((sb(f"v1t{i}", [128, 33, 65], BF16), sb(f"v4t{i}", [128, 4, 9, 65], BF16), sb(f"v16t{i}", [128, 16, 3, 65], BF16)), Tok())
                       for i in range(2)])
            Ur = Ring([(sb(f"U{i}", [128, S], F32), Tok()) for i in range(2)])
            for (bf_, t_bf) in q1r.items + k1r.items + [(q4, t_q4), (k4, t_k4), (q16, t_q16), (k16, t_k16)]:
                fl_ = bf_[:] if len(bf_.shape) == 2 else bf_[:].rearrange("p a b -> p (a b)")
                Sc.op("pool", lambda e: e.memset(fl_, 0.0), writes=[t_bf])
            Sc.op("dve", lambda e: e.memset(zf[:], 0.0), writes=[t_zf])
            for ((a_, b_, c_), t_v) in vr.items:
                for tl in (a_, b_, c_):
                    fl = tl[:].rearrange("p a b -> p (a b)") if len(tl.shape) == 3 else tl[:].rearrange("p a b c -> p (a b c)")
                    Sc.op("dve", lambda e: e.memset(fl, 0.0), writes=[t_v])
                Sc.op("dve", lambda e: e.memset(a_[:, :, 64:65], 1.0), writes=[t_v])
                Sc.op("dve", lambda e: e.memset(b_[:, :, :, 64:65], 1.0), writes=[t_v])
                Sc.op("dve", lambda e: e.memset(c_[:, :, :, 64:65], 1.0), writes=[t_v])
            SCL = 0.125
            NPAIR = int(os.environ.get("K_NP", NB * 8))
            pairs = [(b, hp) for b in range(NB) for hp in range(8)][:NPAIR]
            heads = [(b, hp, hh) for (b, hp) in pairs for hh in range(2)]
            pl, hl = [], []

            def load_pair(i):
                b, hp, hh = heads[i]
                h = 2 * hp + hh
                q1, t_q1 = q1r.next()
                k1, t_k1 = k1r.next()
                Sc.dma("sp", [(q1[0:64, :], self.scr["q1T"][64 * h:64 * h + 64, b * S:(b + 1) * S])], writes=[t_q1])
                Sc.dma("sp", [(k1[0:64, 64:64 + S], self.scr["k1T"][64 * h:64 * h + 64, b * S:(b + 1) * S])], writes=[t_k1])
                pl.append((q1, t_q1, k1, t_k1))

            def load_v(i):
                b, hp, hh = heads[i]
                h = 2 * hp + hh
                (v1t, v4t, v16t), t_v = vr.next()
                vs = self.scr["v1"][b * S:(b + 1) * S, 64 * h:64 * h + 64]
                prs = [(v1t[:, 1:32, 0:64], vs[64:64 + 31 * 128, :].rearrange("(j p) x -> p j x", p=128)),
                       (v1t[64:128, 0, 0:64], vs[0:64, :]), (v1t[0:64, 32, 0:64], vs[S - 64:S, :])]
                v4s = vs.rearrange("(l r) x -> r l x", r=4)
                for r in range(4):
                    prs.append((v4t[:, r, 1:8, 0:64], v4s[r, 64:64 + 7 * 128, :].rearrange("(j p) x -> p j x", p=128)))
                    prs.append((v4t[64:128, r, 0, 0:64], v4s[r, 0:64, :]))
                    prs.append((v4t[0:64, r, 8, 0:64], v4s[r, 960:1024, :]))
                v16s = vs.rearrange("(l r) x -> l r x", r=16)
                prs.append((v16t[64:128, :, 0, 0:64], v16s[0:64, :, :]))
                prs.append((v16t[:, :, 1, 0:64], v16s[64:192, :, :]))
                prs.append((v16t[0:64, :, 2, 0:64], v16s[192:256, :, :]))
                Sc.dma("sp", prs, writes=[t_v])
                hl.append((v1t, v4t, v16t, t_v))

            mk_eng = [0]

            gp = []

            def pv_emit():
                (jr, n, p, t_p, q0, w, v_ap, t_v, o, t_o, fin) = gp.pop(0)
                Sc.op("pe", lambda e: e.matmul(o[0:65, q0:q0 + w], lhsT=v_ap(jr), rhs=p[:, 0:w], start=False, stop=True, skip_group_check=True),
                      reads=[t_p, t_v], writes=[t_o])
                if jr == n - 1:
                    fin()

            def band(hs, k_ap, t_k, q_fn, t_q, nq, v_ap, t_v, first, last, o, t_o, fin):
                n = nq // 128 + 1
                Sc.op("act", lambda e: e.activation(out=o[0:65, 0:nq], in_=zf[0:65, 0:nq], func=AF.Copy), reads=[t_zf], writes=[t_o])
                for jr in range(n):
                    q0 = max(0, 128 * (jr - 1))
                    q1 = min(nq, 128 * (jr + 1))
                    w = q1 - q0
                    m0 = 128 if jr == 0 else 0
                    pat = 1 if (first and jr == 0) else 2 if (last and jr == n - 1) else 0
                    s_, t_s = RR["s"].next()
                    Sc.op("pe", lambda e: e.matmul(s_[:, 0:w], lhsT=k_ap(jr), rhs=q_fn(q0, q1), start=True, stop=True), reads=[t_k, t_q], writes=[t_s])
                    ee, t_e = er.next()
                    Sc.op("act", lambda e: e.activation(out=ee[:, 0:w], in_=s_[:, 0:w], func=AF.Exp, scale=SCL), reads=[t_s], writes=[t_e])
                    p, t_p = RR["pT"].next()
                    mk_eng[0] += 1
                    Sc.op("pool" if mk_eng[0] % 2 else "dve", lambda e: e.tensor_tensor(out=p[:, 0:w], in0=ee[:, 0:w], in1=masks[:, pat, m0:m0 + w], op=ALU.mult),
                          reads=[t_e, t_mask], writes=[t_p])
                    gp.append((jr, n, p, t_p, q0, w, v_ap, t_v, o, t_o, fin))
                    if len(gp) > 3:
                        pv_emit()

            load_pair(0)
            load_v(0)
            hi = 0
            for (b, hp) in pairs:
                for hh in range(2):
                    if hi + 1 < len(heads):
                        load_pair(hi + 1)
                        load_v(hi + 1)
                    hi += 1
                    q1, t_q1, k1, t_k1 = pl.pop(0)
                    Sc.op("act", lambda e: e.activation(out=q4[0:64], in_=q1[0:64, :].rearrange("p (l r) -> p r l", r=4), func=AF.Copy), reads=[t_q1], writes=[t_q4])
                    Sc.op("act", lambda e: e.activation(out=k4[0:64, :, 64:64 + 1024], in_=k1[0:64, 64:64 + S].rearrange("p (l r) -> p r l", r=4), func=AF.Copy),
                          reads=[t_k1], writes=[t_k4])
                    Sc.op("act", lambda e: e.activation(out=q16[0:64], in_=q1[0:64, :].rearrange("p (l r) -> p r l", r=16), func=AF.Copy), reads=[t_q1], writes=[t_q16])
                    Sc.op("act", lambda e: e.activation(out=k16[0:64, :, 64:64 + 256], in_=k1[0:64, 64:64 + S].rearrange("p (l r) -> p r l", r=16), func=AF.Copy),
                          reads=[t_k1], writes=[t_k16])
                    v1t, v4t, v16t, t_v = hl.pop(0)
                    hs = slice(0, 128)
                    U, t_U = Ur.next()
                    for m in range(8):
                        o, t_o = RR["o"].next()
                        band(hs, lambda jr: k1[hs, 128 * (4 * m + jr):128 * (4 * m + jr) + 128], t_k1,
                             lambda a_, b_: q1[hs, m * TT + a_:m * TT + b_], t_q1, TT,
                             lambda jr, m=m, v1t=v1t: v1t[:, 4 * m + jr, :], t_v, m == 0, m == 7, o, t_o,
                             lambda o=o, t_o=t_o, U=U, t_U=t_U, m=m: Sc.op("act", lambda e: e.activation(out=U[0:65, m * TT:(m + 1) * TT], in_=o[0:65, :], func=AF.Copy),
                                                                         reads=[t_o], writes=[t_U]))
                    U4 = U[0:65, :].rearrange("p (l r) -> p r l", r=4)
                    for r in range(4):
                        for m in range(2):
                            o, t_o = RR["o"].next()
                            band(hs, lambda jr: k4[hs, r, 128 * (4 * m + jr):128 * (4 * m + jr) + 128], t_k4,
                                 lambda a_, b_: q4[hs, r, m * TT + a_:m * TT + b_], t_q4, TT,
                                 lambda jr, m=m, r=r, v4t=v4t: v4t[:, r, 4 * m + jr, :], t_v, m == 0, m == 1, o, t_o,
                                 lambda o=o, t_o=t_o, U4=U4, t_U=t_U, m=m, r=r: Sc.op("dve", lambda e: e.tensor_tensor(
                                     out=U4[:, r, m * TT:(m + 1) * TT], in0=U4[:, r, m * TT:(m + 1) * TT], in1=o[0:65, :], op=ALU.add), reads=[t_o], writes=[t_U]))
                    U16 = U[0:65, :].rearrange("p (l r) -> p r l", r=16)
                    for r in range(16):
                        o, t_o = RR["o"].next()
                        band(hs, lambda jr: k16[hs, r, 128 * jr:128 * jr + 128], t_k16, lambda a_, b_: q16[hs, r, a_:b_], t_q16, 256,
                             lambda jr, r=r, v16t=v16t: v16t[:, r, jr, :], t_v, True, True, o, t_o,
                             lambda o=o, t_o=t_o, U16=U16, t_U=t_U, r=r: Sc.op("dve", lambda e: e.tensor_tensor(
                                 out=U16[:, r, :], in0=U16[:, r, :], in1=o[0:65, 0:256], op=ALU.add), reads=[t_o], writes=[t_U]))
                    while gp:
                        pv_emit()
                    h = 2 * hp + hh
                    for nt in range(S // TT):
                        cs = slice(nt * TT, (nt + 1) * TT)
                        rc, t_rc = RR["rec"].next()
                        Sc.op("dve", lambda e: e.reciprocal(out=rc[64:65, :], in_=U[64:65, cs]), reads=[t_U], writes=[t_rc])
                        bc, t_bc = RR["bc"].next()
                        Sc.op("pe", lambda e: e.matmul(bc[0:64, :], lhsT=self.ones_f[64:65, 0:64], rhs=rc[64:65, :], start=True, stop=True),
                              reads=[t_rc, self.t_const], writes=[t_bc])
                        ot, t_ot = RR["outb"].next()
                        Sc.op("dve", lambda e: e.tensor_tensor(out=ot[0:64, :], in0=U[0:64, cs], in1=bc[0:64, :], op=ALU.mult),
                              reads=[t_U, t_bc], writes=[t_ot])
                        Sc.dma("sp", [(self.scr["at1"][64 * h:64 * h + 64, b * S + nt * TT:b * S + (nt + 1) * TT], ot[0:64, :])],
                               reads=[t_ot], writes=[Tok()])

    def phase_p6(self):
        nc, Sc, I = self.nc, self.S, self.I
        NFC = DFE // 128
        with ExitStack() as es:
            sb = lambda n, s, d: es.enter_context(nc.sbuf_tensor(_un(n), s, d))
            R = self.mk_rings(es, nsq=2, nf32=4, nrstd=2, nss=1, nmm=3)
            R["y"] = Ring([(es.enter_context(nc.psum_tensor(_un(f"yps{i}"), [128, TT], F32)), Tok(x=True)) for i in range(4)])
            self.mk_stage(es, n=2, w=1024, nb=0)
            w_out = sb("w_out", [128, 8, D], BF16)
            t_w = Tok()
            rr = lambda a: a.rearrange("(kc p) n -> p kc n", p=128)
            for kc in range(8):
                self.load_cast(w_out[:, kc, :], rr(I["w_out1"])[:, kc, :], t_w)
            router = sb("router", [128, 8, NE], F32)
            ident = sb("ident", [128, 128], F32)
            sel = sb("sel", [8, 1024], F32)
            t_c = Tok()
            Sc.dma("sp", [(router[:], rr(I["router"])), (ident[:], I["ident"][:, :]), (sel[:], I["sel"][:, :])], writes=[t_c])
            xt, t_x = sb("xt", [128, 8, TT], F32), toks(8)
            at, t_at = sb("at", [128, 8, TT], BF16), toks(8)
            hT, t_h = sb("hT", [128, 8, TT], BF16), toks(8)
            h32, t_h32 = sb("h32", [128, 8, TT], F32), toks(8)
            H, t_H = sb("H", [128, NFC, TT], BF16), toks(NFC)
            gb, t_gb = sb("gb", [128, NE, TT], F32), toks(NE)
            lg, t_lg = sb("lg", [128, 4, NE], F32), Tok()
            mx, t_mx = sb("mx", [128, 4, 8], F32), Tok()
            ex, t_ex = sb("ex", [128, 4, NE], F32), Tok()
            mk, t_mk = sb("mk", [128, 4, NE], F32), Tok()
            sm, t_sm = sb("sm", [128, 8], F32), Tok()
            gate, t_gate = sb("gate", [128, 4, NE], F32), Tok()
            gT, t_gT = sb("gT", [8, TT], F32), Tok()
            wgr = Ring([(sb(f"wg{i}", [128, 8, 256], BF16), Tok()) for i in range(3)])
            wur = Ring([(sb(f"wu{i}", [128, 8, 256], BF16), Tok()) for i in range(3)])
            wdr = Ring([(sb(f"wd{i}", [128, 4, 512], BF16), Tok()) for i in range(3)])
            xsrc = self.scr["x2T"].rearrange("(c p) t -> p c t", p=128)
            asrc = self.scr["at1"].rearrange("(c p) t -> p c t", p=128)
            odst = self.out.rearrange("(c p) t -> p c t", p=128)
            NT_RUN = int(os.environ.get('K_NT6', NT))
            for it in range(NT_RUN):
                b, t0 = it // (S // TT), it * TT
                for c in range(8):
                    Sc.dma("sp", [(xt[:, c, :], xsrc[:, c, t0:t0 + TT])], writes=[t_x[c]])
                    Sc.dma("sp", [(at[:, c, :], asrc[:, c, t0:t0 + TT])], writes=[t_at[c]])
                self.outproj_residual(R, w_out, t_w, at, t_at, xt, t_x, 1, b)
                self.norm_tile(R, xt, t_x, 1, 1, b, hT, t_h, h32=h32, t_h32=t_h32)
                lps, t_lps = R["mm"].next()
                for u in range(4):
                    def f(e):
                        for kc in range(8):
                            last = e.matmul(lps[:, u * NE:(u + 1) * NE], lhsT=h32[:, kc, u * 128:(u + 1) * 128], rhs=router[:, kc, :],
                                            start=(kc == 0), stop=(kc == 7))
                        return last
                    Sc.op("pe", f, reads=t_h32 + [t_c], writes=[t_lps])
                Sc.op("dve", lambda e: e.tensor_copy(out=lg[:].rearrange("p u e -> p (u e)"), in_=lps[:, 0:4 * NE]), reads=[t_lps], writes=[t_lg])
                for u in range(4):
                    Sc.op("dve", lambda e: e.max(out=mx[:, u, :], in_=lg[:, u, :]), reads=[t_lg], writes=[t_mx])
                Sc.op("dve", lambda e: e.tensor_scalar(out=sm[:, 0:4], in0=mx[:, :, 0], scalar1=-1.0, scalar2=None, op0=ALU.mult),
                      reads=[t_mx], writes=[t_sm])
                for u in range(4):
                    Sc.op("act", lambda e: e.activation(out=ex[:, u, :], in_=lg[:, u, :], func=AF.Exp, bias=sm[:, u:u + 1], scale=1.0),
                          reads=[t_lg, t_sm], writes=[t_ex])
                    Sc.op("dve", lambda e: e.tensor_scalar(out=mk[:, u, :], in0=lg[:, u, :], scalar1=mx[:, u, 1:2], scalar2=None, op0=ALU.is_ge),
                          reads=[t_lg, t_mx], writes=[t_mk])
                Sc.op("dve", lambda e: e.tensor_tensor(out=mk[:].rearrange("p u e -> p (u e)"), in0=mk[:].rearrange("p u e -> p (u e)"),
                                                       in1=ex[:].rearrange("p u e -> p (u e)"), op=ALU.mult), reads=[t_ex], writes=[t_mk])
                Sc.op("dve", lambda e: e.tensor_reduce(out=sm[:, 4:8], in_=mk[:], axis=AX.X, op=ALU.add), reads=[t_mk], writes=[t_sm])
                Sc.op("dve", lambda e: e.reciprocal(out=sm[:, 4:8], in_=sm[:, 4:8]), reads=[], writes=[t_sm])
                for u in range(4):
                    Sc.op("dve", lambda e: e.tensor_scalar(out=gate[:, u, :], in0=mk[:, u, :], scalar1=sm[:, 4 + u:5 + u], scalar2=None, op0=ALU.mult),
                          reads=[t_mk, t_sm], writes=[t_gate])
                gps, t_gps = R["mm"].next()
                for u in range(4):
                    Sc.op("pe", lambda e: e.matmul(gps[0:NE, u * 128:(u + 1) * 128], lhsT=gate[:, u, :], rhs=ident[:], start=True, stop=True),
                          reads=[t_gate, t_c], writes=[t_gps])
                Sc.op("dve", lambda e: e.tensor_copy(out=gT[:], in_=gps[0:NE, :]), reads=[t_gps], writes=[t_gT])
                for e_ in range(NE):
                    bps, t_bps = R["mm"].next()
                    Sc.op("pe", lambda e: e.matmul(bps[:], lhsT=sel[:, e_ * 128:(e_ + 1) * 128], rhs=gT[:], start=True, stop=True),
                          reads=[t_gT, t_c], writes=[t_bps])
                    Sc.op("act", lambda e: e.activation(out=gb[:, e_, :], in_=bps[:], func=AF.Copy), reads=[t_bps], writes=[t_gb[e_]])
                for e_ in range(NE):
                    wg_src = rr(self.scr["mwg"][e_])
                    wu_src = rr(self.scr["mwu"][e_])
                    wd_src = self.scr["mwd"][e_].rearrange("(fc p) d -> p fc d", p=128)
                    for fg in range(NFC // 2):
                        wg, t_wg = wgr.next()
                        wu, t_wu = wur.next()
                        Sc.dma("sp", [(wg[:], wg_src[:, :, fg * 256:(fg + 1) * 256])], writes=[t_wg])
                        Sc.dma("sp", [(wu[:], wu_src[:, :, fg * 256:(fg + 1) * 256])], writes=[t_wu])
                        for fl in range(2):
                            f_ = fg * 2 + fl
                            gp_, t_g = R["mm"].next()
                            up_, t_u = R["mm"].next()
                            for (pt, tp, w, tw) in ((gp_, t_g, wg, t_wg), (up_, t_u, wu, t_wu)):
                                def f(e):
                                    for kc in range(8):
                                        last = e.matmul(pt[:], lhsT=w[:, kc, fl * 128:(fl + 1) * 128], rhs=hT[:, kc, :],
                                                        start=(kc == 0), stop=(kc == 7))
                                    return last
                                Sc.op("pe", f, reads=t_h + [tw], writes=[tp])
                            sg, t_sg = R["f32"].next()
                            Sc.op("act", lambda e: e.activation(out=sg[:], in_=gp_[:], func=AF.Silu), reads=[t_g], writes=[t_sg])
                            Sc.op("dve", lambda e: e.tensor_tensor(out=H[:, f_, :], in0=sg[:], in1=up_[:], op=ALU.mult),
                                  reads=[t_sg, t_u], writes=[t_H[f_]])
                    for half in range(2):
                        ys = [R["y"].next() for _ in range(4)]
                        for fg in range(NFC // 4):
                            wd, t_wd = wdr.next()
                            Sc.dma("sp", [(wd[:], wd_src[:, 4 * fg:4 * fg + 4, half * 512:(half + 1) * 512])], writes=[t_wd])
                            for fl in range(4):
                                f_ = fg * 4 + fl

                                def f(e):
                                    for j in range(4):
                                        last = e.matmul(ys[j][0][:], lhsT=wd[:, fl, j * 128:(j + 1) * 128], rhs=H[:, f_, :],
                                                        start=(f_ == 0), stop=(f_ == NFC - 1))
                                    return last
                                Sc.op("pe", f, reads=[t_H[f_], t_wd], writes=[y[1] for y in ys])
                        for j in range(4):
                            dc = half * 4 + j
                            if e_ == 0:
                                Sc.op("dve", lambda e: e.tensor_tensor(out=h32[:, dc, :], in0=ys[j][0][:], in1=gb[:, e_, :], op=ALU.mult),
                                      reads=[ys[j][1], t_gb[e_]], writes=[t_h32[dc]])
                            else:
                                tm, t_tm = R["f32"].next()
                                Sc.op("dve", lambda e: e.tensor_tensor(out=tm[:], in0=ys[j][0][:], in1=gb[:, e_, :], op=ALU.mult),
                                      reads=[ys[j][1], t_gb[e_]], writes=[t_tm])
                                Sc.op("pool", lambda e: e.tensor_tensor(out=h32[:, dc, :], in0=h32[:, dc, :], in1=tm[:], op=ALU.add),
                                      reads=[t_tm], writes=[t_h32[dc]])
                for dc in range(8):
                    Sc.op("dve", lambda e: e.scalar_tensor_tensor(out=xt[:, dc, :], in0=h32[:, dc, :], scalar=self.M[1][:, 40 + dc, b:b + 1],
                                                                  in1=xt[:, dc, :], op0=ALU.mult, op1=ALU.add),
                          reads=[t_h32[dc], self.t_mod], writes=[t_x[dc]])
                    Sc.dma("sp", [(odst[:, dc, t0:t0 + TT], xt[:, dc, :])], reads=[t_x[dc]], writes=[Tok()])

    def phase_p6a(self):
        nc, Sc, I = self.nc, self.S, self.I
        with ExitStack() as es:
            sb = lambda n, s, d: es.enter_context(nc.sbuf_tensor(_un(n), s, d))
            R = self.mk_rings(es, nsq=2, nf32=4, nrstd=2, nss=1, nmm=5)
            self.mk_stage(es, n=2, w=1024, nb=0)
            w_out = sb("w_out", [128, 8, D], BF16)
            t_w = Tok()
            rr = lambda a: a.rearrange("(kc p) n -> p kc n", p=128)
            for kc in range(8):
                self.load_cast(w_out[:, kc, :], rr(I["w_out1"])[:, kc, :], t_w)
            router = sb("router", [128, 8, NE], F32)
            ident = sb("ident", [128, 128], F32)
            identb = sb("identb", [128, 128], BF16)
            t_c = Tok()
            Sc.dma("sp", [(router[:], rr(I["router"])), (ident[:], I["ident"][:, :])], writes=[t_c])
            Sc.op("dve", lambda e: e.tensor_copy(out=identb[:], in_=ident[:]), reads=[t_c], writes=[t_c])
            xt, t_x = sb("xt", [128, 8, TT], F32), toks(8)
            at, t_at = sb("at", [128, 8, TT], BF16), toks(8)
            hT, t_h = sb("hT", [128, 8, TT], BF16), toks(8)
            h32, t_h32 = sb("h32", [128, 8, TT], F32), toks(8)
            htr = Ring([(sb(f"htok{i}", [128, 4, D], BF16), Tok()) for i in range(2)])
            lg, t_lg = sb("lg", [128, 4, NE], F32), Tok()
            mx, t_mx = sb("mx", [128, 4, 8], F32), Tok()
            sm, t_sm = sb("sm", [128, 12], F32), Tok()
            zt, t_z = sb("zt", [128, 4, D], BF16), Tok()
            Sc.op("dve", lambda e: e.memset(zt[:].rearrange("p a b -> p (a b)"), 0.0), writes=[t_z])
            Xz = self.scr["Xs"].rearrange("(n p) f -> p n f", p=128)
            for b_ in range(40):
                Sc.dma("sp", [(Xz[:, 4 * b_:4 * b_ + 4, :], zt[:])], reads=[t_z], writes=[Tok()])
            xsrc = self.scr["x2T"].rearrange("(c p) t -> p c t", p=128)
            asrc = self.scr["at1"].rearrange("(c p) t -> p c t", p=128)
            x3dst = self.scr["x3T"].rearrange("(c p) t -> p c t", p=128)
            hdst = self.scr["h_tok"].rearrange("(n p) f -> p n f", p=128)
            for it in range(NT):
                b, t0 = it // (S // TT), it * TT
                for c in range(8):
                    Sc.dma("sp", [(xt[:, c, :], xsrc[:, c, t0:t0 + TT])], writes=[t_x[c]])
                    Sc.dma("sp", [(at[:, c, :], asrc[:, c, t0:t0 + TT])], writes=[t_at[c]])
                self.outproj_residual(R, w_out, t_w, at, t_at, xt, t_x, 1, b)
                for c in range(8):
                    Sc.dma("sp", [(x3dst[:, c, t0:t0 + TT], xt[:, c, :])], reads=[t_x[c]], writes=[Tok()])
                self.norm_tile(R, xt, t_x, 1, 1, b, hT, t_h, h32=h32, t_h32=t_h32)
                lps, t_lps = R["mm"].next()
                for u in range(4):
                    def f(e):
                        for kc in range(8):
                            last = e.matmul(lps[:, u * NE:(u + 1) * NE], lhsT=h32[:, kc, u * 128:(u + 1) * 128], rhs=router[:, kc, :],
                                            start=(kc == 0), stop=(kc == 7))
                        return last
                    Sc.op("pe", f, reads=t_h32 + [t_c], writes=[t_lps])
                Sc.op("dve", lambda e: e.tensor_copy(out=lg[:].rearrange("p u e -> p (u e)"), in_=lps[:, 0:4 * NE]), reads=[t_lps], writes=[t_lg])
                for u in range(4):
                    Sc.op("dve", lambda e: e.max(out=mx[:, u, :], in_=lg[:, u, :]), reads=[t_lg], writes=[t_mx])
                for u in range(4):
                    tt = it * 4 + u
                    Sc.op("dve", lambda e: e.tensor_scalar(out=self.rt_m1[:, tt, :], in0=lg[:, u, :], scalar1=mx[:, u, 0:1], scalar2=None, op0=ALU.is_equal),
                          reads=[t_lg, t_mx], writes=[self.t_rt])
                    Sc.op("dve", lambda e: e.tensor_scalar(out=self.rt_m2[:, tt, :], in0=lg[:, u, :], scalar1=mx[:, u, 1:2], scalar2=None, op0=ALU.is_equal),
                          reads=[t_lg, t_mx], writes=[self.t_rt])
                Sc.op("dve", lambda e: e.tensor_tensor(out=sm[:, 0:4], in0=mx[:, :, 1], in1=mx[:, :, 0], op=ALU.subtract), reads=[t_mx], writes=[t_sm])
                Sc.op("act", lambda e: e.activation(out=sm[:, 4:8], in_=sm[:, 0:4], func=AF.Exp), reads=[t_sm], writes=[t_sm])
                Sc.op("dve", lambda e: e.tensor_scalar(out=sm[:, 8:12], in0=sm[:, 4:8], scalar1=1.0, scalar2=None, op0=ALU.add), reads=[t_sm], writes=[t_sm])
                Sc.op("dve", lambda e: e.reciprocal(out=self.rt_g[:, it * 4:it * 4 + 4, 0], in_=sm[:, 8:12]), reads=[t_sm], writes=[self.t_rt])
                Sc.op("dve", lambda e: e.tensor_tensor(out=self.rt_g[:, it * 4:it * 4 + 4, 1], in0=sm[:, 4:8], in1=self.rt_g[:, it * 4:it * 4 + 4, 0], op=ALU.mult),
                      reads=[t_sm], writes=[self.t_rt])
                htok, t_ht = htr.next()
                k_ = 0
                for u in range(4):
                    for half in range(2):
                        ps, t_ps = R["mm"].next()

                        def f(e):
                            for k4 in range(4):
                                last = e.matmul(ps[:, k4 * 128:(k4 + 1) * 128], lhsT=hT[:, half * 4 + k4, u * 128:(u + 1) * 128], rhs=identb[:],
                                                start=True, stop=True)
                            return last
                        Sc.op("pe", f, reads=t_h + [t_c], writes=[t_ps])
                        eng = "act" if k_ % 2 else "dve"
                        k_ += 1
                        if eng == "act":
                            Sc.op("act", lambda e: e.activation(out=htok[:, u, half * 512:(half + 1) * 512], in_=ps[:], func=AF.Copy), reads=[t_ps], writes=[t_ht])
                        else:
                            Sc.op("dve", lambda e: e.tensor_copy(out=htok[:, u, half * 512:(half + 1) * 512], in_=ps[:]), reads=[t_ps], writes=[t_ht])
                Sc.dma("sp", [(hdst[:, it * 4:it * 4 + 4, :], htok[:])], reads=[t_ht], writes=[Tok()])

    def phase_p6b(self):
        nc, Sc, I = self.nc, self.S, self.I
        I32 = mybir.dt.int32
        with ExitStack() as es:
            sb = lambda n, s, d: es.enter_context(nc.sbuf_tensor(_un(n), s, d))
            pre_ps = es.enter_context(nc.psum_tensor(_un("pre_ps"), [128, TT], F32))
            t_ps = Tok(x=True)
            tri = sb("tri", [128, 128], F32)
            onesF = sb("onesF", [128, 128], F32)
            cst = sb("cst", [128, 64], F32)
            t_c = Tok()
            Sc.dma("sp", [(tri[:], I["tri"][:, :]), (cst[:], I["cst"][:, :])], writes=[t_c])
            Sc.op("dve", lambda e: e.memset(onesF[:], 1.0), writes=[t_c])
            t_a = Tok()
            m = sb("m", [128, 64, NE], F32)
            cA = sb("cA", [128, 64, NE], F32)
            cB = sb("cB", [128, 64, NE], F32)
            slot = sb("slot", [128, 64, NE], F32)
            sv = sb("sv", [128, 64], F32)
            sf = sb("sf", [128, 2, 64], F32)
            eb = sb("eb", [128, 40], F32)
            wf = sb("wf", [128, 40, 14], F32)
            colsum, pre, cnt, nblk, pend, base = (sv[:, 0:8], sv[:, 8:16], sv[:, 16:24], sv[:, 24:32], sv[:, 32:40], sv[:, 40:48])
            fl = lambda t: t[:].rearrange("p t e -> p (t e)")
            D_ = lambda fn, **kw: Sc.op("dve", fn, reads=[t_a, t_c, self.t_rt], writes=[t_a])
            D_(lambda e: e.tensor_tensor(out=fl(m), in0=fl(self.rt_m1), in1=fl(self.rt_m2), op=ALU.add))
            D_(lambda e: e.tensor_reduce(out=colsum, in_=m[:].rearrange("p t e -> p e t"), axis=AX.X, op=ALU.add))
            Sc.op("pe", lambda e: e.matmul(pre_ps[:, 0:8], lhsT=tri[:], rhs=colsum, start=True, stop=True), reads=[t_a, t_c], writes=[t_ps])
            Sc.op("pe", lambda e: e.matmul(pre_ps[:, 8:16], lhsT=onesF[:], rhs=colsum, start=True, stop=True), reads=[t_a, t_c], writes=[t_ps])
            Sc.op("dve", lambda e: e.tensor_copy(out=sv[:, 8:24], in_=pre_ps[:, 0:16]), reads=[t_ps, t_a], writes=[t_a])
            D_(lambda e: e.tensor_scalar(out=nblk, in0=cnt, scalar1=0.0, scalar2=None, op0=ALU.is_gt))
            for k in range(1, 17):
                D_(lambda e: e.scalar_tensor_tensor(out=nblk, in0=cnt, scalar=512.0 * k, in1=nblk, op0=ALU.is_gt, op1=ALU.add))
            D_(lambda e: e.tensor_scalar(out=sv[:, 32:33], in0=sv[:, 24:25], scalar1=512.0, scalar2=None, op0=ALU.mult))
            for e_ in range(1, NE):
                D_(lambda e: e.scalar_tensor_tensor(out=sv[:, 32 + e_:33 + e_], in0=sv[:, 24 + e_:25 + e_], scalar=512.0, in1=sv[:, 31 + e_:32 + e_],
                                                    op0=ALU.mult, op1=ALU.add))
            D_(lambda e: e.scalar_tensor_tensor(out=base, in0=nblk, scalar=-512.0, in1=pend, op0=ALU.mult, op1=ALU.add))
            D_(lambda e: e.tensor_tensor(out=base, in0=base, in1=pre, op=ALU.add))
            D_(lambda e: e.tensor_copy(out=fl(cA), in_=fl(m)))
            src_, dst_ = cA, cB
            for sft in (1, 2, 4, 8, 16, 32):
                D_(lambda e: e.tensor_tensor(out=dst_[:, sft:, :], in0=src_[:, sft:, :], in1=src_[:, 0:64 - sft, :], op=ALU.add))
                D_(lambda e: e.tensor_copy(out=dst_[:, 0:sft, :], in_=src_[:, 0:sft, :]))
                src_, dst_ = dst_, src_
            D_(lambda e: e.tensor_tensor(out=fl(src_), in0=fl(src_), in1=fl(m), op=ALU.subtract))
            for e_ in range(NE):
                D_(lambda e: e.tensor_scalar(out=slot[:, :, e_], in0=src_[:, :, e_], scalar1=sv[:, 40 + e_:41 + e_], scalar2=None, op0=ALU.add))
            for k, mm_ in ((0, self.rt_m1), (1, self.rt_m2)):
                D_(lambda e: e.tensor_tensor(out=fl(dst_), in0=fl(mm_), in1=fl(slot), op=ALU.mult))
                D_(lambda e: e.tensor_reduce(out=sf[:, k, :], in_=dst_[:], axis=AX.X, op=ALU.add))
            Sc.op("dve", lambda e: e.tensor_copy(out=self.rt_s[:].rearrange("p a b -> p (a b)"), in_=sf[:].rearrange("p a b -> p (a b)")),
                  reads=[t_a], writes=[self.t_rt])
            D_(lambda e: e.tensor_scalar(out=eb[:], in0=cst[:, 1:41], scalar1=sv[:, 32:33], scalar2=None, op0=ALU.is_ge))
            for e_ in range(1, NE):
                D_(lambda e: e.scalar_tensor_tensor(out=eb[:], in0=cst[:, 1:41], scalar=sv[:, 32 + e_:33 + e_], in1=eb[:], op0=ALU.is_ge, op1=ALU.add))
            D_(lambda e: e.tensor_scalar(out=eb[:], in0=eb[:], scalar1=float(NE - 1), scalar2=None, op0=ALU.min))
            D_(lambda e: e.tensor_scalar(out=eb[:], in0=eb[:], scalar1=1792.0, scalar2=cst[:, 0:1], op0=ALU.mult, op1=ALU.add))
            for j in range(14):
                D_(lambda e: e.tensor_scalar(out=wf[:, :, j], in0=eb[:], scalar1=128.0 * j, scalar2=None, op0=ALU.add))
            Sc.op("dve", lambda e: e.tensor_copy(out=self.rt_w[:].rearrange("p a b -> p (a b)"), in_=wf[:].rearrange("p a b -> p (a b)")),
                  reads=[t_a], writes=[self.t_rt])
            hr = Ring([(sb(f"hrow{i}", [128, D], BF16), Tok()) for i in range(3)])
            hsrc = self.scr["h_tok"].rearrange("(n p) f -> p n f", p=128)
            Xs = self.scr["Xs"]
            for tt in range(64):
                ht, t_ht = hr.next()
                Sc.dma("sp", [(ht[:], hsrc[:, tt, :])], writes=[t_ht])
                fns = [lambda g, k=k: g.indirect_dma_start(out=Xs[:, :], out_offset=bass.IndirectOffsetOnAxis(ap=self.rt_s[:, k, tt:tt + 1], axis=0),
                                                           in_=ht[:, :], in_offset=None) for k in range(2)]
                Sc.dma_fn("pool", fns, reads=[t_ht, self.t_rt], writes=[Tok()])

    def phase_p7(self):
        nc, Sc, I = self.nc, self.S, self.I
        NFC = DFE // 128
        NBLK = int(os.environ.get("K_NBLK", 39))
        with ExitStack() as es:
            sb = lambda n, s, d: es.enter_context(nc.sbuf_tensor(_un(n), s, d))
            ps = lambda n: es.enter_context(nc.psum_tensor(_un(n), [128, TT], F32))
            mmr = Ring([(ps(f"mm{i}"), Tok(x=True)) for i in range(4)])
            yr = Ring([(ps(f"y{i}"), Tok(x=True)) for i in range(4)])
            ident = sb("ident", [128, 128], F32)
            identb = sb("identb", [128, 128], BF16)
            t_c = Tok()
            Sc.dma("sp", [(ident[:], I["ident"][:, :])], writes=[t_c])
            Sc.op("dve", lambda e: e.tensor_copy(out=identb[:], in_=ident[:]), reads=[t_c], writes=[t_c])
            xkr = Ring([(sb(f"xtok{i}", [128, 4, D], BF16), Tok()) for i in range(2)])
            xTr = Ring([(sb(f"xT{i}", [128, 8, TT], BF16), toks(8)) for i in range(2)])
            wgr = Ring([(sb(f"wg{i}", [128, 8, 256], BF16), Tok()) for i in range(3)])
            wur = Ring([(sb(f"wu{i}", [128, 8, 256], BF16), Tok()) for i in range(3)])
            wdr = Ring([(sb(f"wd{i}", [128, 4, 512], BF16), Tok()) for i in range(3)])
            H, t_H = sb("H", [128, NFC, TT], BF16), toks(NFC)
            sgr = Ring([(sb(f"sg{i}", [128, TT], F32), Tok()) for i in range(3)])
            yTr = Ring([(sb(f"yT{i}", [128, TT], F32), Tok()) for i in range(4)])
            ytok, t_yt = sb("ytok", [128, 4, D], F32), Tok()
            Xs = self.scr["Xs"].rearrange("(n p) f -> p n f", p=128)
            Ys = self.scr["Ys"].rearrange("(n p) f -> p n f", p=128)
            gath = lambda dst, src, idx: (lambda g: g.indirect_dma_start(out=dst, out_offset=None, in_=src,
                                                                         in_offset=bass.IndirectOffsetOnAxis(ap=idx, axis=0)))
            loaded = []

            def load_x(b_):
                xk, t_xk = xkr.next()
                Sc.dma("sp", [(xk[:], Xs[:, 4 * b_:4 * b_ + 4, :])], writes=[t_xk])
                loaded.append((xk, t_xk))
            load_x(0)
            for b_ in range(NBLK):
                if b_ + 1 < NBLK:
                    load_x(b_ + 1)
                xk, t_xk = loaded.pop(0)
                xT, t_xT = xTr.next()
                for kc in range(8):
                    pt, t_pt = mmr.next()

                    def f(e):
                        for u in range(4):
                            last = e.matmul(pt[:, u * 128:(u + 1) * 128], lhsT=xk[:, u, kc * 128:(kc + 1) * 128], rhs=identb[:], start=True, stop=True)
                        return last
                    Sc.op("pe", f, reads=[t_xk, t_c], writes=[t_pt])
                    if kc % 2:
                        Sc.op("act", lambda e: e.activation(out=xT[:, kc, :], in_=pt[:], func=AF.Copy), reads=[t_pt], writes=[t_xT[kc]])
                    else:
                        Sc.op("dve", lambda e: e.tensor_copy(out=xT[:, kc, :], in_=pt[:]), reads=[t_pt], writes=[t_xT[kc]])
                for fg in range(NFC // 2):
                    wg, t_wg = wgr.next()
                    wu, t_wu = wur.next()
                    Sc.dma_fn("pool", [gath(wg[:].rearrange("p k c -> p (k c)"), self.scr["mwg2"][:, :], self.rt_w[:, b_, fg:fg + 1])],
                              reads=[self.t_rt], writes=[t_wg])
                    Sc.dma_fn("pool", [gath(wu[:].rearrange("p k c -> p (k c)"), self.scr["mwu2"][:, :], self.rt_w[:, b_, fg:fg + 1])],
                              reads=[self.t_rt], writes=[t_wu])
                    for fl in range(2):
                        f_ = fg * 2 + fl
                        gp_, t_g = mmr.next()
                        up_, t_u = mmr.next()
                        for (pt, tp, w, tw) in ((gp_, t_g, wg, t_wg), (up_, t_u, wu, t_wu)):
                            def f(e):
                                for kc in range(8):
                                    last = e.matmul(pt[:], lhsT=w[:, kc, fl * 128:(fl + 1) * 128], rhs=xT[:, kc, :], start=(kc == 0), stop=(kc == 7))
                                return last
                            Sc.op("pe", f, reads=t_xT + [tw], writes=[tp])
                        sg, t_sg = sgr.next()
                        Sc.op("act", lambda e: e.activation(out=sg[:], in_=gp_[:], func=AF.Silu), reads=[t_g], writes=[t_sg])
                        Sc.op("dve", lambda e: e.tensor_tensor(out=H[:, f_, :], in0=sg[:], in1=up_[:], op=ALU.mult), reads=[t_sg, t_u], writes=[t_H[f_]])
                for half in range(2):
                    ys = [yr.next() for _ in range(4)]
                    for g4 in range(NFC // 4):
                        wd, t_wd = wdr.next()
                        Sc.dma_fn("pool", [gath(wd[:].rearrange("p k c -> p (k c)"), self.scr["mwd2"][:, :], self.rt_w[:, b_, half * 7 + g4:half * 7 + g4 + 1])],
                                  reads=[self.t_rt], writes=[t_wd])
                        for fl in range(4):
                            f_ = g4 * 4 + fl

                            def f(e):
                                for j in range(4):
                                    last = e.matmul(ys[j][0][:], lhsT=wd[:, fl, j * 128:(j + 1) * 128], rhs=H[:, f_, :], start=(f_ == 0), stop=(f_ == NFC - 1))
                                return last
                            Sc.op("pe", f, reads=[t_H[f_], t_wd], writes=[y[1] for y in ys])
                    yts = []
                    for j in range(4):
                        yT, t_yT = yTr.next()
                        if j % 2:
                            Sc.op("act", lambda e: e.activation(out=yT[:], in_=ys[j][0][:], func=AF.Copy), reads=[ys[j][1]], writes=[t_yT])
                        else:
                            Sc.op("dve", lambda e: e.tensor_copy(out=yT[:], in_=ys[j][0][:]), reads=[ys[j][1]], writes=[t_yT])
                        yts.append((yT, t_yT))
                    for u in range(4):
                        pt, t_pt = mmr.next()

                        def f(e):
                            for j in range(4):
                                last = e.matmul(pt[:, j * 128:(j + 1) * 128], lhsT=yts[j][0][:, u * 128:(u + 1) * 128], rhs=ident[:], start=True, stop=True)
                            return last
                        Sc.op("pe", f, reads=[y[1] for y in yts] + [t_c], writes=[t_pt])
                        if u % 2:
                            Sc.op("act", lambda e: e.activation(out=ytok[:, u, half * 512:(half + 1) * 512], in_=pt[:], func=AF.Copy), reads=[t_pt], writes=[t_yt])
                        else:
                            Sc.op("dve", lambda e: e.tensor_copy(out=ytok[:, u, half * 512:(half + 1) * 512], in_=pt[:]), reads=[t_pt], writes=[t_yt])
                Sc.dma("sp", [(Ys[:, 4 * b_:4 * b_ + 4, :], ytok[:])], reads=[t_yt], writes=[Tok()])

    def phase_p8(self):
        nc, Sc, I = self.nc, self.S, self.I
        with ExitStack() as es:
            sb = lambda n, s, d: es.enter_context(nc.sbuf_tensor(_un(n), s, d))
            ps = lambda n: es.enter_context(nc.psum_tensor(_un(n), [128, TT], F32))
            pr = Ring([(ps(f"pp{i}"), Tok(x=True)) for i in range(8)])
            ident = sb("ident", [128, 128], F32)
            t_c = Tok()
            Sc.dma("sp", [(ident[:], I["ident"][:, :])], writes=[t_c])
            y1r = Ring([(sb(f"y1_{i}", [128, D], F32), Tok()) for i in range(3)])
            y2r = Ring([(sb(f"y2_{i}", [128, D], F32), Tok()) for i in range(3)])
            ycr = Ring([(sb(f"yc{i}", [128, 4, D], F32), toks(4)) for i in range(2)])
            xr = Ring([(sb(f"x3_{i}", [128, 8, TT], F32), toks(8)) for i in range(2)])
            x3src = self.scr["x3T"].rearrange("(c p) t -> p c t", p=128)
            odst = self.out.rearrange("(c p) t -> p c t", p=128)
            Ysf = self.scr["Ys"]
            NT_RUN = int(os.environ.get('K_NT8', NT))
            for it in range(NT_RUN):
                b, t0 = it // (S // TT), it * TT
                xt, t_x = xr.next()
                for c in range(8):
                    Sc.dma("sp", [(xt[:, c, :], x3src[:, c, t0:t0 + TT])], writes=[t_x[c]])
                yc, t_yc = ycr.next()
                for u in range(4):
                    tt = it * 4 + u
                    y1, t_y1 = y1r.next()
                    y2, t_y2 = y2r.next()
                    for (yy, ty, k) in ((y1, t_y1, 0), (y2, t_y2, 1)):
                        Sc.dma_fn("pool", [lambda g: g.indirect_dma_start(out=yy[:, :], out_offset=None, in_=Ysf[:, :],
                                                                           in_offset=bass.IndirectOffsetOnAxis(ap=self.rt_s[:, k, tt:tt + 1], axis=0))],
                                  reads=[self.t_rt], writes=[ty])
                    Sc.op("dve", lambda e: e.tensor_scalar(out=yc[:, u, :], in0=y1[:], scalar1=self.rt_g[:, tt, 0:1], scalar2=None, op0=ALU.mult),
                          reads=[t_y1, self.t_rt], writes=[t_yc[u]])
                    Sc.op("pool", lambda e: e.scalar_tensor_tensor(out=yc[:, u, :], in0=y2[:], scalar=self.rt_g[:, tt, 1:2], in1=yc[:, u, :],
                                                                   op0=ALU.mult, op1=ALU.add), reads=[t_y2, self.t_rt], writes=[t_yc[u]]) \
                        if False else \
                        Sc.op("dve", lambda e: e.scalar_tensor_tensor(out=yc[:, u, :], in0=y2[:], scalar=self.rt_g[:, tt, 1:2], in1=yc[:, u, :],
                                                                      op0=ALU.mult, op1=ALU.add), reads=[t_y2, self.t_rt], writes=[t_yc[u]])
                for dc in range(8):
                    pt, t_pt = pr.next()

                    def f(e):
                        for u in range(4):
                            last = e.matmul(pt[:, u * 128:(u + 1) * 128], lhsT=yc[:, u, dc * 128:(dc + 1) * 128], rhs=ident[:], start=True, stop=True)
                        return last
                    Sc.op("pe", f, reads=t_yc + [t_c], writes=[t_pt])
                    Sc.op("dve", lambda e: e.scalar_tensor_tensor(out=xt[:, dc, :], in0=pt[:], scalar=self.M[1][:, 40 + dc, b:b + 1],
                                                                  in1=xt[:, dc, :], op0=ALU.mult, op1=ALU.add),
                          reads=[t_pt, self.t_mod], writes=[t_x[dc]])
                    Sc.dma("sp", [(odst[:, dc, t0:t0 + TT], xt[:, dc, :])], reads=[t_x[dc]], writes=[Tok()])

    def mk_stage(self, es, n=2, w=3584, nb=None):
        nc = self.nc
        nb = n if nb is None else nb
        self.stg = Ring([(es.enter_context(nc.sbuf_tensor(_un(f"stg{i}"), [128, w], F32)), Tok()) for i in range(n)])
        self.stgb = Ring([(es.enter_context(nc.sbuf_tensor(_un(f"stgb{i}"), [128, w], BF16)), Tok()) for i in range(nb)])

    def pump_casts(self, n, eng="pool"):
        if not hasattr(self, "pump_inflight"):
            self.pump_inflight = []
        for _ in range(n):
            if self.cast_jobs:
                job = self.cast_jobs.pop(0)
                st, t_st = self.stg.next()
                w = job[1].shape[-1]
                self.S.dma("pool", [(st[:, 0:w], job[1])], writes=[t_st])
                self.pump_inflight.append((job, st, t_st, w))
            if self.pump_inflight and (len(self.pump_inflight) > 1 or not self.cast_jobs):
                job, st, t_st, w = self.pump_inflight.pop(0)
                sb_, t_sb = self.stgb.next()
                self.S.op(eng, lambda e: e.tensor_copy(out=sb_[:, 0:w], in_=st[:, 0:w]), reads=[t_st], writes=[t_sb])
                src_sb = sb_[:, 0:w]
                if len(job) > 2:
                    src_sb = src_sb.rearrange("p (a b) -> p a b", a=job[2][0])
                self.S.dma("pool", [(job[0], src_sb)], reads=[t_sb], writes=[Tok()])

    def load_cast(self, dst, src, t_dst, eng="pool"):
        w = src.shape[-1]
        st, t_st = self.stg.next()
        self.S.dma("sp", [(st[:, 0:w], src)], writes=[t_st])
        self.S.op(eng, lambda e: e.tensor_copy(out=dst, in_=st[:, 0:w]), reads=[t_st], writes=[t_dst])


ALL_PHASES = ["p0", "p1", "p2", "p3", "p4", "p5", "p6"] if os.environ.get("K_DENSE") else ["p0", "p1", "p2", "p3", "p4", "p5", "p6a", "p6b", "p7", "p8"]


def make_in_maps(inputs, cores=range(NCORES)):
    inp = {k: np.asarray(v) for k, v in inputs.items()}
    sh = _shared_inputs(inp)
    maps = []
    for c in cores:
        xs = inp["x"][NB * c:NB * (c + 1)]
        xT = np.ascontiguousarray(xs.reshape(T, D).T)
        cs = inp["c"][NB * c:NB * (c + 1)]
        cT = np.ascontiguousarray(cs.reshape(NB, 8, 128).transpose(2, 1, 0))
        m = dict(sh)
        m["xT"] = xT
        m["cT"] = cT
        maps.append(m)
    return maps, sh["vecs"].shape[1]


def kernel(**inputs):
    maps, nvec = make_in_maps(inputs)
    prog = Prog(nvec=nvec)
    prog.build(ALL_PHASES)
    res = run_bass_kernel_spmd(prog.nc, maps, core_ids=list(range(NCORES)))
    out = np.empty((NCORES * NB, S, D), np.float32)
    for c in range(NCORES):
        out[NB * c:NB * (c + 1)] = res.results[c]["outT"].T.reshape(NB, S, D)
    return out
```
